# Optimizing a Trainium2 kernel written in Bass

```python
import math
import jax, jax.numpy as jnp
from jax import lax
import numpy as np

D_MODEL = 2048
BATCH = 1
SEQ = 16384
DEPTH = 1

HEAD_DIM = 128
N_HEADS_DSA = 8
N_HEADS_FOX = 8
DSA_WIDTH = N_HEADS_DSA * HEAD_DIM
FOX_WIDTH = N_HEADS_FOX * HEAD_DIM
MIX_WIDTH = DSA_WIDTH + FOX_WIDTH
IDX_HEADS = 8
IDX_DIM = 64
TOPK_MAX = 256
N_BUCKETS = 32
MAX_DISTANCE = 128
Q_BLOCK = 128
D_FF = 5504
CONV_WIDTH = 3
EPS = 1e-6
NEG = -1e30

IN_SPLITS = (
    DSA_WIDTH, DSA_WIDTH, DSA_WIDTH,
    IDX_HEADS * IDX_DIM, IDX_DIM, IDX_HEADS,
    FOX_WIDTH, FOX_WIDTH, FOX_WIDTH,
    FOX_WIDTH, N_HEADS_FOX,
)
IN_WIDTH = sum(IN_SPLITS)

kernel_name = "hybrid_dsa_fox_convffn_adaln"


def rmsnorm(x, g):
    xf = x.astype(jnp.float32)
    y = xf * lax.rsqrt(jnp.mean(xf * xf, axis=-1, keepdims=True) + EPS)
    return (y * g.astype(jnp.float32)).astype(x.dtype)


def modulate(h, shift, scale):
    return h * (1 + scale[:, None, :]) + shift[:, None, :]


def t5_bucket(dist):
    max_exact = N_BUCKETS // 2
    d = jnp.maximum(dist, 0)
    df = jnp.maximum(d, 1).astype(jnp.float32)
    large = max_exact + (jnp.log(df / max_exact) / math.log(MAX_DISTANCE / max_exact)
                         * (N_BUCKETS - max_exact)).astype(jnp.int32)
    large = jnp.minimum(large, N_BUCKETS - 1)
    return jnp.where(d < max_exact, d, large)


def dsa_attention(q, k, v, q_idx, k_idx, w_idx, rel_bias):
    B, S, H, Dh = q.shape
    n_sel = min(TOPK_MAX, S // 4)
    key_pos = jnp.arange(S, dtype=jnp.int32)
    k_idx_f = k_idx.astype(jnp.float32)

    def block(i):
        start = i * Q_BLOCK
        qb = lax.dynamic_slice_in_dim(q, start, Q_BLOCK, axis=1)
        qib = lax.dynamic_slice_in_dim(q_idx, start, Q_BLOCK, axis=1)
        wb = lax.dynamic_slice_in_dim(w_idx, start, Q_BLOCK, axis=1)
        q_pos = start + jnp.arange(Q_BLOCK, dtype=jnp.int32)
        causal = key_pos[None, :] <= q_pos[:, None]
        idx_logits = jnp.einsum('bqhd,bsd->bqhs', qib.astype(jnp.float32), k_idx_f) * (IDX_DIM ** -0.5)
        score = jnp.einsum('bqhs,bqh->bqs', jax.nn.relu(idx_logits),
                           wb.astype(jnp.float32)) * (IDX_HEADS ** -0.5)
        score = jnp.where(causal[None], score, NEG)
        _, sel = lax.top_k(score, n_sel)
        valid = sel <= q_pos[None, :, None]
        k_sel = jax.vmap(lambda kk, ii: kk[ii])(k, sel)
        v_sel = jax.vmap(lambda vv, ii: vv[ii])(v, sel)
        logits = jnp.einsum('bqhd,bqkhd->bhqk', qb.astype(jnp.float32),
                            k_sel.astype(jnp.float32)) * (Dh ** -0.5)
        bucket = t5_bucket(q_pos[None, :, None] - sel)
        bias = rel_bias.astype(jnp.float32)[bucket]
        logits = logits + jnp.transpose(bias, (0, 3, 1, 2))
        logits = jnp.where(valid[:, None], logits, NEG)
        p = jax.nn.softmax(logits, axis=-1)
        out = jnp.einsum('bhqk,bqkhd->bqhd', p, v_sel.astype(jnp.float32))
        return out.astype(v.dtype)

    outs = lax.map(block, jnp.arange(S // Q_BLOCK))
    return jnp.transpose(outs, (1, 0, 2, 3, 4)).reshape(B, S, H, Dh)


def fox_attention(q, k, v, log_f):
    B, S, H, Dh = q.shape
    F = jnp.transpose(jnp.cumsum(log_f, axis=1), (0, 2, 1))
    key_pos = jnp.arange(S, dtype=jnp.int32)
    k_f = k.astype(jnp.float32)
    v_f = v.astype(jnp.float32)

    def block(i):
        start = i * Q_BLOCK
        qb = lax.dynamic_slice_in_dim(q, start, Q_BLOCK, axis=1)
        Fq = lax.dynamic_slice_in_dim(F, start, Q_BLOCK, axis=2)
        q_pos = start + jnp.arange(Q_BLOCK, dtype=jnp.int32)
        causal = key_pos[None, :] <= q_pos[:, None]
        logits = jnp.einsum('bqhd,bshd->bhqs', qb.astype(jnp.float32), k_f) * (Dh ** -0.5)
        logits = logits + (Fq[..., None] - F[:, :, None, :])
        logits = jnp.where(causal[None, None], logits, NEG)
        p = jax.nn.softmax(logits, axis=-1)
        out = jnp.einsum('bhqs,bshd->bqhd', p, v_f)
        return out.astype(v.dtype)

    outs = lax.map(block, jnp.arange(S // Q_BLOCK))
    return jnp.transpose(outs, (1, 0, 2, 3, 4)).reshape(B, S, H, Dh)


def causal_dwconv(u, w, b):
    C = u.shape[-1]
    y = lax.conv_general_dilated(
        u, w[:, None, :].astype(u.dtype), window_strides=(1,),
        padding=[(CONV_WIDTH - 1, 0)], dimension_numbers=('NWC', 'WIO', 'NWC'),
        feature_group_count=C)
    return y + b


def setup_inputs(seed: int = 0) -> dict:
    key = jax.random.key(seed)
    ks = jax.random.split(key, 16)
    D = D_MODEL
    nrm = jax.random.normal
    x = nrm(ks[0], (BATCH, SEQ, D), jnp.float32)
    c = nrm(ks[1], (BATCH, D), jnp.float32)
    rel_bias = 0.5 * nrm(ks[2], (N_BUCKETS, N_HEADS_DSA), jnp.float32)
    w_ada = nrm(ks[3], (DEPTH, D, 6 * D), jnp.float32) * (0.5 * D ** -0.5)
    b_ada = 0.01 * nrm(ks[4], (DEPTH, 6 * D), jnp.float32)
    g_attn = 1.0 + 0.02 * nrm(ks[5], (DEPTH, D), jnp.float32)
    w_in = nrm(ks[6], (DEPTH, D, IN_WIDTH), jnp.float32) * D ** -0.5
    b_forget = jax.random.uniform(ks[7], (DEPTH, N_HEADS_FOX), jnp.float32, minval=1.0, maxval=4.0)
    w_out = nrm(ks[8], (DEPTH, MIX_WIDTH, D), jnp.float32) * MIX_WIDTH ** -0.5
    g_mlp = 1.0 + 0.02 * nrm(ks[9], (DEPTH, D), jnp.float32)
    w_up = nrm(ks[10], (DEPTH, D, 2 * D_FF), jnp.float32) * D ** -0.5
    conv_w = nrm(ks[11], (DEPTH, CONV_WIDTH, 2 * D_FF), jnp.float32) * CONV_WIDTH ** -0.5
    conv_b = 0.01 * nrm(ks[12], (DEPTH, 2 * D_FF), jnp.float32)
    w_down = nrm(ks[13], (DEPTH, D_FF, D), jnp.float32) * D_FF ** -0.5
    g_final = 1.0 + 0.02 * nrm(ks[14], (D,), jnp.float32)
    return {"x": x, "c": c, "rel_bias": rel_bias, "w_ada": w_ada, "b_ada": b_ada,
            "g_attn": g_attn, "w_in": w_in, "b_forget": b_forget, "w_out": w_out,
            "g_mlp": g_mlp, "w_up": w_up, "conv_w": conv_w, "conv_b": conv_b,
            "w_down": w_down, "g_final": g_final}


def reference(x, c, rel_bias, w_ada, b_ada, g_attn, w_in, b_forget, w_out,
              g_mlp, w_up, conv_w, conv_b, w_down, g_final):
    B, S, D = x.shape
    split_at = np.cumsum(IN_SPLITS)[:-1].tolist()
    c_act = jax.nn.silu(c)
    for l in range(DEPTH):
        mod = c_act @ w_ada[l] + b_ada[l]
        shift_a, scale_a, gate_a, shift_m, scale_m, gate_m = jnp.split(mod, 6, axis=-1)

        h = modulate(rmsnorm(x, g_attn[l]), shift_a, scale_a)
        proj = h @ w_in[l]
        (qa, ka, va, qi, ki, wi, qb, kb, vb, gb, fb) = jnp.split(proj, split_at, axis=-1)
        qa = qa.reshape(B, S, N_HEADS_DSA, HEAD_DIM)
        ka = ka.reshape(B, S, N_HEADS_DSA, HEAD_DIM)
        va = va.reshape(B, S, N_HEADS_DSA, HEAD_DIM)
        qi = qi.reshape(B, S, IDX_HEADS, IDX_DIM)
        o_a = dsa_attention(qa, ka, va, qi, ki, wi, rel_bias)

        qb = qb.reshape(B, S, N_HEADS_FOX, HEAD_DIM)
        kb = kb.reshape(B, S, N_HEADS_FOX, HEAD_DIM)
        vb = vb.reshape(B, S, N_HEADS_FOX, HEAD_DIM)
        log_f = jax.nn.log_sigmoid((fb + b_forget[l]).astype(jnp.float32))
        o_b = fox_attention(qb, kb, vb, log_f)
        o_b = o_b.reshape(B, S, FOX_WIDTH) * jax.nn.sigmoid(gb)

        mixed = jnp.concatenate([o_a.reshape(B, S, DSA_WIDTH), o_b], axis=-1)
        x = x + gate_a[:, None, :] * (mixed @ w_out[l])

        h = modulate(rmsnorm(x, g_mlp[l]), shift_m, scale_m)
        u = causal_dwconv(h @ w_up[l], conv_w[l], conv_b[l])
        u_gate, u_val = jnp.split(u, 2, axis=-1)
        x = x + gate_m[:, None, :] * ((jax.nn.silu(u_gate) * u_val) @ w_down[l])

    return rmsnorm(x, g_final)
```

```python
import math
from contextlib import ExitStack

import numpy as np
import ml_dtypes

import concourse.bass as bass
import concourse.mybir as mybir
from concourse.bass_utils import run_bass_kernel_spmd

F32 = mybir.dt.float32
BF = mybir.dt.bfloat16
U8 = mybir.dt.uint8
AF = mybir.ActivationFunctionType
ALU = mybir.AluOpType
AX = mybir.AxisListType

D = 2048
SEQ = 16384
NCORE = 8
NT = 17
NBLK = 134
NTOK = NBLK * 128
SEG = NTOK // 16
NEG = -1.0e30
EPS = 1e-6
NITER = 24
BW = 512
DFF = 5504
NG_UP = 86
NFC = 43


def Lk(k):
    return 1008 * k + 882


def pad_of(i):
    return 884 - 126 * i


class Dep:
    __slots__ = ("w", "r", "p", "ds")

    def __init__(self):
        self.w = {}
        self.r = {}
        self.p = {}
        self.ds = None

    def reset(self, key, ev):
        p = dict(self.w)
        for k, (s_, v) in self.r.items():
            if k not in p or p[k][1] < v:
                p[k] = (s_, v)
        self.p = p
        self.w = {key: ev}
        self.r = {}


class DSem:
    def __init__(self, sem, key):
        self.sem = sem
        self.key = key
        self.n = 0


class Sch:
    def __init__(self, nc, es):
        self.nc = nc
        self.es = es
        self.E = {"pe": nc.tensor, "act": nc.scalar, "dve": nc.vector, "pool": nc.gpsimd, "sp": nc.sync}
        self.sem = {k: es.enter_context(nc.semaphore("s_" + k)) for k in self.E}
        self.cnt = {k: 0 for k in self.E}
        self.waited = {k: {} for k in self.E}
        self.dsems = []

    def _need(self, rd, wr, acc):
        need = {}

        def add(dct):
            for k, (s, v) in dct.items():
                if k not in need or need[k][1] < v:
                    need[k] = (s, v)

        for d in rd:
            add(d.w)
        for d in wr:
            add(d.w)
            add(d.r)
        for d in acc:
            add(d.r)
            add(d.p)
        return need

    def _wait(self, eng, need):
        for key, (sem, val) in need.items():
            if self.waited[eng].get(key, 0) < val:
                self.E[eng].wait_ge(sem, val)
                self.waited[eng][key] = val

    def op(self, eng, fn, rd=(), wr=(), acc=()):
        need = self._need(rd, wr, acc)
        if eng == "pe":
            need.pop("pe", None)
        self._wait(eng, need)
        inst = fn()
        self.cnt[eng] += 1
        v = self.cnt[eng]
        inst.then_inc(self.sem[eng], 1)
        ev = (self.sem[eng], v)
        for d in wr:
            d.reset(eng, ev)
        for d in acc:
            d.w[eng] = ev
        for d in rd:
            d.r[eng] = ev
        return inst

    def _dsem(self, d):
        if d.ds is None:
            key = "d%d" % len(self.dsems)
            sem = self.es.enter_context(self.nc.semaphore(key))
            d.ds = DSem(sem, key)
            self.dsems.append(d.ds)
        return d.ds

    def dma(self, q, out, in_, rd=(), wr=(), acc=(), own=None, **kw):
        need = self._need(rd, wr, acc)
        self._wait(q, need)
        inst = self.E[q].dma_start(out=out, in_=in_, **kw)
        ds = self._dsem(own)
        ds.n += 16
        inst.then_inc(ds.sem, 16)
        ev = (ds.sem, ds.n)
        for d in wr:
            d.reset(ds.key, ev)
        for d in acc:
            d.w[ds.key] = ev
        for d in rd:
            d.r[ds.key] = ev
        return inst

    def barrier(self):
        need = {k: (self.sem[k], self.cnt[k]) for k in self.E if self.cnt[k] > 0}
        for ds in self.dsems:
            if ds.n > 0:
                need[ds.key] = (ds.sem, ds.n)
        for e in self.E:
            nd = dict(need)
            if e in nd and e != "sp":
                pass
            self._wait(e, nd)


class Ring:
    def __init__(self, tiles):
        self.t = tiles
        self.d = [Dep() for _ in tiles]
        self.i = -1

    def nxt(self):
        self.i = (self.i + 1) % len(self.t)
        return self.t[self.i], self.d[self.i]


RUN_NT = NT
RUN_NBLK = NBLK


def build_program():
    NT_R, NBLK_R = RUN_NT, RUN_NBLK
    nc = bass.Bass("TRN2", target_bir_lowering=False)

    def din(name, shape, dt=F32):
        return nc.dram_tensor(name, list(shape), dt, kind="ExternalInput").ap()

    def dscr(name, shape, dt):
        return nc.dram_tensor(name, list(shape), dt, kind="Internal").ap()

    xl = din("xl", [NTOK, D])
    cT_d = din("cT", [128, 16])
    gattn_d = din("gattn", [128, 16])
    gmlp_d = din("gmlp", [128, 16])
    bada_d = din("bada", [128, 96])
    wada_d = din("wada", [96, 128, 2048])
    wkf_d = din("wkf", [17, 128, 2048])
    wfb_d = din("wfb", [128, 128])
    wv_d = din("wv", [128, 16 * 2048])
    wqf_d = din("wqf", [28, 128, 2048])
    wwi_d = din("wwi", [128, 128])
    bfor_d = din("bfor", [128, 1])
    wout_d = din("wout", [4, 128, 16 * 512])
    wup_d = din("wup", [NG_UP, 128, 2048])
    cw_d = din("cw", [128, 3 * NG_UP])
    cb_d = din("cb", [128, NG_UP])
    wdown_d = din("wdown", [NFC, 128, 2048])
    gF_d = din("gF", [128, D])
    identf_d = din("identf", [128, 128])
    tm_d = din("tm", [128, 128])
    padb_d = din("padb", [128, 1024])
    Ib_d = din("Ib", [128, 512])
    Cb_d = din("Cb", [128, BW])
    bankB_d = din("bankB", [128, 8 * BW])
    rb31_d = din("rb31", [128, 8])
    hv_d = din("hv", [128, NT])
    fixl_t = din("fixl_t", [8, NTOK], BF)
    fixr_t = din("fixr_t", [8, NTOK], BF)

    y = nc.dram_tensor("y", [NT * 128, D], F32, kind="ExternalOutput").ap()

    KT = dscr("KT", [16, 128, NTOK], BF)
    V = dscr("V", [NTOK, 2048], BF)
    KI = dscr("KI", [128, NTOK], BF)
    FBT = dscr("FBT", [8, NTOK], F32)
    FIXL = dscr("FIXL", [8, 8, NTOK], BF)
    FIXR = dscr("FIXR", [8, 8, NTOK], BF)
    QS = dscr("QS", [20, 128, NT * 128], BF)
    GS = dscr("GS", [8, 128, NT * 128], F32)
    WIS = dscr("WIS", [NT * 128, 8], F32)
    MIX = dscr("MIX", [NT, 128, 2048], BF)
    dKT, dV, dKI, dFBT, dFIXL, dFIXR, dQS, dGS, dWIS, dMIX, dY = [Dep() for _ in range(11)]

    with ExitStack() as es:
        es.enter_context(nc.allow_low_precision("bf16 matmul operands, fp32 accumulation"))
        s = Sch(nc, es)
        op, dma = s.op, s.dma
        V_, A_, P_ = nc.vector, nc.scalar, nc.tensor

        nmc = [0]

        def sbt(stack, name, shape, dt):
            nmc[0] += 1
            return stack.enter_context(nc.sbuf_tensor("sb%d_%s" % (nmc[0], name), list(shape), dt))

        def ring(stack, name, n, shape, dt):
            return Ring([sbt(stack, "%s%d" % (name, i), shape, dt) for i in range(n)])

        PS = [es.enter_context(nc.psum_tensor("ps%d" % i, [128, 512], F32)) for i in range(7)]
        PSD = [[Dep()] * 4 for _ in range(7)]
        PST = es.enter_context(nc.psum_tensor("pst", [128, 1024], BF))
        PSTD = [Dep()] * 2

        identf = sbt(es, "identf", [128, 128], F32)
        identb = sbt(es, "identb", [128, 128], BF)
        onesb = sbt(es, "onesb", [128, 128], BF)
        onesf = sbt(es, "onesf", [128, 128], F32)
        modT = sbt(es, "modT", [128, 96], F32)
        aA = sbt(es, "aA", [128, 16], F32)
        aM = sbt(es, "aM", [128, 16], F32)
        hv = sbt(es, "hv", [128, NT], F32)
        dC = Dep()
        dma("sp", identf[:], identf_d, wr=[dC], own=dC)
        dma("sp", hv[:], hv_d, acc=[dC], own=dC)
        op("dve", lambda: V_.tensor_copy(out=identb[:], in_=identf[:]), rd=[dC], acc=[dC])
        op("dve", lambda: V_.memset(onesb[:], 1.0), acc=[dC])
        op("dve", lambda: V_.memset(onesf[:], 1.0), acc=[dC])

        with ExitStack() as ph:
            cact = sbt(ph, "cact", [128, 16], F32)
            g1 = sbt(ph, "g1", [128, 16], F32)
            g2 = sbt(ph, "g2", [128, 16], F32)
            bada = sbt(ph, "bada", [128, 96], F32)
            tmp16 = sbt(ph, "tmp16", [128, 16], F32)
            d0 = Dep()
            dma("sp", cact[:], cT_d, wr=[d0], own=d0)
            dma("sp", g1[:], gattn_d, acc=[d0], own=d0)
            dma("sp", g2[:], gmlp_d, acc=[d0], own=d0)
            dma("sp", bada[:], bada_d, acc=[d0], own=d0)
            dca = Dep()
            op("act", lambda: A_.activation(out=cact[:], in_=cact[:], func=AF.Silu), rd=[d0], wr=[dca])
            war = ring(ph, "wa", 3, [128, 2048], F32)
            for g in range(96):
                wt, wd = war.nxt()
                dma("sp", wt[:], wada_d[g], wr=[wd], own=wd)
                for c in range(16):
                    op("pe", lambda: P_.matmul(PS[0][:, g:g + 1], wt[:, c * 128:(c + 1) * 128], cact[:, c:c + 1],
                                               start=(c == 0), stop=(c == 15)),
                       rd=[wd, dca], wr=PSD[0] if (g == 0 and c == 0) else (), acc=() if (g == 0 and c == 0) else PSD[0])
            dmod = Dep()
            op("dve", lambda: V_.tensor_tensor(out=modT[:], in0=PS[0][:, 0:96], in1=bada[:], op=ALU.add),
               rd=PSD[0] + [d0], wr=[dmod])
            op("dve", lambda: V_.tensor_scalar(out=tmp16[:], in0=modT[:, 16:32], scalar1=1.0, scalar2=None, op0=ALU.add),
               rd=[dmod], wr=[d0])
            op("dve", lambda: V_.tensor_tensor(out=aA[:], in0=tmp16[:], in1=g1[:], op=ALU.mult), rd=[d0], acc=[dC])
            op("dve", lambda: V_.tensor_scalar(out=tmp16[:], in0=modT[:, 64:80], scalar1=1.0, scalar2=None, op0=ALU.add),
               rd=[dmod], wr=[d0])
            op("dve", lambda: V_.tensor_tensor(out=aM[:], in0=tmp16[:], in1=g2[:], op=ALU.mult), rd=[d0], acc=[dC])
            s.barrier()
        bA = modT[:, 0:16]
        bM = modT[:, 48:64]

        def norm_transpose(stack_tiles, x_ap, x_dep, aT, bT, dst_fn, dst_dep, first):
            junk, ss, ms, sd, rstd, xn, dsm, dxn = stack_tiles
            op("act", lambda: A_.activation(out=junk[:], in_=x_ap, func=AF.Square, accum_out=ss[:]),
               rd=[x_dep], wr=[dsm])
            op("dve", lambda: V_.tensor_scalar(out=ms[:], in0=ss[:], scalar1=1.0 / D, scalar2=EPS, op0=ALU.mult, op1=ALU.add),
               rd=[dsm], wr=[dsm])
            op("act", lambda: A_.activation(out=sd[:], in_=ms[:], func=AF.Sqrt), rd=[dsm], wr=[dsm])
            op("dve", lambda: V_.reciprocal(out=rstd[:], in_=sd[:]), rd=[dsm], wr=[dsm])
            op("dve", lambda: V_.tensor_scalar(out=xn[:], in0=x_ap, scalar1=rstd[:, 0:1], scalar2=None, op0=ALU.mult),
               rd=[dsm, x_dep], wr=[dxn])
            for j in range(4):
                b = 5 + j % 2
                for q in range(4):
                    c = 4 * j + q
                    op("pe", lambda: P_.transpose(PS[b][:, q * 128:(q + 1) * 128], xn[:, c * 128:(c + 1) * 128], identf[:]),
                       rd=[dxn, dC], wr=[PSD[b][q]] if q == 0 else (), acc=() if q == 0 else [PSD[b][q]])
                for q in range(4):
                    c = 4 * j + q
                    op("act", lambda: A_.activation(out=dst_fn(c), in_=PS[b][:, q * 128:(q + 1) * 128], func=AF.Identity,
                                                    scale=aT[:, c:c + 1], bias=bT[:, c:c + 1]),
                       rd=[PSD[b][q], dmod, dC], wr=[dst_dep] if (first and c == 0) else (), acc=() if (first and c == 0) else [dst_dep])

        ntc = [0]

        def nt_tiles(stack):
            ntc[0] += 1
            u = "_%d" % ntc[0]
            return (sbt(stack, "junk" + u, [128, D], BF), sbt(stack, "ss" + u, [128, 1], F32), sbt(stack, "ms" + u, [128, 1], F32),
                    sbt(stack, "sd" + u, [128, 1], F32), sbt(stack, "rstd" + u, [128, 1], F32), sbt(stack, "xn" + u, [128, D], F32),
                    Dep(), Dep())

        bankctr = [0]

        def next_bank(lo=0, hi=5):
            bankctr[0] = (bankctr[0] + 1) % (hi - lo)
            return lo + bankctr[0]

        evctr = [0]

        def evac_copy(out_ap, in_ap, rd, wr=(), acc=(), scale=None):
            evctr[0] += 1
            if evctr[0] % 2 == 0:
                if scale is None:
                    op("act", lambda: A_.copy(out=out_ap, in_=in_ap), rd=rd, wr=wr, acc=acc)
                else:
                    op("act", lambda: A_.activation(out=out_ap, in_=in_ap, func=AF.Copy, scale=scale), rd=rd, wr=wr, acc=acc)
            else:
                if scale is None:
                    op("dve", lambda: V_.tensor_copy(out=out_ap, in_=in_ap), rd=rd, wr=wr, acc=acc)
                else:
                    op("dve", lambda: V_.tensor_scalar(out=out_ap, in0=in_ap, scalar1=scale, scalar2=None, op0=ALU.mult),
                       rd=rd, wr=wr, acc=acc)

        def proj_fm(wring, hT, hT_dep, ntok, wsrc, M, evac):
            wt, wd = wring.nxt()
            dma("pool", wt[:, :16 * M], wsrc, wr=[wd], own=wd)
            for t0 in range(0, ntok, 512):
                n = min(512, ntok - t0)
                b = next_bank()
                for c in range(16):
                    op("pe", lambda: P_.matmul(PS[b][:M, :n], wt[:, c * M:(c + 1) * M], hT[:, c, t0:t0 + n],
                                               start=(c == 0), stop=(c == 15)),
                       rd=[wd, hT_dep], wr=PSD[b] if c == 0 else (), acc=() if c == 0 else PSD[b])
                evac(b, t0, n)

        with ExitStack() as ph:
            SB = 8
            hT = sbt(ph, "hT", [128, 16, SB * 128], BF)
            dhT = Dep()
            wv = sbt(ph, "wv", [128, 16 * 2048], BF)
            dwv = Dep()
            for j in range(16):
                dma("pool", wv[:, j * 2048:(j + 1) * 2048], wv_d[:, j * 2048:(j + 1) * 2048],
                    wr=[dwv] if j == 0 else (), acc=() if j == 0 else [dwv], own=dwv)
            xr = ring(ph, "xb", 2, [128, D], F32)
            ntt = nt_tiles(ph)
            wgr = ring(ph, "wg", 3, [128, 2048], BF)
            stg = ring(ph, "stg", 2, [128, SB * 128], BF)
            stf = sbt(ph, "stf", [8, SB * 128], F32)
            dstf = Dep()
            vst = ring(ph, "vst", 2, [128, 2048], BF)
            for sb0 in range(0, NBLK_R, SB):
                nb = min(SB, NBLK_R - sb0)
                ntok = nb * 128
                tok0 = sb0 * 128
                for bi in range(nb):
                    xt, xd = xr.nxt()
                    dma("sp", xt[:], xl[tok0 + bi * 128: tok0 + (bi + 1) * 128, :], wr=[xd], own=xd)
                    norm_transpose(ntt, xt[:], xd, aA, bA, lambda c: hT[:, c, bi * 128:(bi + 1) * 128], dhT, bi == 0)
                for g in range(17):
                    st, sd_ = stg.nxt()

                    def ev(b, t0, n, st=st, sd_=sd_):
                        evac_copy(st[:, t0:t0 + n], PS[b][:, :n], rd=PSD[b], wr=[sd_] if t0 == 0 else (),
                                  acc=() if t0 == 0 else [sd_])
                    proj_fm(wgr, hT, dhT, ntok, wkf_d[g], 128, ev)
                    if g < 16:
                        dma("sp", KT[g, :, tok0:tok0 + ntok], st[:, :ntok], rd=[sd_], acc=[dKT], own=sd_)
                    else:
                        dma("sp", KI[:, tok0:tok0 + ntok], st[:, :ntok], rd=[sd_], acc=[dKI], own=sd_)

                def evf(b, t0, n):
                    evac_copy(stf[:, t0:t0 + n], PS[b][:8, :n], rd=PSD[b], wr=[dstf] if t0 == 0 else (),
                              acc=() if t0 == 0 else [dstf])
                proj_fm(wgr, hT, dhT, ntok, wfb_d, 8, evf)
                dma("sp", FBT[:, tok0:tok0 + ntok], stf[:, :ntok], rd=[dstf], acc=[dFBT], own=dstf)
                for bi in range(nb):
                    vt, vd = vst.nxt()
                    for cg in range(4):
                        b = next_bank()
                        for c in range(16):
                            op("pe", lambda: P_.matmul(PS[b][:, :], hT[:, c, bi * 128:(bi + 1) * 128],
                                                       wv[:, c * 2048 + cg * 512: c * 2048 + (cg + 1) * 512],
                                                       start=(c == 0), stop=(c == 15)),
                               rd=[dwv, dhT], wr=PSD[b] if c == 0 else (), acc=() if c == 0 else PSD[b])
                        evac_copy(vt[:, cg * 512:(cg + 1) * 512], PS[b][:, :], rd=PSD[b], wr=[vd] if cg == 0 else (),
                                  acc=() if cg == 0 else [vd])
                    dma("sp", V[tok0 + bi * 128: tok0 + (bi + 1) * 128, :], vt[:], rd=[vd], acc=[dV], own=vd)
            s.barrier()

        with ExitStack() as ph:
            fl = sbt(ph, "fl", [128, SEG], F32)
            e1 = sbt(ph, "e1", [128, SEG], F32)
            onesg = sbt(ph, "onesg", [128, SEG], F32)
            Fc = sbt(ph, "Fc", [128, SEG], F32)
            r1 = sbt(ph, "r1", [128, SEG], F32)
            r2 = sbt(ph, "r2", [128, SEG], F32)
            fb16 = [sbt(ph, "fb16_%d" % i, [128, SEG], BF) for i in range(6)]
            tm = sbt(ph, "tm", [128, 128], F32)
            nbf = sbt(ph, "nbf", [128, 1], F32)
            off = sbt(ph, "off", [128, 1], F32)
            tot = sbt(ph, "tot", [128, 1], F32)
            dF = Dep()
            dF2 = Dep()
            dma("sp", fl[:], FBT.rearrange("h (g n) -> (h g) n", n=SEG), rd=[dFBT], wr=[dF], own=dF)
            dma("sp", tm[:], tm_d, acc=[dF], own=dF)
            dma("sp", nbf[:], bfor_d, acc=[dF], own=dF)
            dTL = Dep()
            for h in range(8):
                dma("sp", FIXL[:, h, :], fixl_t, acc=[dFIXL, dTL], own=dTL)
                dma("sp", FIXR[:, h, :], fixr_t, acc=[dFIXR, dTL], own=dTL)
            op("dve", lambda: V_.tensor_scalar(out=nbf[:], in0=nbf[:], scalar1=-1.0, scalar2=None, op0=ALU.mult),
               rd=[dF], wr=[dF2])
            op("dve", lambda: V_.memset(onesg[:], 1.0), acc=[dF2])
            op("act", lambda: A_.activation(out=e1[:], in_=fl[:], func=AF.Exp, scale=-1.0, bias=nbf[:, 0:1]),
               rd=[dF, dF2], acc=[dF2])
            op("act", lambda: A_.activation(out=e1[:], in_=e1[:], func=AF.Ln, bias=1.0, scale=1.0), rd=[dF2], wr=[dF2])
            op("dve", lambda: V_.tensor_tensor_scan(Fc[:], onesg[:], e1[:], 0.0, ALU.mult, ALU.subtract),
               rd=[dF2], wr=[dF])
            op("dve", lambda: V_.tensor_copy(out=tot[:], in_=Fc[:, SEG - 1:SEG]), rd=[dF], acc=[dF])
            op("pe", lambda: P_.matmul(PS[0][:, 0:1], tm[:], tot[:], start=True, stop=True), rd=[dF], wr=PSD[0])
            op("dve", lambda: V_.tensor_copy(out=off[:], in_=PS[0][:, 0:1]), rd=PSD[0], acc=[dF])
            op("dve", lambda: V_.tensor_scalar(out=Fc[:], in0=Fc[:], scalar1=off[:, 0:1], scalar2=None, op0=ALU.add),
               rd=[dF], wr=[dF])
            op("dve", lambda: V_.tensor_copy(out=fb16[0][:], in_=Fc[:]), rd=[dF], wr=[dF2])
            op("dve", lambda: V_.tensor_tensor(out=r1[:], in0=Fc[:], in1=fb16[0][:], op=ALU.subtract), rd=[dF, dF2], acc=[dF2])
            op("dve", lambda: V_.tensor_copy(out=fb16[1][:], in_=r1[:]), rd=[dF2], acc=[dF2])
            op("dve", lambda: V_.tensor_tensor(out=r2[:], in0=r1[:], in1=fb16[1][:], op=ALU.subtract), rd=[dF2], acc=[dF2])
            op("dve", lambda: V_.tensor_copy(out=fb16[2][:], in_=r2[:]), rd=[dF2], acc=[dF2])
            for j in range(3):
                op("dve", lambda: V_.tensor_scalar(out=fb16[3 + j][:], in0=fb16[j][:], scalar1=-1.0, scalar2=None, op0=ALU.mult),
                   rd=[dF2], acc=[dF2])
            dst = Dep()
            for j in range(3):
                dma("sp", FIXR[j].rearrange("h (g n) -> (h g) n", n=SEG), fb16[j][:], rd=[dF2, dTL], acc=[dFIXR], own=dst)
                dma("sp", FIXL[3 + j].rearrange("h (g n) -> (h g) n", n=SEG), fb16[3 + j][:], rd=[dF2, dTL], acc=[dFIXL], own=dst)
            s.barrier()

        with ExitStack() as ph:
            hq = sbt(ph, "hq", [128, 16, 512], BF)
            dhq = Dep()
            xr = ring(ph, "xq", 2, [128, D], F32)
            ntt = nt_tiles(ph)
            wgr = ring(ph, "wgq", 3, [128, 2048], BF)
            stq = ring(ph, "stq", 2, [128, 512], BF)
            stgf = ring(ph, "stgf", 2, [128, 512], F32)
            wwi = sbt(ph, "wwi", [128, 128], BF)
            wis = sbt(ph, "wis", [128, 8], F32)
            dwwi = Dep()
            dwis = Dep()
            dma("pool", wwi[:], wwi_d, wr=[dwwi], own=dwwi)
            for k0 in range(0, NT_R, 4):
                nt_ = min(4, NT_R - k0)
                ntok = nt_ * 128
                for ti in range(nt_):
                    k = k0 + ti
                    xt, xd = xr.nxt()
                    dma("sp", xt[:], xl[Lk(k): Lk(k) + 128, :], wr=[xd], own=xd)
                    norm_transpose(ntt, xt[:], xd, aA, bA, lambda c: hq[:, c, ti * 128:(ti + 1) * 128], dhq, ti == 0)
                for g in range(28):
                    if 16 <= g < 24:
                        st, sd_ = stgf.nxt()

                        def ev(b, t0, n, st=st, sd_=sd_):
                            op("act", lambda: A_.activation(out=st[:, t0:t0 + n], in_=PS[b][:, :n], func=AF.Sigmoid),
                               rd=PSD[b], wr=[sd_])
                        proj_fm(wgr, hq, dhq, ntok, wqf_d[g], 128, ev)
                        dma("sp", GS[g - 16, :, k0 * 128:k0 * 128 + ntok], st[:, :ntok], rd=[sd_], acc=[dGS], own=sd_)
                    else:
                        st, sd_ = stq.nxt()
                        sc = (128.0 ** -0.5) if g < 16 else 1.0

                        def ev(b, t0, n, st=st, sd_=sd_, sc=sc):
                            evac_copy(st[:, t0:t0 + n], PS[b][:, :n], rd=PSD[b], wr=[sd_], scale=sc)
                        proj_fm(wgr, hq, dhq, ntok, wqf_d[g], 128, ev)
                        gi = g if g < 16 else g - 8
                        dma("sp", QS[gi, :, k0 * 128:k0 * 128 + ntok], st[:, :ntok], rd=[sd_], acc=[dQS], own=sd_)
                for ti in range(nt_):
                    k = k0 + ti
                    b = next_bank()
                    for c in range(16):
                        op("pe", lambda: P_.matmul(PS[b][:, 0:8], hq[:, c, ti * 128:(ti + 1) * 128], wwi[:, c * 8:(c + 1) * 8],
                                                   start=(c == 0), stop=(c == 15)),
                           rd=[dwwi, dhq], wr=PSD[b] if c == 0 else (), acc=() if c == 0 else PSD[b])
                    op("dve", lambda: V_.tensor_copy(out=wis[:], in_=PS[b][:, 0:8]), rd=PSD[b], wr=[dwis])
                    dma("sp", WIS[k * 128:(k + 1) * 128, :], wis[:], rd=[dwis], acc=[dWIS], own=dwis)
            s.barrier()

        with ExitStack() as ph:
            SMAX = NTOK
            scores = sbt(ph, "scores", [128, SMAX], F32)
            dsc = Dep()
            maskT = sbt(ph, "maskT", [128, SMAX], U8)
            dmk = Dep()
            qT = sbt(ph, "qT", [128, 20, 128], BF)
            dq = Dep()
            wi = sbt(ph, "wi", [128, 8], F32)
            dwi = Dep()
            dg = sbt(ph, "dg", [128, 8, 128], BF)
            ddg = Dep()
            kir = ring(ph, "kich", 3, [128, 512], BF)
            rbr = ring(ph, "rbuf", 3, [128, 512], BF)
            mcr = ring(ph, "mch", 2, [128, 512], BF)
            ktr = ring(ph, "kt", 2, [128, 8, 256], BF)
            vcr = ring(ph, "vch", 2, [128, 2, 1024], BF)
            ptr_ = ring(ph, "pT", 3, [128, 512], BF)
            pmr = ring(ph, "pm", 3, [128, 512], BF)
            bankH = sbt(ph, "bankH", [128, 8, BW], BF)
            Cb = sbt(ph, "Cb", [128, BW], BF)
            Ib = sbt(ph, "Ib", [128, 512], F32)
            padb = sbt(ph, "padb", [128, 1024], F32)
            rb31 = sbt(ph, "rb31", [128, 8], F32)
            flr = ring(ph, "fixl", 2, [8, 8, 256], BF)
            fixr = sbt(ph, "fixr", [8, 8, 128], BF)
            dfr = Dep()
            GT = sbt(ph, "GT", [128, 8, 128], F32)
            dGT = Dep()
            rr = sbt(ph, "rr", [128, 1024], F32)
            drr = Dep()
            mixT = sbt(ph, "mixT", [128, 16, 128], BF)
            dmx = Dep()
            sm = {n: sbt(ph, "sm_" + n, [128, 1], F32) for n in ("mx", "mn", "w0", "lo", "mid", "cnt", "pred")}
            dsm = Dep()
            dK = Dep()
            bankf = scores[:, 0:8 * BW].rearrange("p (h u) -> p h u", u=BW)
            dma("sp", scores[:, 0:8 * BW], bankB_d, wr=[dsc], own=dsc)
            dma("sp", rb31[:], rb31_d, wr=[dK], own=dK)
            dma("sp", Ib[:], Ib_d, acc=[dK], own=dK)
            dma("sp", padb[:], padb_d, acc=[dK], own=dK)
            dK2 = Dep()
            dma("pool", Cb[:], Cb_d, wr=[dK2], own=dK2)
            dKb = Dep()
            for h in range(8):
                op("dve", lambda: V_.tensor_scalar(out=bankH[:, h, :], in0=bankf[:, h, :], scalar1=rb31[:, h:h + 1], scalar2=None,
                                                   op0=ALU.subtract), rd=[dK, dsc], acc=[dKb])

            for k in range(NT_R):
                L = Lk(k)
                bq, o = L // 128, L % 128
                NB = bq + 2
                S = NB * 128
                dma("sp", qT[:], QS[:, :, k * 128:(k + 1) * 128].rearrange("g p t -> p g t"), rd=[dQS], wr=[dq], own=dq)
                dma("sp", wi[:], WIS[k * 128:(k + 1) * 128, :], rd=[dWIS], wr=[dwi], own=dwi)
                dma("sp", GT[:], GS[:, :, k * 128:(k + 1) * 128].rearrange("g p t -> p g t"), rd=[dGS], wr=[dGT], own=dGT)
                dma("sp", fixr[:], FIXR[:, :, L:L + 128], rd=[dFIXR], wr=[dfr], own=dfr)
                for h in range(8):
                    op("dve", lambda: V_.tensor_scalar(out=dg[:, h, :], in0=identf[:], scalar1=wi[:, h:h + 1], scalar2=None,
                                                       op0=ALU.mult), rd=[dwi, dC], wr=[ddg] if h == 0 else (),
                       acc=() if h == 0 else [ddg])
                for c0 in range(0, S, 512):
                    n = min(512, S - c0)
                    kt_, kd = kir.nxt()
                    dma("sp", kt_[:, :n], KI[:, c0:c0 + n], rd=[dKI], wr=[kd], own=kd)
                    ba = 3 + (c0 // 512) % 2
                    for h in range(8):
                        g, po = h // 2, 64 * (h % 2)
                        b = (c0 // 512 * 8 + h) % 3
                        op("pe", lambda: P_.matmul(PS[b][:, :n], qT[po:po + 64, 16 + g, :], kt_[po:po + 64, :n],
                                                   start=True, stop=True), rd=[dq, kd], wr=PSD[b])
                        rt, rd_ = rbr.nxt()
                        op("act", lambda: A_.activation(out=rt[:, :n], in_=PS[b][:, :n], func=AF.Relu), rd=PSD[b], wr=[rd_])
                        op("pe", lambda: P_.matmul(PS[ba][:, :n], dg[:, h, :], rt[:, :n], start=(h == 0), stop=(h == 7)),
                           rd=[ddg, rd_], wr=PSD[ba] if h == 0 else (), acc=() if h == 0 else PSD[ba])
                    evac_copy(scores[:, c0:c0 + n], PS[ba][:, :n], rd=PSD[ba], wr=[dsc] if c0 == 0 else (),
                              acc=() if c0 == 0 else [dsc])
                op("dve", lambda: V_.tensor_reduce(out=sm["mx"][:], in_=scores[:, :S], axis=AX.X, op=ALU.max), rd=[dsc], wr=[dsm])
                op("dve", lambda: V_.tensor_reduce(out=sm["mn"][:], in_=scores[:, :S], axis=AX.X, op=ALU.min), rd=[dsc], acc=[dsm])
                op("dve", lambda: V_.tensor_tensor(out=scores[:, 0:1024], in0=scores[:, 0:1024], in1=padb[:], op=ALU.add),
                   rd=[dsc, dK], wr=[dsc])
                op("dve", lambda: V_.tensor_tensor(out=scores[:, bq * 128:bq * 128 + 256], in0=scores[:, bq * 128:bq * 128 + 256],
                                                   in1=Ib[:, 128 - o:128 - o + 256], op=ALU.add), rd=[dsc, dK], wr=[dsc])
                op("dve", lambda: V_.tensor_tensor(out=sm["w0"][:], in0=sm["mx"][:], in1=sm["mn"][:], op=ALU.subtract),
                   rd=[dsm], wr=[dsm])
                op("dve", lambda: V_.tensor_scalar(out=sm["w0"][:], in0=sm["w0"][:], scalar1=1.0001, scalar2=1e-6,
                                                   op0=ALU.mult, op1=ALU.add), rd=[dsm], wr=[dsm])
                op("dve", lambda: V_.tensor_copy(out=sm["lo"][:], in_=sm["mn"][:]), rd=[dsm], wr=[dsm])
                for it in range(NITER):
                    hf = 2.0 ** -(it + 1)
                    op("dve", lambda: V_.scalar_tensor_tensor(out=sm["mid"][:], in0=sm["w0"][:], scalar=hf, in1=sm["lo"][:],
                                                              op0=ALU.mult, op1=ALU.add), rd=[dsm], wr=[dsm])
                    op("dve", lambda: V_.tensor_scalar(out=maskT[:, :S], in0=scores[:, :S], scalar1=sm["mid"][:, 0:1], scalar2=None,
                                                       op0=ALU.is_ge, op1=ALU.add, accum_out=sm["cnt"][:]),
                       rd=[dsc, dsm], wr=[dmk, dsm])
                    op("dve", lambda: V_.tensor_scalar(out=sm["pred"][:], in0=sm["cnt"][:], scalar1=255.5, scalar2=hf,
                                                       op0=ALU.is_ge, op1=ALU.mult), rd=[dsm], wr=[dsm])
                    op("dve", lambda: V_.scalar_tensor_tensor(out=sm["lo"][:], in0=sm["pred"][:], scalar=sm["w0"][:, 0:1],
                                                              in1=sm["lo"][:], op0=ALU.mult, op1=ALU.add), rd=[dsm], wr=[dsm])
                for c0 in range(0, S, 512):
                    n = min(512, S - c0)
                    mt, md = mcr.nxt()
                    op("dve", lambda: V_.tensor_scalar(out=mt[:, :n], in0=scores[:, c0:c0 + n], scalar1=sm["lo"][:, 0:1], scalar2=None,
                                                       op0=ALU.is_ge), rd=[dsc, dsm], wr=[md])
                    hlf = (c0 // 512) % 2
                    for jb in range(n // 128):
                        op("pe", lambda: P_.transpose(PST[:, hlf * 512 + jb * 128: hlf * 512 + (jb + 1) * 128],
                                                      mt[:, jb * 128:(jb + 1) * 128], identb[:]),
                           rd=[md, dC], wr=[PSTD[hlf]] if jb == 0 else (), acc=() if jb == 0 else [PSTD[hlf]])
                    op("act", lambda: A_.copy(out=maskT[:, c0:c0 + n], in_=PST[:, hlf * 512: hlf * 512 + n]),
                       rd=[PSTD[hlf]], wr=[dmk] if c0 == 0 else (), acc=() if c0 == 0 else [dmk])

                for fox in (0, 1):
                    for b2 in range(0, NB, 2):
                        kt_, kd = ktr.nxt()
                        dma("sp", kt_[:], KT[8 * fox:8 * fox + 8, :, b2 * 128:b2 * 128 + 256].rearrange("h d s -> d h s"),
                            rd=[dKT], wr=[kd], own=kd)
                        vt_, vd = vcr.nxt()
                        dma("sp", vt_[:], V[b2 * 128:b2 * 128 + 256, 1024 * fox:1024 * fox + 1024].rearrange("(b s) c -> s b c", s=128),
                            rd=[dV], wr=[vd], own=vd)
                        if fox:
                            fl_, fd = flr.nxt()
                            dma("sp", fl_[:], FIXL[:, :, b2 * 128:b2 * 128 + 256], rd=[dFIXL], wr=[fd], own=fd)
                        for bb in range(2):
                            b = b2 + bb
                            if b >= NB:
                                continue
                            m = bq - b
                            ws = o + 128 * m + 128
                            for grp in range(2):
                                pb = 4 + (b * 2 + grp) % 3
                                for hh in range(4):
                                    h = 4 * grp + hh
                                    reg = PS[pb][:, hh * 128:(hh + 1) * 128]
                                    extra = (fox == 1) or (m <= 1)
                                    op("pe", lambda: P_.matmul(reg, kt_[:, h, bb * 128:(bb + 1) * 128], qT[:, 8 * fox + h, :],
                                                               start=True, stop=not extra),
                                       rd=[kd, dq], wr=[PSD[pb][hh]])
                                    if fox:
                                        last = not (m <= 0)
                                        op("pe", lambda: P_.matmul(reg, fl_[0:8, h, bb * 128:(bb + 1) * 128], fixr[0:8, h, :],
                                                                   start=False, stop=last),
                                           rd=[fd, dfr], acc=[PSD[pb][hh]])
                                        if m <= 0:
                                            op("pe", lambda: P_.matmul(reg, identb[:], Cb[:, ws:ws + 128], start=False, stop=True),
                                               rd=[dC, dK2], acc=[PSD[pb][hh]])
                                    elif m <= 1:
                                        op("pe", lambda: P_.matmul(reg, identb[:], bankH[:, h, ws:ws + 128], start=False, stop=True),
                                           rd=[dC, dKb], acc=[PSD[pb][hh]])
                                pt, pd = ptr_.nxt()
                                op("act", lambda: A_.activation(out=pt[:], in_=PS[pb][:, :], func=AF.Exp), rd=PSD[pb], wr=[pd])
                                if fox:
                                    pm_, pmd = pt, pd
                                else:
                                    pm_, pmd = pmr.nxt()
                                    op("dve", lambda: V_.tensor_tensor(
                                        out=pm_[:].rearrange("p (h t) -> p h t", h=4),
                                        in0=pt[:].rearrange("p (h t) -> p h t", h=4),
                                        in1=maskT[:, b * 128:(b + 1) * 128].unsqueeze(1).broadcast_to([128, 4, 128]),
                                        op=ALU.mult), rd=[pd, dmk], wr=[pmd])
                                for hh in range(4):
                                    h = 4 * grp + hh
                                    first = (b == 0 and hh == 0)
                                    op("pe", lambda: P_.matmul(PS[grp][:, hh * 128:(hh + 1) * 128],
                                                               vt_[:, bb, h * 128:(h + 1) * 128], pm_[:, hh * 128:(hh + 1) * 128],
                                                               start=first, stop=(b == NB - 1), skip_group_check=True),
                                       rd=[vd, pmd], wr=PSD[grp] if first else (), acc=() if first else PSD[grp])
                                op("pe", lambda: P_.matmul(PS[2 + grp][:, :], onesb[:], pm_[:], start=(b == 0), stop=(b == NB - 1)),
                                   rd=[dC, pmd], wr=PSD[2 + grp] if b == 0 else (), acc=() if b == 0 else PSD[2 + grp])
                    for grp in range(2):
                        op("dve", lambda: V_.tensor_scalar(out=rr[:, grp * 512:(grp + 1) * 512], in0=PS[2 + grp][:, :], scalar1=1e-30,
                                                           scalar2=None, op0=ALU.max), rd=PSD[2 + grp], wr=[drr] if grp == 0 else (),
                           acc=() if grp == 0 else [drr])
                    op("dve", lambda: V_.reciprocal(out=rr[:], in_=rr[:]), rd=[drr], wr=[drr])
                    for grp in range(2):
                        first = (fox == 0 and grp == 0)
                        if fox:
                            op("dve", lambda: V_.tensor_tensor(out=rr[:, grp * 512:(grp + 1) * 512], in0=rr[:, grp * 512:(grp + 1) * 512],
                                                               in1=GT[:, 4 * grp:4 * grp + 4, :].rearrange("p h t -> p (h t)"),
                                                               op=ALU.mult), rd=[drr, dGT], wr=[drr])
                        op("dve", lambda: V_.tensor_tensor(out=mixT[:, 8 * fox + 4 * grp: 8 * fox + 4 * grp + 4, :].rearrange("p h t -> p (h t)"),
                                                           in0=PS[grp][:, :], in1=rr[:, grp * 512:(grp + 1) * 512], op=ALU.mult),
                           rd=PSD[grp] + [drr], wr=[dmx] if first else (), acc=() if first else [dmx])
                dma("sp", MIX[k], mixT[:].rearrange("p h t -> p (h t)"), rd=[dmx], acc=[dMIX], own=dmx)
            s.barrier()

        with ExitStack() as ph:
            CB = 3
            xs = sbt(ph, "xs", [128, CB, D], F32)
            dxs = [Dep() for _ in range(CB)]
            h2 = sbt(ph, "h2", [128, 16, CB * 128], BF)
            dh2 = Dep()
            ntt = nt_tiles(ph)
            mixb = sbt(ph, "mixb", [128, 16, 128], BF)
            dmb = Dep()
            wo = sbt(ph, "wo", [128, 16 * 512], BF)
            dwo = Dep()
            gA = sbt(ph, "gA", [128, D], F32)
            gM = sbt(ph, "gM", [128, D], F32)
            gFt = sbt(ph, "gFt", [128, D], F32)
            dgt = Dep()
            wgr = ring(ph, "wgu", 3, [128, 2048], BF)
            aT = sbt(ph, "aT", [128, NFC, CB * 128], BF)
            daT = Dep()
            t1r = ring(ph, "t1", 2, [128, 512], F32)
            t2r = ring(ph, "t2", 2, [128, 512], F32)
            wdr = ring(ph, "wd", 3, [128, 2048], BF)
            cw = sbt(ph, "cw", [128, 3 * NG_UP], F32)
            cb = sbt(ph, "cb", [128, NG_UP], F32)
            dgr = sbt(ph, "dgr", [128, 128], F32)
            ddgr = Dep()
            dma("sp", cw[:], cw_d, wr=[dgt], own=dgt)
            dma("sp", cb[:], cb_d, acc=[dgt], own=dgt)
            dma("sp", gFt[:], gF_d, acc=[dgt], own=dgt)
            for (gt_, col0) in ((gA, 32), (gM, 80)):
                for c in range(16):
                    op("dve", lambda: V_.tensor_scalar(out=dgr[:], in0=identf[:], scalar1=modT[:, col0 + c:col0 + c + 1], scalar2=None,
                                                       op0=ALU.mult), rd=[dC, dmod], wr=[ddgr])
                    b = next_bank()
                    op("pe", lambda: P_.matmul(PS[b][:, 0:128], onesf[:], dgr[:], start=True, stop=True), rd=[dC, ddgr], wr=PSD[b])
                    op("act", lambda: A_.copy(out=gt_[:, c * 128:(c + 1) * 128], in_=PS[b][:, 0:128]), rd=PSD[b], acc=[dgt])

            for k0 in range(0, NT_R, CB):
                nt_ = min(CB, NT_R - k0)
                ntok = nt_ * 128
                for ti in range(nt_):
                    k = k0 + ti
                    dma("sp", xs[:, ti, :], xl[Lk(k):Lk(k) + 128, :], wr=[dxs[ti]], own=dxs[ti])
                    dma("sp", mixb[:].rearrange("p h t -> p (h t)"), MIX[k], rd=[dMIX], wr=[dmb], own=dmb)
                    for cg in range(4):
                        dma("pool", wo[:], wout_d[cg], wr=[dwo], own=dwo, max_dma_last_dim=8192)
                        b = next_bank()
                        for c in range(16):
                            op("pe", lambda: P_.matmul(PS[b][:, :], mixb[:, c, :], wo[:, c * 512:(c + 1) * 512],
                                                       start=(c == 0), stop=(c == 15)),
                               rd=[dmb, dwo], wr=PSD[b] if c == 0 else (), acc=() if c == 0 else PSD[b])
                        t1, t1d = t1r.nxt()
                        op("dve", lambda: V_.tensor_tensor(out=t1[:], in0=PS[b][:, :], in1=gA[:, cg * 512:(cg + 1) * 512], op=ALU.mult),
                           rd=PSD[b] + [dgt], wr=[t1d])
                        op("dve", lambda: V_.tensor_tensor(out=xs[:, ti, cg * 512:(cg + 1) * 512], in0=xs[:, ti, cg * 512:(cg + 1) * 512],
                                                           in1=t1[:], op=ALU.add), rd=[t1d], wr=[dxs[ti]])
                    norm_transpose(ntt, xs[:, ti, :], dxs[ti], aM, bM, lambda c: h2[:, c, ti * 128:(ti + 1) * 128], dh2, ti == 0)
                for g in range(NFC):
                    res = []
                    for half in range(2):
                        gg = g + NFC * half
                        wt, wd = wgr.nxt()
                        dma("pool", wt[:], wup_d[gg], wr=[wd], own=wd)
                        b = next_bank()
                        for c in range(16):
                            op("pe", lambda: P_.matmul(PS[b][:, :ntok], wt[:, c * 128:(c + 1) * 128], h2[:, c, :ntok],
                                                       start=(c == 0), stop=(c == 15)),
                               rd=[wd, dh2], wr=PSD[b] if c == 0 else (), acc=() if c == 0 else PSD[b])
                        pv = PS[b][:, :ntok].rearrange("p (t r) -> p t r", r=128)
                        op("dve", lambda: V_.tensor_tensor(out=pv[:, :, 0:2], in0=pv[:, :, 0:2],
                                                           in1=hv[:, k0:k0 + nt_].unsqueeze(2).broadcast_to([128, nt_, 2]),
                                                           op=ALU.mult), rd=PSD[b] + [dC], wr=PSD[b])
                        t, td = (t1r if half == 0 else t2r).nxt()
                        tv = t[:, :ntok].rearrange("p (t r) -> p t r", r=128)
                        op("act", lambda: A_.activation(out=t[:, :ntok], in_=PS[b][:, :ntok], func=AF.Identity,
                                                        scale=cw[:, 2 * NG_UP + gg:2 * NG_UP + gg + 1], bias=cb[:, gg:gg + 1]),
                           rd=PSD[b] + [dgt], wr=[td])
                        op("dve", lambda: V_.scalar_tensor_tensor(out=tv[:, :, 2:128], in0=pv[:, :, 1:127],
                                                                  scalar=cw[:, NG_UP + gg:NG_UP + gg + 1], in1=tv[:, :, 2:128],
                                                                  op0=ALU.mult, op1=ALU.add), rd=PSD[b] + [td, dgt], wr=[td])
                        op("dve", lambda: V_.scalar_tensor_tensor(out=tv[:, :, 2:128], in0=pv[:, :, 0:126],
                                                                  scalar=cw[:, gg:gg + 1], in1=tv[:, :, 2:128],
                                                                  op0=ALU.mult, op1=ALU.add), rd=PSD[b] + [td, dgt], wr=[td])
                        res.append((t, td))
                    (tg, tgd), (tv_, tvd) = res
                    op("act", lambda: A_.activation(out=tg[:, :ntok], in_=tg[:, :ntok], func=AF.Silu), rd=[tgd], wr=[tgd])
                    op("dve", lambda: V_.tensor_tensor(out=aT[:, g, :ntok], in0=tg[:, :ntok], in1=tv_[:, :ntok], op=ALU.mult),
                       rd=[tgd, tvd], wr=[daT] if g == 0 else (), acc=() if g == 0 else [daT])
                for ti in range(nt_):
                    k = k0 + ti
                    for f in range(NFC):
                        wt, wd = wdr.nxt()
                        dma("pool", wt[:], wdown_d[f], wr=[wd], own=wd)
                        for cg in range(4):
                            op("pe", lambda: P_.matmul(PS[cg][:, :], aT[:, f, ti * 128:(ti + 1) * 128], wt[:, cg * 512:(cg + 1) * 512],
                                                       start=(f == 0), stop=(f == NFC - 1)),
                               rd=[wd, daT], wr=PSD[cg] if f == 0 else (), acc=() if f == 0 else PSD[cg])
                    for cg in range(4):
                        t1, t1d = t1r.nxt()
                        op("dve", lambda: V_.tensor_tensor(out=t1[:], in0=PS[cg][:, :], in1=gM[:, cg * 512:(cg + 1) * 512], op=ALU.mult),
                           rd=PSD[cg] + [dgt], wr=[t1d])
                        op("dve", lambda: V_.tensor_tensor(out=xs[:, ti, cg * 512:(cg + 1) * 512], in0=xs[:, ti, cg * 512:(cg + 1) * 512],
                                                           in1=t1[:], op=ALU.add), rd=[t1d], wr=[dxs[ti]])
                    junk, ss, ms, sd, rstd, xn, dsm2, dxn = ntt
                    xa = xs[:, ti, :]
                    op("act", lambda: A_.activation(out=junk[:], in_=xa, func=AF.Square, accum_out=ss[:]), rd=[dxs[ti]], wr=[dsm2])
                    op("dve", lambda: V_.tensor_scalar(out=ms[:], in0=ss[:], scalar1=1.0 / D, scalar2=EPS, op0=ALU.mult, op1=ALU.add),
                       rd=[dsm2], wr=[dsm2])
                    op("act", lambda: A_.activation(out=sd[:], in_=ms[:], func=AF.Sqrt), rd=[dsm2], wr=[dsm2])
                    op("dve", lambda: V_.reciprocal(out=rstd[:], in_=sd[:]), rd=[dsm2], wr=[dsm2])
                    op("dve", lambda: V_.scalar_tensor_tensor(out=xn[:], in0=xa, scalar=rstd[:, 0:1], in1=gFt[:], op0=ALU.mult, op1=ALU.mult),
                       rd=[dsm2, dxs[ti], dgt], wr=[dxn])
                    dma("sp", y[k * 128:(k + 1) * 128, :], xn[:], rd=[dxn], acc=[dY], own=dxn)
            s.barrier()
    return nc


def _t5_bucket_np(d):
    d = np.maximum(d, 0)
    df = np.maximum(d, 1).astype(np.float32)
    large = 16 + (np.log(df / np.float32(16)) / np.float32(math.log(8.0)) * np.float32(16)).astype(np.int32)
    large = np.minimum(large, 31)
    return np.where(d < 16, d, large)


def _lay(w, gsz):
    ncols = w.shape[1]
    G = ncols // gsz
    return np.ascontiguousarray(w.reshape(16, 128, G, gsz).transpose(2, 1, 0, 3)).reshape(G, 128, 16 * gsz)


_PROGRAM = None


def prep(x, c, rel_bias, w_ada, b_ada, g_attn, w_in, b_forget, w_out, g_mlp, w_up, conv_w, conv_b, w_down, g_final):
    f32 = np.float32
    x = np.asarray(x, f32)[0]
    w_in = np.asarray(w_in, f32)[0]
    qa, ka, va = w_in[:, 0:1024], w_in[:, 1024:2048], w_in[:, 2048:3072]
    qi, ki, wi = w_in[:, 3072:3584], w_in[:, 3584:3648], w_in[:, 3648:3656]
    qb, kb, vb = w_in[:, 3656:4680], w_in[:, 4680:5704], w_in[:, 5704:6728]
    gb, fb = w_in[:, 6728:7752], w_in[:, 7752:7760]

    common = {}
    common["cT"] = np.ascontiguousarray(np.asarray(c, f32)[0].reshape(16, 128).T)
    common["gattn"] = np.ascontiguousarray(np.asarray(g_attn, f32)[0].reshape(16, 128).T)
    common["gmlp"] = np.ascontiguousarray(np.asarray(g_mlp, f32)[0].reshape(16, 128).T)
    common["bada"] = np.ascontiguousarray(np.asarray(b_ada, f32)[0].reshape(96, 128).T)
    common["wada"] = _lay(np.asarray(w_ada, f32)[0], 128)
    common["wkf"] = _lay(np.concatenate([ka, kb, ki, ki], axis=1), 128)
    wfb = np.zeros((128, 128), f32)
    wfb[:, :] = fb.reshape(16, 128, 8).transpose(1, 0, 2).reshape(128, 128)
    common["wfb"] = wfb
    common["wv"] = np.ascontiguousarray(np.concatenate([va, vb], axis=1).reshape(16, 128, 2048).transpose(1, 0, 2)).reshape(128, 16 * 2048)
    common["wqf"] = _lay(np.concatenate([qa, qb, gb, qi], axis=1), 128)
    common["wwi"] = np.ascontiguousarray(wi.reshape(16, 128, 8).transpose(1, 0, 2)).reshape(128, 128)
    common["bfor"] = np.ascontiguousarray(np.repeat(np.asarray(b_forget, f32)[0], 16).reshape(128, 1))
    wo = np.asarray(w_out, f32)[0]
    common["wout"] = np.ascontiguousarray(wo.reshape(16, 128, 4, 512).transpose(2, 1, 0, 3)).reshape(4, 128, 16 * 512)
    common["wup"] = _lay(np.asarray(w_up, f32)[0], 128)
    cwv = np.asarray(conv_w, f32)[0]
    common["cw"] = np.ascontiguousarray(cwv.reshape(3, NG_UP, 128).transpose(2, 0, 1)).reshape(128, 3 * NG_UP)
    common["cb"] = np.ascontiguousarray(np.asarray(conv_b, f32)[0].reshape(NG_UP, 128).T)
    common["wdown"] = np.ascontiguousarray(np.asarray(w_down, f32)[0].reshape(NFC, 128, 2048))
    common["gF"] = np.ascontiguousarray(np.broadcast_to(np.asarray(g_final, f32)[None, :], (128, D)))
    common["identf"] = np.eye(128, dtype=f32)
    tm = np.zeros((128, 128), f32)
    for h in range(8):
        for s2 in range(16):
            tm[h * 16: h * 16 + s2, h * 16 + s2] = 1.0
    common["tm"] = tm
    tl = np.arange(128)[:, None]
    u = np.arange(512)[None, :]
    common["Ib"] = np.where(u - 128 <= tl, 0.0, NEG).astype(f32)
    ub = np.arange(BW)[None, :]
    dd = ub - tl - 128
    common["Cb"] = np.where(dd < 0, NEG, 0.0).astype(f32)
    rb = np.asarray(rel_bias, f32)
    bk = _t5_bucket_np(dd)
    bank = rb[bk]
    bank = np.where((dd >= 0)[:, :, None], bank, np.float32(0.0))
    common["bankB"] = np.ascontiguousarray(bank.transpose(0, 2, 1)).reshape(128, 8 * BW).astype(f32)
    common["rb31"] = np.ascontiguousarray(np.broadcast_to(rb[31][None, :], (128, 8))).astype(f32)

    in_maps = []
    for i in range(NCORE):
        pad = pad_of(i)
        n = min(SEQ, NTOK - pad)
        xloc = np.zeros((NTOK, D), f32)
        xloc[pad:pad + n] = x[:n]
        p = np.arange(NTOK)
        isp = (p < pad) | (p >= pad + SEQ)
        padrow = np.where(isp, NEG, 0.0).astype(f32)
        m = dict(common)
        m["xl"] = xloc
        m["padb"] = np.ascontiguousarray(np.broadcast_to(padrow[None, :1024], (128, 1024)))
        hvv = np.ones((NT,), f32)
        if i == 0:
            hvv[0] = 0.0
        m["hv"] = np.ascontiguousarray(np.broadcast_to(hvv[None, :], (128, NT)))
        fl_t = np.zeros((8, NTOK), f32)
        fl_t[0:3] = 1.0
        fl_t[6] = padrow
        fr_t = np.zeros((8, NTOK), f32)
        fr_t[3:7] = 1.0
        m["fixl_t"] = fl_t.astype(ml_dtypes.bfloat16)
        m["fixr_t"] = fr_t.astype(ml_dtypes.bfloat16)
        in_maps.append(m)
    return in_maps


def kernel(**inputs):
    global _PROGRAM
    f32 = np.float32
    in_maps = prep(**inputs)
    if _PROGRAM is None:
        _PROGRAM = build_program()
    res = run_bass_kernel_spmd(_PROGRAM, in_maps, core_ids=list(range(NCORE)), trace=True)
    out = np.zeros((1, SEQ, D), f32)
    for i in range(NCORE):
        yi = np.asarray(res.results[i]["y"]).reshape(NT, 128, D)
        for k in range(NT):
            j = 8 * k + i
            g0 = 126 * j
            if g0 >= SEQ:
                continue
            cnt = min(126, SEQ - g0)
            out[0, g0:g0 + cnt] = yi[k, 2:2 + cnt]
    return out
```

```python
import math
from contextlib import ExitStack

import numpy as np
import ml_dtypes

import concourse.bass as bass
import concourse.mybir as mybir
from concourse.bass_utils import run_bass_kernel_spmd

F32 = mybir.dt.float32
BF = mybir.dt.bfloat16
U8 = mybir.dt.uint8
AF = mybir.ActivationFunctionType
ALU = mybir.AluOpType
AX = mybir.AxisListType

D = 2048
SEQ = 16384
NCORE = 8
NT = 17
NBLK = 134
NTOK = NBLK * 128
SEG = NTOK // 16
NEG = -1.0e30
EPS = 1e-6
NITER = 24
BW = 512
DFF = 5504
NG_UP = 86
NFC = 43


def Lk(k):
    return 1008 * k + 882


def pad_of(i):
    return 884 - 126 * i


class Dep:
    __slots__ = ("w", "r", "p", "ds")

    def __init__(self):
        self.w = {}
        self.r = {}
        self.p = {}
        self.ds = None

    def reset(self, key, ev):
        p = dict(self.w)
        for k, (s_, v) in self.r.items():
            if k not in p or p[k][1] < v:
                p[k] = (s_, v)
        self.p = p
        self.w = {key: ev}
        self.r = {}


class DSem:
    def __init__(self, sem, key):
        self.sem = sem
        self.key = key
        self.n = 0


class Sch:
    def __init__(self, nc, es):
        self.nc = nc
        self.es = es
        self.E = {"pe": nc.tensor, "act": nc.scalar, "dve": nc.vector, "pool": nc.gpsimd, "sp": nc.sync}
        self.sem = {k: es.enter_context(nc.semaphore("s_" + k)) for k in self.E}
        self.cnt = {k: 0 for k in self.E}
        self.waited = {k: {} for k in self.E}
        self.dsems = []

    def _need(self, rd, wr, acc):
        need = {}

        def add(dct):
            for k, (s, v) in dct.items():
                if k not in need or need[k][1] < v:
                    need[k] = (s, v)

        for d in rd:
            add(d.w)
        for d in wr:
            add(d.w)
            add(d.r)
        for d in acc:
            add(d.r)
            add(d.p)
        return need

    def _wait(self, eng, need):
        for key, (sem, val) in need.items():
            if self.waited[eng].get(key, 0) < val:
                self.E[eng].wait_ge(sem, val)
                self.waited[eng][key] = val

    def op(self, eng, fn, rd=(), wr=(), acc=()):
        need = self._need(rd, wr, acc)
        if eng == "pe":
            need.pop("pe", None)
        self._wait(eng, need)
        inst = fn()
        self.cnt[eng] += 1
        v = self.cnt[eng]
        inst.then_inc(self.sem[eng], 1)
        ev = (self.sem[eng], v)
        for d in wr:
            d.reset(eng, ev)
        for d in acc:
            d.w[eng] = ev
        for d in rd:
            d.r[eng] = ev
        return inst

    def _dsem(self, d):
        if d.ds is None:
            key = "d%d" % len(self.dsems)
            sem = self.es.enter_context(self.nc.semaphore(key))
            d.ds = DSem(sem, key)
            self.dsems.append(d.ds)
        return d.ds

    def dma(self, q, out, in_, rd=(), wr=(), acc=(), own=None, **kw):
        need = self._need(rd, wr, acc)
        self._wait(q, need)
        inst = self.E[q].dma_start(out=out, in_=in_, **kw)
        ds = self._dsem(own)
        ds.n += 16
        inst.then_inc(ds.sem, 16)
        ev = (ds.sem, ds.n)
        for d in wr:
            d.reset(ds.key, ev)
        for d in acc:
            d.w[ds.key] = ev
        for d in rd:
            d.r[ds.key] = ev
        return inst

    def barrier(self):
        need = {k: (self.sem[k], self.cnt[k]) for k in self.E if self.cnt[k] > 0}
        for ds in self.dsems:
            if ds.n > 0:
                need[ds.key] = (ds.sem, ds.n)
        for e in self.E:
            nd = dict(need)
            if e in nd and e != "sp":
                pass
            self._wait(e, nd)


class Ring:
    def __init__(self, tiles):
        self.t = tiles
        self.d = [Dep() for _ in tiles]
        self.i = -1

    def nxt(self):
        self.i = (self.i + 1) % len(self.t)
        return self.t[self.i], self.d[self.i]


RUN_NT = NT
RUN_NBLK = NBLK


def build_program():
    NT_R, NBLK_R = RUN_NT, RUN_NBLK
    nc = bass.Bass("TRN2", target_bir_lowering=False)

    def din(name, shape, dt=F32):
        return nc.dram_tensor(name, list(shape), dt, kind="ExternalInput").ap()

    def dscr(name, shape, dt):
        return nc.dram_tensor(name, list(shape), dt, kind="Internal").ap()

    xl = din("xl", [NTOK, D])
    cT_d = din("cT", [128, 16])
    gattn_d = din("gattn", [128, 16])
    gmlp_d = din("gmlp", [128, 16])
    bada_d = din("bada", [128, 96])
    wada_d = din("wada", [96, 128, 2048])
    wkf_d = din("wkf", [17, 128, 2048])
    wfb_d = din("wfb", [128, 128])
    wv_d = din("wv", [128, 16 * 2048])
    wqf_d = din("wqf", [28, 128, 2048])
    wwi_d = din("wwi", [128, 128])
    bfor_d = din("bfor", [128, 1])
    wout_d = din("wout", [4, 128, 16 * 512])
    wup_d = din("wup", [NG_UP, 128, 2048])
    cw_d = din("cw", [128, 3 * NG_UP])
    cb_d = din("cb", [128, NG_UP])
    wdown_d = din("wdown", [NFC, 128, 2048])
    gF_d = din("gF", [128, D])
    identf_d = din("identf", [128, 128])
    tm_d = din("tm", [128, 128])
    padb_d = din("padb", [128, 1024])
    Ib_d = din("Ib", [128, 512])
    Cb_d = din("Cb", [128, BW])
    bankB_d = din("bankB", [128, 8 * BW])
    rb31_d = din("rb31", [128, 8])
    hv_d = din("hv", [128, NT])
    fixl_t = din("fixl_t", [8, NTOK], BF)
    fixr_t = din("fixr_t", [8, NTOK], BF)

    y = nc.dram_tensor("y", [NT * 128, D], F32, kind="ExternalOutput").ap()

    KT = dscr("KT", [16, 128, NTOK], BF)
    V = dscr("V", [NTOK, 2048], BF)
    KI = dscr("KI", [128, NTOK], BF)
    FBT = dscr("FBT", [8, NTOK], F32)
    FIXL = dscr("FIXL", [8, 8, NTOK], BF)
    FIXR = dscr("FIXR", [8, 8, NTOK], BF)
    QS = dscr("QS", [20, 128, NT * 128], BF)
    GS = dscr("GS", [8, 128, NT * 128], F32)
    WIS = dscr("WIS", [NT * 128, 8], F32)
    MIX = dscr("MIX", [NT, 128, 2048], BF)
    dKT, dV, dKI, dFBT, dFIXL, dFIXR, dQS, dGS, dWIS, dMIX, dY = [Dep() for _ in range(11)]

    with ExitStack() as es:
        es.enter_context(nc.allow_low_precision("bf16 matmul operands, fp32 accumulation"))
        s = Sch(nc, es)
        op, dma = s.op, s.dma
        V_, A_, P_ = nc.vector, nc.scalar, nc.tensor

        nmc = [0]

        def sbt(stack, name, shape, dt):
            nmc[0] += 1
            return stack.enter_context(nc.sbuf_tensor("sb%d_%s" % (nmc[0], name), list(shape), dt))

        def ring(stack, name, n, shape, dt):
            return Ring([sbt(stack, "%s%d" % (name, i), shape, dt) for i in range(n)])

        PS = [es.enter_context(nc.psum_tensor("ps%d" % i, [128, 512], F32)) for i in range(7)]
        PSD = [[Dep()] * 4 for _ in range(7)]
        PST = es.enter_context(nc.psum_tensor("pst", [128, 1024], BF))
        PSTD = [Dep()] * 2

        identf = sbt(es, "identf", [128, 128], F32)
        identb = sbt(es, "identb", [128, 128], BF)
        onesb = sbt(es, "onesb", [128, 128], BF)
        onesf = sbt(es, "onesf", [128, 128], F32)
        modT = sbt(es, "modT", [128, 96], F32)
        aA = sbt(es, "aA", [128, 16], F32)
        aM = sbt(es, "aM", [128, 16], F32)
        hv = sbt(es, "hv", [128, NT], F32)
        dC = Dep()
        dma("sp", identf[:], identf_d, wr=[dC], own=dC)
        dma("sp", hv[:], hv_d, acc=[dC], own=dC)
        op("dve", lambda: V_.tensor_copy(out=identb[:], in_=identf[:]), rd=[dC], acc=[dC])
        op("dve", lambda: V_.memset(onesb[:], 1.0), acc=[dC])
        op("dve", lambda: V_.memset(onesf[:], 1.0), acc=[dC])

        with ExitStack() as ph:
            cact = sbt(ph, "cact", [128, 16], F32)
            g1 = sbt(ph, "g1", [128, 16], F32)
            g2 = sbt(ph, "g2", [128, 16], F32)
            bada = sbt(ph, "bada", [128, 96], F32)
            tmp16 = sbt(ph, "tmp16", [128, 16], F32)
            d0 = Dep()
            dma("sp", cact[:], cT_d, wr=[d0], own=d0)
            dma("sp", g1[:], gattn_d, acc=[d0], own=d0)
            dma("sp", g2[:], gmlp_d, acc=[d0], own=d0)
            dma("sp", bada[:], bada_d, acc=[d0], own=d0)
            dca = Dep()
            op("act", lambda: A_.activation(out=cact[:], in_=cact[:], func=AF.Silu), rd=[d0], wr=[dca])
            war = ring(ph, "wa", 3, [128, 2048], F32)
            for g in range(96):
                wt, wd = war.nxt()
                dma("sp", wt[:], wada_d[g], wr=[wd], own=wd)
                for c in range(16):
                    op("pe", lambda: P_.matmul(PS[0][:, g:g + 1], wt[:, c * 128:(c + 1) * 128], cact[:, c:c + 1],
                                               start=(c == 0), stop=(c == 15)),
                       rd=[wd, dca], wr=PSD[0] if (g == 0 and c == 0) else (), acc=() if (g == 0 and c == 0) else PSD[0])
            dmod = Dep()
            op("dve", lambda: V_.tensor_tensor(out=modT[:], in0=PS[0][:, 0:96], in1=bada[:], op=ALU.add),
               rd=PSD[0] + [d0], wr=[dmod])
            op("dve", lambda: V_.tensor_scalar(out=tmp16[:], in0=modT[:, 16:32], scalar1=1.0, scalar2=None, op0=ALU.add),
               rd=[dmod], wr=[d0])
            op("dve", lambda: V_.tensor_tensor(out=aA[:], in0=tmp16[:], in1=g1[:], op=ALU.mult), rd=[d0], acc=[dC])
            op("dve", lambda: V_.tensor_scalar(out=tmp16[:], in0=modT[:, 64:80], scalar1=1.0, scalar2=None, op0=ALU.add),
               rd=[dmod], wr=[d0])
            op("dve", lambda: V_.tensor_tensor(out=aM[:], in0=tmp16[:], in1=g2[:], op=ALU.mult), rd=[d0], acc=[dC])
            s.barrier()
        bA = modT[:, 0:16]
        bM = modT[:, 48:64]

        def norm_transpose(stack_tiles, x_ap, x_dep, aT, bT, dst_fn, dst_dep, first):
            junk, ss, ms, sd, rstd, xn, dsm, dxn = stack_tiles
            op("act", lambda: A_.activation(out=junk[:], in_=x_ap, func=AF.Square, accum_out=ss[:]),
               rd=[x_dep], wr=[dsm])
            op("dve", lambda: V_.tensor_scalar(out=ms[:], in0=ss[:], scalar1=1.0 / D, scalar2=EPS, op0=ALU.mult, op1=ALU.add),
               rd=[dsm], wr=[dsm])
            op("act", lambda: A_.activation(out=sd[:], in_=ms[:], func=AF.Sqrt), rd=[dsm], wr=[dsm])
            op("dve", lambda: V_.reciprocal(out=rstd[:], in_=sd[:]), rd=[dsm], wr=[dsm])
            op("dve", lambda: V_.tensor_scalar(out=xn[:], in0=x_ap, scalar1=rstd[:, 0:1], scalar2=None, op0=ALU.mult),
               rd=[dsm, x_dep], wr=[dxn])
            for j in range(4):
                b = 5 + j % 2
                for q in range(4):
                    c = 4 * j + q
                    op("pe", lambda: P_.transpose(PS[b][:, q * 128:(q + 1) * 128], xn[:, c * 128:(c + 1) * 128], identf[:]),
                       rd=[dxn, dC], wr=[PSD[b][q]] if q == 0 else (), acc=() if q == 0 else [PSD[b][q]])
                for q in range(4):
                    c = 4 * j + q
                    op("act", lambda: A_.activation(out=dst_fn(c), in_=PS[b][:, q * 128:(q + 1) * 128], func=AF.Identity,
                                                    scale=aT[:, c:c + 1], bias=bT[:, c:c + 1]),
                       rd=[PSD[b][q], dmod, dC], wr=[dst_dep] if (first and c == 0) else (), acc=() if (first and c == 0) else [dst_dep])

        ntc = [0]

        def nt_tiles(stack):
            ntc[0] += 1
            u = "_%d" % ntc[0]
            return (sbt(stack, "junk" + u, [128, D], BF), sbt(stack, "ss" + u, [128, 1], F32), sbt(stack, "ms" + u, [128, 1], F32),
                    sbt(stack, "sd" + u, [128, 1], F32), sbt(stack, "rstd" + u, [128, 1], F32), sbt(stack, "xn" + u, [128, D], F32),
                    Dep(), Dep())

        bankctr = [0]

        def next_bank(lo=0, hi=5):
            bankctr[0] = (bankctr[0] + 1) % (hi - lo)
            return lo + bankctr[0]

        evctr = [0]

        def evac_copy(out_ap, in_ap, rd, wr=(), acc=(), scale=None):
            evctr[0] += 1
            if evctr[0] % 2 == 0:
                if scale is None:
                    op("act", lambda: A_.copy(out=out_ap, in_=in_ap), rd=rd, wr=wr, acc=acc)
                else:
                    op("act", lambda: A_.activation(out=out_ap, in_=in_ap, func=AF.Copy, scale=scale), rd=rd, wr=wr, acc=acc)
            else:
                if scale is None:
                    op("dve", lambda: V_.tensor_copy(out=out_ap, in_=in_ap), rd=rd, wr=wr, acc=acc)
                else:
                    op("dve", lambda: V_.tensor_scalar(out=out_ap, in0=in_ap, scalar1=scale, scalar2=None, op0=ALU.mult),
                       rd=rd, wr=wr, acc=acc)

        def proj_fm(wring, hT, hT_dep, ntok, wsrc, M, evac):
            wt, wd = wring.nxt()
            dma("pool", wt[:, :16 * M], wsrc, wr=[wd], own=wd)
            for t0 in range(0, ntok, 512):
                n = min(512, ntok - t0)
                b = next_bank()
                for c in range(16):
                    op("pe", lambda: P_.matmul(PS[b][:M, :n], wt[:, c * M:(c + 1) * M], hT[:, c, t0:t0 + n],
                                               start=(c == 0), stop=(c == 15)),
                       rd=[wd, hT_dep], wr=PSD[b] if c == 0 else (), acc=() if c == 0 else PSD[b])
                evac(b, t0, n)

        with ExitStack() as ph:
            SB = 8
            hT = sbt(ph, "hT", [128, 16, SB * 128], BF)
            dhT = Dep()
            wv = sbt(ph, "wv", [128, 16 * 2048], BF)
            dwv = Dep()
            for j in range(16):
                dma("pool", wv[:, j * 2048:(j + 1) * 2048], wv_d[:, j * 2048:(j + 1) * 2048],
                    wr=[dwv] if j == 0 else (), acc=() if j == 0 else [dwv], own=dwv)
            xr = ring(ph, "xb", 2, [128, D], F32)
            ntt = nt_tiles(ph)
            wgr = ring(ph, "wg", 3, [128, 2048], BF)
            stg = ring(ph, "stg", 2, [128, SB * 128], BF)
            stf = sbt(ph, "stf", [8, SB * 128], F32)
            dstf = Dep()
            vst = ring(ph, "vst", 2, [128, 2048], BF)
            for sb0 in range(0, NBLK_R, SB):
                nb = min(SB, NBLK_R - sb0)
                ntok = nb * 128
                tok0 = sb0 * 128
                for bi in range(nb):
                    xt, xd = xr.nxt()
                    dma("sp", xt[:], xl[tok0 + bi * 128: tok0 + (bi + 1) * 128, :], wr=[xd], own=xd)
                    norm_transpose(ntt, xt[:], xd, aA, bA, lambda c: hT[:, c, bi * 128:(bi + 1) * 128], dhT, bi == 0)
                for g in range(17):
                    st, sd_ = stg.nxt()

                    def ev(b, t0, n, st=st, sd_=sd_):
                        evac_copy(st[:, t0:t0 + n], PS[b][:, :n], rd=PSD[b], wr=[sd_] if t0 == 0 else (),
                                  acc=() if t0 == 0 else [sd_])
                    proj_fm(wgr, hT, dhT, ntok, wkf_d[g], 128, ev)
                    if g < 16:
                        dma("sp", KT[g, :, tok0:tok0 + ntok], st[:, :ntok], rd=[sd_], acc=[dKT], own=sd_)
                    else:
                        dma("sp", KI[:, tok0:tok0 + ntok], st[:, :ntok], rd=[sd_], acc=[dKI], own=sd_)

                def evf(b, t0, n):
                    evac_copy(stf[:, t0:t0 + n], PS[b][:8, :n], rd=PSD[b], wr=[dstf] if t0 == 0 else (),
                              acc=() if t0 == 0 else [dstf])
                proj_fm(wgr, hT, dhT, ntok, wfb_d, 8, evf)
                dma("sp", FBT[:, tok0:tok0 + ntok], stf[:, :ntok], rd=[dstf], acc=[dFBT], own=dstf)
                for bi in range(nb):
                    vt, vd = vst.nxt()
                    for cg in range(4):
                        b = next_bank()
                        for c in range(16):
                            op("pe", lambda: P_.matmul(PS[b][:, :], hT[:, c, bi * 128:(bi + 1) * 128],
                                                       wv[:, c * 2048 + cg * 512: c * 2048 + (cg + 1) * 512],
                                                       start=(c == 0), stop=(c == 15)),
                               rd=[dwv, dhT], wr=PSD[b] if c == 0 else (), acc=() if c == 0 else PSD[b])
                        evac_copy(vt[:, cg * 512:(cg + 1) * 512], PS[b][:, :], rd=PSD[b], wr=[vd] if cg == 0 else (),
                                  acc=() if cg == 0 else [vd])
                    dma("sp", V[tok0 + bi * 128: tok0 + (bi + 1) * 128, :], vt[:], rd=[vd], acc=[dV], own=vd)
            s.barrier()

        with ExitStack() as ph:
            fl = sbt(ph, "fl", [128, SEG], F32)
            e1 = sbt(ph, "e1", [128, SEG], F32)
            onesg = sbt(ph, "onesg", [128, SEG], F32)
            Fc = sbt(ph, "Fc", [128, SEG], F32)
            r1 = sbt(ph, "r1", [128, SEG], F32)
            r2 = sbt(ph, "r2", [128, SEG], F32)
            fb16 = [sbt(ph, "fb16_%d" % i, [128, SEG], BF) for i in range(6)]
            tm = sbt(ph, "tm", [128, 128], F32)
            nbf = sbt(ph, "nbf", [128, 1], F32)
            off = sbt(ph, "off", [128, 1], F32)
            tot = sbt(ph, "tot", [128, 1], F32)
            dF = Dep()
            dF2 = Dep()
            dma("sp", fl[:], FBT.rearrange("h (g n) -> (h g) n", n=SEG), rd=[dFBT], wr=[dF], own=dF)
            dma("sp", tm[:], tm_d, acc=[dF], own=dF)
            dma("sp", nbf[:], bfor_d, acc=[dF], own=dF)
            dTL = Dep()
            for h in range(8):
                dma("sp", FIXL[:, h, :], fixl_t, acc=[dFIXL, dTL], own=dTL)
                dma("sp", FIXR[:, h, :], fixr_t, acc=[dFIXR, dTL], own=dTL)
            op("dve", lambda: V_.tensor_scalar(out=nbf[:], in0=nbf[:], scalar1=-1.0, scalar2=None, op0=ALU.mult),
               rd=[dF], wr=[dF2])
            op("dve", lambda: V_.memset(onesg[:], 1.0), acc=[dF2])
            op("act", lambda: A_.activation(out=e1[:], in_=fl[:], func=AF.Exp, scale=-1.0, bias=nbf[:, 0:1]),
               rd=[dF, dF2], acc=[dF2])
            op("act", lambda: A_.activation(out=e1[:], in_=e1[:], func=AF.Ln, bias=1.0, scale=1.0), rd=[dF2], wr=[dF2])
            op("dve", lambda: V_.tensor_tensor_scan(Fc[:], onesg[:], e1[:], 0.0, ALU.mult, ALU.subtract),
               rd=[dF2], wr=[dF])
            op("dve", lambda: V_.tensor_copy(out=tot[:], in_=Fc[:, SEG - 1:SEG]), rd=[dF], acc=[dF])
            op("pe", lambda: P_.matmul(PS[0][:, 0:1], tm[:], tot[:], start=True, stop=True), rd=[dF], wr=PSD[0])
            op("dve", lambda: V_.tensor_copy(out=off[:], in_=PS[0][:, 0:1]), rd=PSD[0], acc=[dF])
            op("dve", lambda: V_.tensor_scalar(out=Fc[:], in0=Fc[:], scalar1=off[:, 0:1], scalar2=None, op0=ALU.add),
               rd=[dF], wr=[dF])
            op("dve", lambda: V_.tensor_copy(out=fb16[0][:], in_=Fc[:]), rd=[dF], wr=[dF2])
            op("dve", lambda: V_.tensor_tensor(out=r1[:], in0=Fc[:], in1=fb16[0][:], op=ALU.subtract), rd=[dF, dF2], acc=[dF2])
            op("dve", lambda: V_.tensor_copy(out=fb16[1][:], in_=r1[:]), rd=[dF2], acc=[dF2])
            op("dve", lambda: V_.tensor_tensor(out=r2[:], in0=r1[:], in1=fb16[1][:], op=ALU.subtract), rd=[dF2], acc=[dF2])
            op("dve", lambda: V_.tensor_copy(out=fb16[2][:], in_=r2[:]), rd=[dF2], acc=[dF2])
            for j in range(3):
                op("dve", lambda: V_.tensor_scalar(out=fb16[3 + j][:], in0=fb16[j][:], scalar1=-1.0, scalar2=None, op0=ALU.mult),
                   rd=[dF2], acc=[dF2])
            dst = Dep()
            for j in range(3):
                dma("sp", FIXR[j].rearrange("h (g n) -> (h g) n", n=SEG), fb16[j][:], rd=[dF2, dTL], acc=[dFIXR], own=dst)
                dma("sp", FIXL[3 + j].rearrange("h (g n) -> (h g) n", n=SEG), fb16[3 + j][:], rd=[dF2, dTL], acc=[dFIXL], own=dst)
            s.barrier()

        with ExitStack() as ph:
            hq = sbt(ph, "hq", [128, 16, 512], BF)
            dhq = Dep()
            xr = ring(ph, "xq", 2, [128, D], F32)
            ntt = nt_tiles(ph)
            wgr = ring(ph, "wgq", 3, [128, 2048], BF)
            stq = ring(ph, "stq", 2, [128, 512], BF)
            stgf = ring(ph, "stgf", 2, [128, 512], F32)
            wwi = sbt(ph, "wwi", [128, 128], BF)
            wis = sbt(ph, "wis", [128, 8], F32)
            dwwi = Dep()
            dwis = Dep()
            dma("pool", wwi[:], wwi_d, wr=[dwwi], own=dwwi)
            for k0 in range(0, NT_R, 4):
                nt_ = min(4, NT_R - k0)
                ntok = nt_ * 128
                for ti in range(nt_):
                    k = k0 + ti
                    xt, xd = xr.nxt()
                    dma("sp", xt[:], xl[Lk(k): Lk(k) + 128, :], wr=[xd], own=xd)
                    norm_transpose(ntt, xt[:], xd, aA, bA, lambda c: hq[:, c, ti * 128:(ti + 1) * 128], dhq, ti == 0)
                for g in range(28):
                    if 16 <= g < 24:
                        st, sd_ = stgf.nxt()

                        def ev(b, t0, n, st=st, sd_=sd_):
                            op("act", lambda: A_.activation(out=st[:, t0:t0 + n], in_=PS[b][:, :n], func=AF.Sigmoid),
                               rd=PSD[b], wr=[sd_])
                        proj_fm(wgr, hq, dhq, ntok, wqf_d[g], 128, ev)
                        dma("sp", GS[g - 16, :, k0 * 128:k0 * 128 + ntok], st[:, :ntok], rd=[sd_], acc=[dGS], own=sd_)
                    else:
                        st, sd_ = stq.nxt()
                        sc = (128.0 ** -0.5) if g < 16 else 1.0

                        def ev(b, t0, n, st=st, sd_=sd_, sc=sc):
                            evac_copy(st[:, t0:t0 + n], PS[b][:, :n], rd=PSD[b], wr=[sd_], scale=sc)
                        proj_fm(wgr, hq, dhq, ntok, wqf_d[g], 128, ev)
                        gi = g if g < 16 else g - 8
                        dma("sp", QS[gi, :, k0 * 128:k0 * 128 + ntok], st[:, :ntok], rd=[sd_], acc=[dQS], own=sd_)
                for ti in range(nt_):
                    k = k0 + ti
                    b = next_bank()
                    for c in range(16):
                        op("pe", lambda: P_.matmul(PS[b][:, 0:8], hq[:, c, ti * 128:(ti + 1) * 128], wwi[:, c * 8:(c + 1) * 8],
                                                   start=(c == 0), stop=(c == 15)),
                           rd=[dwwi, dhq], wr=PSD[b] if c == 0 else (), acc=() if c == 0 else PSD[b])
                    op("dve", lambda: V_.tensor_copy(out=wis[:], in_=PS[b][:, 0:8]), rd=PSD[b], wr=[dwis])
                    dma("sp", WIS[k * 128:(k + 1) * 128, :], wis[:], rd=[dwis], acc=[dWIS], own=dwis)
            s.barrier()

        with ExitStack() as ph:
            SMAX = NTOK
            scores = sbt(ph, "scores", [128, SMAX], F32)
            dsc = Dep()
            maskT = sbt(ph, "maskT", [128, SMAX], U8)
            dmk = Dep()
            qT = sbt(ph, "qT", [128, 20, 128], BF)
            dq = Dep()
            wi = sbt(ph, "wi", [128, 8], F32)
            dwi = Dep()
            dg = sbt(ph, "dg", [128, 8, 128], BF)
            ddg = Dep()
            kir = ring(ph, "kich", 3, [128, 512], BF)
            rbr = ring(ph, "rbuf", 3, [128, 512], BF)
            mcr = ring(ph, "mch", 2, [128, 512], BF)
            ktr = ring(ph, "kt", 3, [128, 8, 256], BF)
            vcr = ring(ph, "vch", 3, [128, 2, 1024], BF)
            ptr_ = ring(ph, "pT", 3, [128, 512], BF)
            pmr = ring(ph, "pm", 3, [128, 512], BF)
            bankH = sbt(ph, "bankH", [128, 8, BW], BF)
            Cb = sbt(ph, "Cb", [128, BW], BF)
            Ib = sbt(ph, "Ib", [128, 512], F32)
            padb = sbt(ph, "padb", [128, 1024], F32)
            rb31 = sbt(ph, "rb31", [128, 8], F32)
            flr = ring(ph, "fixl", 3, [8, 8, 256], BF)
            fixr = sbt(ph, "fixr", [8, 8, 128], BF)
            dfr = Dep()
            GT = sbt(ph, "GT", [128, 8, 128], F32)
            dGT = Dep()
            rr = sbt(ph, "rr", [128, 1024], F32)
            drr = Dep()
            mixT = sbt(ph, "mixT", [128, 16, 128], BF)
            dmx = Dep()
            sm = {n: sbt(ph, "sm_" + n, [128, 1], F32) for n in ("mx", "mn", "w0", "lo", "mid", "cnt", "pred")}
            dsm = Dep()
            dK = Dep()
            bankf = scores[:, 0:8 * BW].rearrange("p (h u) -> p h u", u=BW)
            dma("sp", scores[:, 0:8 * BW], bankB_d, wr=[dsc], own=dsc)
            dma("sp", rb31[:], rb31_d, wr=[dK], own=dK)
            dma("sp", Ib[:], Ib_d, acc=[dK], own=dK)
            dma("sp", padb[:], padb_d, acc=[dK], own=dK)
            dK2 = Dep()
            dma("pool", Cb[:], Cb_d, wr=[dK2], own=dK2)
            dKb = Dep()
            for h in range(8):
                op("dve", lambda: V_.tensor_scalar(out=bankH[:, h, :], in0=bankf[:, h, :], scalar1=rb31[:, h:h + 1], scalar2=None,
                                                   op0=ALU.subtract), rd=[dK, dsc], acc=[dKb])

            for k in range(NT_R):
                L = Lk(k)
                bq, o = L // 128, L % 128
                NB = bq + 2
                S = NB * 128
                dma("sp", qT[:], QS[:, :, k * 128:(k + 1) * 128].rearrange("g p t -> p g t"), rd=[dQS], wr=[dq], own=dq)
                dma("sp", wi[:], WIS[k * 128:(k + 1) * 128, :], rd=[dWIS], wr=[dwi], own=dwi)
                dma("sp", GT[:], GS[:, :, k * 128:(k + 1) * 128].rearrange("g p t -> p g t"), rd=[dGS], wr=[dGT], own=dGT)
                dma("sp", fixr[:], FIXR[:, :, L:L + 128], rd=[dFIXR], wr=[dfr], own=dfr)
                for h in range(8):
                    op("dve", lambda: V_.tensor_scalar(out=dg[:, h, :], in0=identf[:], scalar1=wi[:, h:h + 1], scalar2=None,
                                                       op0=ALU.mult), rd=[dwi, dC], wr=[ddg] if h == 0 else (),
                       acc=() if h == 0 else [ddg])
                for c0 in range(0, S, 512):
                    n = min(512, S - c0)
                    kt_, kd = kir.nxt()
                    dma("sp", kt_[:, :n], KI[:, c0:c0 + n], rd=[dKI], wr=[kd], own=kd)
                    ba = 3 + (c0 // 512) % 2
                    for h in range(8):
                        g, po = h // 2, 64 * (h % 2)
                        b = (c0 // 512 * 8 + h) % 3
                        op("pe", lambda: P_.matmul(PS[b][:, :n], qT[po:po + 64, 16 + g, :], kt_[po:po + 64, :n],
                                                   start=True, stop=True), rd=[dq, kd], wr=PSD[b])
                        rt, rd_ = rbr.nxt()
                        op("act", lambda: A_.activation(out=rt[:, :n], in_=PS[b][:, :n], func=AF.Relu), rd=PSD[b], wr=[rd_])
                        op("pe", lambda: P_.matmul(PS[ba][:, :n], dg[:, h, :], rt[:, :n], start=(h == 0), stop=(h == 7)),
                           rd=[ddg, rd_], wr=PSD[ba] if h == 0 else (), acc=() if h == 0 else PSD[ba])
                    evac_copy(scores[:, c0:c0 + n], PS[ba][:, :n], rd=PSD[ba], wr=[dsc] if c0 == 0 else (),
                              acc=() if c0 == 0 else [dsc])
                def emit_topk():
                    op("dve", lambda: V_.tensor_reduce(out=sm["mx"][:], in_=scores[:, :S], axis=AX.X, op=ALU.max), rd=[dsc], wr=[dsm])
                    op("dve", lambda: V_.tensor_reduce(out=sm["mn"][:], in_=scores[:, :S], axis=AX.X, op=ALU.min), rd=[dsc], acc=[dsm])
                    op("dve", lambda: V_.tensor_tensor(out=scores[:, 0:1024], in0=scores[:, 0:1024], in1=padb[:], op=ALU.add),
                       rd=[dsc, dK], wr=[dsc])
                    op("dve", lambda: V_.tensor_tensor(out=scores[:, bq * 128:bq * 128 + 256], in0=scores[:, bq * 128:bq * 128 + 256],
                                                       in1=Ib[:, 128 - o:128 - o + 256], op=ALU.add), rd=[dsc, dK], wr=[dsc])
                    op("dve", lambda: V_.tensor_tensor(out=sm["w0"][:], in0=sm["mx"][:], in1=sm["mn"][:], op=ALU.subtract),
                       rd=[dsm], wr=[dsm])
                    op("dve", lambda: V_.tensor_scalar(out=sm["w0"][:], in0=sm["w0"][:], scalar1=1.0001, scalar2=1e-6,
                                                       op0=ALU.mult, op1=ALU.add), rd=[dsm], wr=[dsm])
                    op("dve", lambda: V_.tensor_copy(out=sm["lo"][:], in_=sm["mn"][:]), rd=[dsm], wr=[dsm])
                    for it in range(NITER):
                        hf = 2.0 ** -(it + 1)
                        op("dve", lambda: V_.scalar_tensor_tensor(out=sm["mid"][:], in0=sm["w0"][:], scalar=hf, in1=sm["lo"][:],
                                                                  op0=ALU.mult, op1=ALU.add), rd=[dsm], wr=[dsm])
                        op("dve", lambda: V_.tensor_scalar(out=maskT[:, :S], in0=scores[:, :S], scalar1=sm["mid"][:, 0:1], scalar2=None,
                                                           op0=ALU.is_ge, op1=ALU.add, accum_out=sm["cnt"][:]),
                           rd=[dsc, dsm], wr=[dmk, dsm])
                        op("dve", lambda: V_.tensor_scalar(out=sm["pred"][:], in0=sm["cnt"][:], scalar1=255.5, scalar2=hf,
                                                           op0=ALU.is_ge, op1=ALU.mult), rd=[dsm], wr=[dsm])
                        op("dve", lambda: V_.scalar_tensor_tensor(out=sm["lo"][:], in0=sm["pred"][:], scalar=sm["w0"][:, 0:1],
                                                                  in1=sm["lo"][:], op0=ALU.mult, op1=ALU.add), rd=[dsm], wr=[dsm])
                    for c0 in range(0, S, 512):
                        n = min(512, S - c0)
                        mt, md = mcr.nxt()
                        op("dve", lambda: V_.tensor_scalar(out=mt[:, :n], in0=scores[:, c0:c0 + n], scalar1=sm["lo"][:, 0:1], scalar2=None,
                                                           op0=ALU.is_ge), rd=[dsc, dsm], wr=[md])
                        hlf = (c0 // 512) % 2
                        for jb in range(n // 128):
                            op("pe", lambda: P_.transpose(PST[:, hlf * 512 + jb * 128: hlf * 512 + (jb + 1) * 128],
                                                          mt[:, jb * 128:(jb + 1) * 128], identb[:]),
                               rd=[md, dC], wr=[PSTD[hlf]] if jb == 0 else (), acc=() if jb == 0 else [PSTD[hlf]])
                        op("act", lambda: A_.copy(out=maskT[:, c0:c0 + n], in_=PST[:, hlf * 512: hlf * 512 + n]),
                           rd=[PSTD[hlf]], wr=[dmk] if c0 == 0 else (), acc=() if c0 == 0 else [dmk])

                def emit_pass(fox):
                    units = [(bb_, grp) for bb_ in range(NB) for grp in range(2)]
                    bufs = {}
                    state = {}

                    def stage_a(ui):
                        b, grp = units[ui]
                        b2, bb = (b // 2) * 2, b % 2
                        if b2 not in bufs:
                            kt_, kd = ktr.nxt()
                            dma("sp", kt_[:], KT[8 * fox:8 * fox + 8, :, b2 * 128:b2 * 128 + 256].rearrange("h d s -> d h s"),
                                rd=[dKT], wr=[kd], own=kd)
                            vt_, vd = vcr.nxt()
                            dma("sp", vt_[:], V[b2 * 128:b2 * 128 + 256, 1024 * fox:1024 * fox + 1024].rearrange("(b s) c -> s b c", s=128),
                                rd=[dV], wr=[vd], own=vd)
                            fl_, fd = None, None
                            if fox:
                                fl_, fd = flr.nxt()
                                dma("sp", fl_[:], FIXL[:, :, b2 * 128:b2 * 128 + 256], rd=[dFIXL], wr=[fd], own=fd)
                            bufs[b2] = (kt_, kd, vt_, vd, fl_, fd)
                        kt_, kd, vt_, vd, fl_, fd = bufs[b2]
                        m = bq - b
                        ws = o + 128 * m + 128
                        pb = 4 + ui % 3
                        for hh in range(4):
                            h = 4 * grp + hh
                            reg = PS[pb][:, hh * 128:(hh + 1) * 128]
                            extra = (fox == 1) or (m <= 1)
                            op("pe", lambda: P_.matmul(reg, kt_[:, h, bb * 128:(bb + 1) * 128], qT[:, 8 * fox + h, :],
                                                       start=True, stop=not extra),
                               rd=[kd, dq], wr=[PSD[pb][hh]])
                            if fox:
                                last = not (m <= 0)
                                op("pe", lambda: P_.matmul(reg, fl_[0:8, h, bb * 128:(bb + 1) * 128], fixr[0:8, h, :],
                                                           start=False, stop=last),
                                   rd=[fd, dfr], acc=[PSD[pb][hh]])
                                if m <= 0:
                                    op("pe", lambda: P_.matmul(reg, identb[:], Cb[:, ws:ws + 128], start=False, stop=True),
                                       rd=[dC, dK2], acc=[PSD[pb][hh]])
                            elif m <= 1:
                                op("pe", lambda: P_.matmul(reg, identb[:], bankH[:, h, ws:ws + 128], start=False, stop=True),
                                   rd=[dC, dKb], acc=[PSD[pb][hh]])
                        pt, pd = ptr_.nxt()
                        op("act", lambda: A_.activation(out=pt[:], in_=PS[pb][:, :], func=AF.Exp), rd=PSD[pb], wr=[pd])
                        if fox:
                            pm_, pmd = pt, pd
                        else:
                            pm_, pmd = pmr.nxt()
                            op("dve", lambda: V_.tensor_tensor(
                                out=pm_[:].rearrange("p (h t) -> p h t", h=4),
                                in0=pt[:].rearrange("p (h t) -> p h t", h=4),
                                in1=maskT[:, b * 128:(b + 1) * 128].unsqueeze(1).broadcast_to([128, 4, 128]),
                                op=ALU.mult), rd=[pd, dmk], wr=[pmd])
                        state[ui] = (pm_, pmd, vt_, vd)

                    def stage_b(ui):
                        b, grp = units[ui]
                        bb = b % 2
                        pm_, pmd, vt_, vd = state.pop(ui)
                        for hh in range(4):
                            h = 4 * grp + hh
                            first = (b == 0 and hh == 0)
                            op("pe", lambda: P_.matmul(PS[grp][:, hh * 128:(hh + 1) * 128],
                                                       vt_[:, bb, h * 128:(h + 1) * 128], pm_[:, hh * 128:(hh + 1) * 128],
                                                       start=first, stop=(b == NB - 1), skip_group_check=True),
                               rd=[vd, pmd], wr=PSD[grp] if first else (), acc=() if first else PSD[grp])
                        op("pe", lambda: P_.matmul(PS[2 + grp][:, :], onesb[:], pm_[:], start=(b == 0), stop=(b == NB - 1)),
                           rd=[dC, pmd], wr=PSD[2 + grp] if b == 0 else (), acc=() if b == 0 else PSD[2 + grp])

                    LAG = 2
                    for ui in range(len(units) + LAG):
                        if ui < len(units):
                            stage_a(ui)
                        if ui - LAG >= 0:
                            stage_b(ui - LAG)

                def emit_norm(fox):
                    for grp in range(2):
                        op("dve", lambda: V_.tensor_scalar(out=rr[:, grp * 512:(grp + 1) * 512], in0=PS[2 + grp][:, :], scalar1=1e-30,
                                                           scalar2=None, op0=ALU.max), rd=PSD[2 + grp], wr=[drr] if grp == 0 else (),
                           acc=() if grp == 0 else [drr])
                    op("dve", lambda: V_.reciprocal(out=rr[:], in_=rr[:]), rd=[drr], wr=[drr])
                    for grp in range(2):
                        first = (fox == 1 and grp == 0)
                        if fox:
                            op("dve", lambda: V_.tensor_tensor(out=rr[:, grp * 512:(grp + 1) * 512], in0=rr[:, grp * 512:(grp + 1) * 512],
                                                               in1=GT[:, 4 * grp:4 * grp + 4, :].rearrange("p h t -> p (h t)"),
                                                               op=ALU.mult), rd=[drr, dGT], wr=[drr])
                        op("dve", lambda: V_.tensor_tensor(out=mixT[:, 8 * fox + 4 * grp: 8 * fox + 4 * grp + 4, :].rearrange("p h t -> p (h t)"),
                                                           in0=PS[grp][:, :], in1=rr[:, grp * 512:(grp + 1) * 512], op=ALU.mult),
                           rd=PSD[grp] + [drr], wr=[dmx] if first else (), acc=() if first else [dmx])

                emit_pass(1)
                emit_topk()
                emit_norm(1)
                emit_pass(0)
                emit_norm(0)
                dma("sp", MIX[k], mixT[:].rearrange("p h t -> p (h t)"), rd=[dmx], acc=[dMIX], own=dmx)
            s.barrier()

        with ExitStack() as ph:
            CB = 3
            xs = sbt(ph, "xs", [128, CB, D], F32)
            dxs = [Dep() for _ in range(CB)]
            h2 = sbt(ph, "h2", [128, 16, CB * 128], BF)
            dh2 = Dep()
            ntt = nt_tiles(ph)
            mixb = sbt(ph, "mixb", [128, CB, 16, 128], BF)
            dmb = [Dep() for _ in range(CB)]
            wo = sbt(ph, "wo", [128, 16 * 512], BF)
            dwo = Dep()
            gA = sbt(ph, "gA", [128, D], F32)
            gM = sbt(ph, "gM", [128, D], F32)
            gFt = sbt(ph, "gFt", [128, D], F32)
            dgt = Dep()
            wgr = ring(ph, "wgu", 3, [128, 2048], BF)
            aT = sbt(ph, "aT", [128, NFC, CB * 128], BF)
            daT = Dep()
            t1r = ring(ph, "t1", 2, [128, 512], F32)
            t2r = ring(ph, "t2", 2, [128, 512], F32)
            wdr = ring(ph, "wd", 3, [128, 2048], BF)
            cw = sbt(ph, "cw", [128, 3 * NG_UP], F32)
            cb = sbt(ph, "cb", [128, NG_UP], F32)
            dgr = sbt(ph, "dgr", [128, 128], F32)
            ddgr = Dep()
            dma("sp", cw[:], cw_d, wr=[dgt], own=dgt)
            dma("sp", cb[:], cb_d, acc=[dgt], own=dgt)
            dma("sp", gFt[:], gF_d, acc=[dgt], own=dgt)
            for (gt_, col0) in ((gA, 32), (gM, 80)):
                for c in range(16):
                    op("dve", lambda: V_.tensor_scalar(out=dgr[:], in0=identf[:], scalar1=modT[:, col0 + c:col0 + c + 1], scalar2=None,
                                                       op0=ALU.mult), rd=[dC, dmod], wr=[ddgr])
                    b = next_bank()
                    op("pe", lambda: P_.matmul(PS[b][:, 0:128], onesf[:], dgr[:], start=True, stop=True), rd=[dC, ddgr], wr=PSD[b])
                    op("act", lambda: A_.copy(out=gt_[:, c * 128:(c + 1) * 128], in_=PS[b][:, 0:128]), rd=PSD[b], acc=[dgt])

            for k0 in range(0, NT_R, CB):
                nt_ = min(CB, NT_R - k0)
                ntok = nt_ * 128
                for ti in range(nt_):
                    k = k0 + ti
                    dma("sp", xs[:, ti, :], xl[Lk(k):Lk(k) + 128, :], wr=[dxs[ti]], own=dxs[ti])
                    dma("sp", mixb[:, ti].rearrange("p h t -> p (h t)"), MIX[k], rd=[dMIX], wr=[dmb[ti]], own=dmb[ti])
                for cg in range(4):
                    dma("pool", wo[:], wout_d[cg], wr=[dwo], own=dwo, max_dma_last_dim=8192)
                    for ti in range(nt_):
                        b = next_bank()
                        for c in range(16):
                            op("pe", lambda: P_.matmul(PS[b][:, :], mixb[:, ti, c, :], wo[:, c * 512:(c + 1) * 512],
                                                       start=(c == 0), stop=(c == 15)),
                               rd=[dmb[ti], dwo], wr=PSD[b] if c == 0 else (), acc=() if c == 0 else PSD[b])
                        t1, t1d = t1r.nxt()
                        op("dve", lambda: V_.tensor_tensor(out=t1[:], in0=PS[b][:, :], in1=gA[:, cg * 512:(cg + 1) * 512], op=ALU.mult),
                           rd=PSD[b] + [dgt], wr=[t1d])
                        op("dve", lambda: V_.tensor_tensor(out=xs[:, ti, cg * 512:(cg + 1) * 512], in0=xs[:, ti, cg * 512:(cg + 1) * 512],
                                                           in1=t1[:], op=ALU.add), rd=[t1d], wr=[dxs[ti]])
                for ti in range(nt_):
                    norm_transpose(ntt, xs[:, ti, :], dxs[ti], aM, bM, lambda c: h2[:, c, ti * 128:(ti + 1) * 128], dh2, ti == 0)
                for g in range(NFC):
                    res = []
                    for half in range(2):
                        gg = g + NFC * half
                        wt, wd = wgr.nxt()
                        dma("pool", wt[:], wup_d[gg], wr=[wd], own=wd)
                        b = next_bank()
                        for c in range(16):
                            op("pe", lambda: P_.matmul(PS[b][:, :ntok], wt[:, c * 128:(c + 1) * 128], h2[:, c, :ntok],
                                                       start=(c == 0), stop=(c == 15)),
                               rd=[wd, dh2], wr=PSD[b] if c == 0 else (), acc=() if c == 0 else PSD[b])
                        pv = PS[b][:, :ntok].rearrange("p (t r) -> p t r", r=128)
                        op("dve", lambda: V_.tensor_tensor(out=pv[:, :, 0:2], in0=pv[:, :, 0:2],
                                                           in1=hv[:, k0:k0 + nt_].unsqueeze(2).broadcast_to([128, nt_, 2]),
                                                           op=ALU.mult), rd=PSD[b] + [dC], wr=PSD[b])
                        t, td = (t1r if half == 0 else t2r).nxt()
                        tv = t[:, :ntok].rearrange("p (t r) -> p t r", r=128)
                        op("act", lambda: A_.activation(out=t[:, :ntok], in_=PS[b][:, :ntok], func=AF.Identity,
                                                        scale=cw[:, 2 * NG_UP + gg:2 * NG_UP + gg + 1], bias=cb[:, gg:gg + 1]),
                           rd=PSD[b] + [dgt], wr=[td])
                        op("dve", lambda: V_.scalar_tensor_tensor(out=tv[:, :, 2:128], in0=pv[:, :, 1:127],
                                                                  scalar=cw[:, NG_UP + gg:NG_UP + gg + 1], in1=tv[:, :, 2:128],
                                                                  op0=ALU.mult, op1=ALU.add), rd=PSD[b] + [td, dgt], wr=[td])
                        op("dve", lambda: V_.scalar_tensor_tensor(out=tv[:, :, 2:128], in0=pv[:, :, 0:126],
                                                                  scalar=cw[:, gg:gg + 1], in1=tv[:, :, 2:128],
                                                                  op0=ALU.mult, op1=ALU.add), rd=PSD[b] + [td, dgt], wr=[td])
                        res.append((t, td))
                    (tg, tgd), (tv_, tvd) = res
                    op("act", lambda: A_.activation(out=tg[:, :ntok], in_=tg[:, :ntok], func=AF.Silu), rd=[tgd], wr=[tgd])
                    op("dve", lambda: V_.tensor_tensor(out=aT[:, g, :ntok], in0=tg[:, :ntok], in1=tv_[:, :ntok], op=ALU.mult),
                       rd=[tgd, tvd], wr=[daT] if g == 0 else (), acc=() if g == 0 else [daT])
                for half in range(2):
                    for f in range(NFC):
                        wt, wd = wdr.nxt()
                        dma("pool", wt[:, 0:1024], wdown_d[f, :, half * 1024:(half + 1) * 1024], wr=[wd], own=wd)
                        for ti in range(nt_):
                            for c2 in range(2):
                                bk = ti * 2 + c2
                                op("pe", lambda: P_.matmul(PS[bk][:, :], aT[:, f, ti * 128:(ti + 1) * 128], wt[:, c2 * 512:(c2 + 1) * 512],
                                                           start=(f == 0), stop=(f == NFC - 1)),
                                   rd=[wd, daT], wr=PSD[bk] if f == 0 else (), acc=() if f == 0 else PSD[bk])
                    for ti in range(nt_):
                        for c2 in range(2):
                            bk = ti * 2 + c2
                            cg = half * 2 + c2
                            t1, t1d = t1r.nxt()
                            op("dve", lambda: V_.tensor_tensor(out=t1[:], in0=PS[bk][:, :], in1=gM[:, cg * 512:(cg + 1) * 512], op=ALU.mult),
                               rd=PSD[bk] + [dgt], wr=[t1d])
                            op("dve", lambda: V_.tensor_tensor(out=xs[:, ti, cg * 512:(cg + 1) * 512], in0=xs[:, ti, cg * 512:(cg + 1) * 512],
                                                               in1=t1[:], op=ALU.add), rd=[t1d], wr=[dxs[ti]])
                for ti in range(nt_):
                    k = k0 + ti
                    junk, ss, ms, sd, rstd, xn, dsm2, dxn = ntt
                    xa = xs[:, ti, :]
                    op("act", lambda: A_.activation(out=junk[:], in_=xa, func=AF.Square, accum_out=ss[:]), rd=[dxs[ti]], wr=[dsm2])
                    op("dve", lambda: V_.tensor_scalar(out=ms[:], in0=ss[:], scalar1=1.0 / D, scalar2=EPS, op0=ALU.mult, op1=ALU.add),
                       rd=[dsm2], wr=[dsm2])
                    op("act", lambda: A_.activation(out=sd[:], in_=ms[:], func=AF.Sqrt), rd=[dsm2], wr=[dsm2])
                    op("dve", lambda: V_.reciprocal(out=rstd[:], in_=sd[:]), rd=[dsm2], wr=[dsm2])
                    op("dve", lambda: V_.scalar_tensor_tensor(out=xn[:], in0=xa, scalar=rstd[:, 0:1], in1=gFt[:], op0=ALU.mult, op1=ALU.mult),
                       rd=[dsm2, dxs[ti], dgt], wr=[dxn])
                    dma("sp", y[k * 128:(k + 1) * 128, :], xn[:], rd=[dxn], acc=[dY], own=dxn)
            s.barrier()
    return nc


def _t5_bucket_np(d):
    d = np.maximum(d, 0)
    df = np.maximum(d, 1).astype(np.float32)
    large = 16 + (np.log(df / np.float32(16)) / np.float32(math.log(8.0)) * np.float32(16)).astype(np.int32)
    large = np.minimum(large, 31)
    return np.where(d < 16, d, large)


def _lay(w, gsz):
    ncols = w.shape[1]
    G = ncols // gsz
    return np.ascontiguousarray(w.reshape(16, 128, G, gsz).transpose(2, 1, 0, 3)).reshape(G, 128, 16 * gsz)


_PROGRAM = None


def prep(x, c, rel_bias, w_ada, b_ada, g_attn, w_in, b_forget, w_out, g_mlp, w_up, conv_w, conv_b, w_down, g_final):
    f32 = np.float32
    x = np.asarray(x, f32)[0]
    w_in = np.asarray(w_in, f32)[0]
    qa, ka, va = w_in[:, 0:1024], w_in[:, 1024:2048], w_in[:, 2048:3072]
    qi, ki, wi = w_in[:, 3072:3584], w_in[:, 3584:3648], w_in[:, 3648:3656]
    qb, kb, vb = w_in[:, 3656:4680], w_in[:, 4680:5704], w_in[:, 5704:6728]
    gb, fb = w_in[:, 6728:7752], w_in[:, 7752:7760]

    common = {}
    common["cT"] = np.ascontiguousarray(np.asarray(c, f32)[0].reshape(16, 128).T)
    common["gattn"] = np.ascontiguousarray(np.asarray(g_attn, f32)[0].reshape(16, 128).T)
    common["gmlp"] = np.ascontiguousarray(np.asarray(g_mlp, f32)[0].reshape(16, 128).T)
    common["bada"] = np.ascontiguousarray(np.asarray(b_ada, f32)[0].reshape(96, 128).T)
    common["wada"] = _lay(np.asarray(w_ada, f32)[0], 128)
    common["wkf"] = _lay(np.concatenate([ka, kb, ki, ki], axis=1), 128)
    wfb = np.zeros((128, 128), f32)
    wfb[:, :] = fb.reshape(16, 128, 8).transpose(1, 0, 2).reshape(128, 128)
    common["wfb"] = wfb
    common["wv"] = np.ascontiguousarray(np.concatenate([va, vb], axis=1).reshape(16, 128, 2048).transpose(1, 0, 2)).reshape(128, 16 * 2048)
    common["wqf"] = _lay(np.concatenate([qa, qb, gb, qi], axis=1), 128)
    common["wwi"] = np.ascontiguousarray(wi.reshape(16, 128, 8).transpose(1, 0, 2)).reshape(128, 128)
    common["bfor"] = np.ascontiguousarray(np.repeat(np.asarray(b_forget, f32)[0], 16).reshape(128, 1))
    wo = np.asarray(w_out, f32)[0]
    common["wout"] = np.ascontiguousarray(wo.reshape(16, 128, 4, 512).transpose(2, 1, 0, 3)).reshape(4, 128, 16 * 512)
    common["wup"] = _lay(np.asarray(w_up, f32)[0], 128)
    cwv = np.asarray(conv_w, f32)[0]
    common["cw"] = np.ascontiguousarray(cwv.reshape(3, NG_UP, 128).transpose(2, 0, 1)).reshape(128, 3 * NG_UP)
    common["cb"] = np.ascontiguousarray(np.asarray(conv_b, f32)[0].reshape(NG_UP, 128).T)
    common["wdown"] = np.ascontiguousarray(np.asarray(w_down, f32)[0].reshape(NFC, 128, 2048))
    common["gF"] = np.ascontiguousarray(np.broadcast_to(np.asarray(g_final, f32)[None, :], (128, D)))
    common["identf"] = np.eye(128, dtype=f32)
    tm = np.zeros((128, 128), f32)
    for h in range(8):
        for s2 in range(16):
            tm[h * 16: h * 16 + s2, h * 16 + s2] = 1.0
    common["tm"] = tm
    tl = np.arange(128)[:, None]
    u = np.arange(512)[None, :]
    common["Ib"] = np.where(u - 128 <= tl, 0.0, NEG).astype(f32)
    ub = np.arange(BW)[None, :]
    dd = ub - tl - 128
    common["Cb"] = np.where(dd < 0, NEG, 0.0).astype(f32)
    rb = np.asarray(rel_bias, f32)
    bk = _t5_bucket_np(dd)
    bank = rb[bk]
    bank = np.where((dd >= 0)[:, :, None], bank, np.float32(0.0))
    common["bankB"] = np.ascontiguousarray(bank.transpose(0, 2, 1)).reshape(128, 8 * BW).astype(f32)
    common["rb31"] = np.ascontiguousarray(np.broadcast_to(rb[31][None, :], (128, 8))).astype(f32)

    in_maps = []
    for i in range(NCORE):
        pad = pad_of(i)
        n = min(SEQ, NTOK - pad)
        xloc = np.zeros((NTOK, D), f32)
        xloc[pad:pad + n] = x[:n]
        p = np.arange(NTOK)
        isp = (p < pad) | (p >= pad + SEQ)
        padrow = np.where(isp, NEG, 0.0).astype(f32)
        m = dict(common)
        m["xl"] = xloc
        m["padb"] = np.ascontiguousarray(np.broadcast_to(padrow[None, :1024], (128, 1024)))
        hvv = np.ones((NT,), f32)
        if i == 0:
            hvv[0] = 0.0
        m["hv"] = np.ascontiguousarray(np.broadcast_to(hvv[None, :], (128, NT)))
        fl_t = np.zeros((8, NTOK), f32)
        fl_t[0:3] = 1.0
        fl_t[6] = padrow
        fr_t = np.zeros((8, NTOK), f32)
        fr_t[3:7] = 1.0
        m["fixl_t"] = fl_t.astype(ml_dtypes.bfloat16)
        m["fixr_t"] = fr_t.astype(ml_dtypes.bfloat16)
        in_maps.append(m)
    return in_maps


def kernel(**inputs):
    global _PROGRAM
    f32 = np.float32
    in_maps = prep(**inputs)
    if _PROGRAM is None:
        _PROGRAM = build_program()
    res = run_bass_kernel_spmd(_PROGRAM, in_maps, core_ids=list(range(NCORE)), trace=True)
    out = np.zeros((1, SEQ, D), f32)
    for i in range(NCORE):
        yi = np.asarray(res.results[i]["y"]).reshape(NT, 128, D)
        for k in range(NT):
            j = 8 * k + i
            g0 = 126 * j
            if g0 >= SEQ:
                continue
            cnt = min(126, SEQ - g0)
            out[0, g0:g0 + cnt] = yi[k, 2:2 + cnt]
    return out
```

```python
import math
from contextlib import ExitStack

import numpy as np
import ml_dtypes

import concourse.bass as bass
import concourse.mybir as mybir
from concourse.bass_utils import run_bass_kernel_spmd

F32 = mybir.dt.float32
BF = mybir.dt.bfloat16
U8 = mybir.dt.uint8
AF = mybir.ActivationFunctionType
ALU = mybir.AluOpType
AX = mybir.AxisListType

D = 2048
SEQ = 16384
NCORE = 8
NT = 17
NBLK = 134
NTOK = NBLK * 128
SEG = NTOK // 16
NEG = -1.0e30
EPS = 1e-6
NITER = 24
BW = 512
DFF = 5504
NG_UP = 86
NFC = 43


def Lk(k):
    return 1008 * k + 882


def pad_of(i):
    return 884 - 126 * i


class Dep:
    __slots__ = ("w", "r", "p", "ds")

    def __init__(self):
        self.w = {}
        self.r = {}
        self.p = {}
        self.ds = None

    def reset(self, key, ev):
        p = dict(self.w)
        for k, (s_, v) in self.r.items():
            if k not in p or p[k][1] < v:
                p[k] = (s_, v)
        self.p = p
        self.w = {key: ev}
        self.r = {}


class DSem:
    def __init__(self, sem, key):
        self.sem = sem
        self.key = key
        self.n = 0


class Sch:
    def __init__(self, nc, es):
        self.nc = nc
        self.es = es
        self.E = {"pe": nc.tensor, "act": nc.scalar, "dve": nc.vector, "pool": nc.gpsimd, "sp": nc.sync}
        self.sem = {k: es.enter_context(nc.semaphore("s_" + k)) for k in self.E}
        self.cnt = {k: 0 for k in self.E}
        self.waited = {k: {} for k in self.E}
        self.dsems = []

    def _need(self, rd, wr, acc):
        need = {}

        def add(dct):
            for k, (s, v) in dct.items():
                if k not in need or need[k][1] < v:
                    need[k] = (s, v)

        for d in rd:
            add(d.w)
        for d in wr:
            add(d.w)
            add(d.r)
        for d in acc:
            add(d.r)
            add(d.p)
        return need

    def _wait(self, eng, need):
        for key, (sem, val) in need.items():
            if self.waited[eng].get(key, 0) < val:
                self.E[eng].wait_ge(sem, val)
                self.waited[eng][key] = val

    def op(self, eng, fn, rd=(), wr=(), acc=()):
        need = self._need(rd, wr, acc)
        if eng == "pe":
            need.pop("pe", None)
        self._wait(eng, need)
        inst = fn()
        self.cnt[eng] += 1
        v = self.cnt[eng]
        inst.then_inc(self.sem[eng], 1)
        ev = (self.sem[eng], v)
        for d in wr:
            d.reset(eng, ev)
        for d in acc:
            d.w[eng] = ev
        for d in rd:
            d.r[eng] = ev
        return inst

    def _dsem(self, d):
        if d.ds is None:
            key = "d%d" % len(self.dsems)
            sem = self.es.enter_context(self.nc.semaphore(key))
            d.ds = DSem(sem, key)
            self.dsems.append(d.ds)
        return d.ds

    def dma(self, q, out, in_, rd=(), wr=(), acc=(), own=None, **kw):
        need = self._need(rd, wr, acc)
        self._wait(q, need)
        inst = self.E[q].dma_start(out=out, in_=in_, **kw)
        ds = self._dsem(own)
        ds.n += 16
        inst.then_inc(ds.sem, 16)
        ev = (ds.sem, ds.n)
        for d in wr:
            d.reset(ds.key, ev)
        for d in acc:
            d.w[ds.key] = ev
        for d in rd:
            d.r[ds.key] = ev
        return inst

    def barrier(self):
        need = {k: (self.sem[k], self.cnt[k]) for k in self.E if self.cnt[k] > 0}
        for ds in self.dsems:
            if ds.n > 0:
                need[ds.key] = (ds.sem, ds.n)
        for e in self.E:
            nd = dict(need)
            if e in nd and e != "sp":
                pass
            self._wait(e, nd)


class Ring:
    def __init__(self, tiles):
        self.t = tiles
        self.d = [Dep() for _ in tiles]
        self.i = -1

    def nxt(self):
        self.i = (self.i + 1) % len(self.t)
        return self.t[self.i], self.d[self.i]


RUN_NT = NT
RUN_NBLK = NBLK


def build_program():
    NT_R, NBLK_R = RUN_NT, RUN_NBLK
    nc = bass.Bass("TRN2", target_bir_lowering=False)

    def din(name, shape, dt=F32):
        return nc.dram_tensor(name, list(shape), dt, kind="ExternalInput").ap()

    def dscr(name, shape, dt):
        return nc.dram_tensor(name, list(shape), dt, kind="Internal").ap()

    xl = din("xl", [NTOK, D])
    cT_d = din("cT", [128, 16])
    gattn_d = din("gattn", [128, 16])
    gmlp_d = din("gmlp", [128, 16])
    bada_d = din("bada", [128, 96])
    wada_d = din("wada", [96, 128, 2048])
    wkf_d = din("wkf", [17, 128, 2048])
    wfb_d = din("wfb", [128, 128])
    wv_d = din("wv", [128, 16 * 2048])
    wqf_d = din("wqf", [28, 128, 2048])
    wwi_d = din("wwi", [128, 128])
    bfor_d = din("bfor", [128, 1])
    wout_d = din("wout", [4, 128, 16 * 512])
    wup_d = din("wup", [NG_UP, 128, 2048])
    cw_d = din("cw", [128, 3 * NG_UP])
    cb_d = din("cb", [128, NG_UP])
    wdown_d = din("wdown", [NFC, 128, 2048])
    gF_d = din("gF", [128, D])
    identf_d = din("identf", [128, 128])
    tm_d = din("tm", [128, 128])
    padb_d = din("padb", [128, 1024])
    Ib_d = din("Ib", [128, 512])
    Cb_d = din("Cb", [128, BW])
    bankB_d = din("bankB", [128, 8 * BW])
    rb31_d = din("rb31", [128, 8])
    hv_d = din("hv", [128, NT])
    fixl_t = din("fixl_t", [8, NTOK], BF)
    fixr_t = din("fixr_t", [8, NTOK], BF)

    y = nc.dram_tensor("y", [NT * 128, D], F32, kind="ExternalOutput").ap()

    KT = dscr("KT", [16, 128, NTOK], BF)
    V = dscr("V", [NTOK, 2048], BF)
    KI = dscr("KI", [128, NTOK], BF)
    FBT = dscr("FBT", [8, NTOK], F32)
    FIXL = dscr("FIXL", [8, 8, NTOK], BF)
    FIXR = dscr("FIXR", [8, 8, NTOK], BF)
    QS = dscr("QS", [20, 128, NT * 128], BF)
    GS = dscr("GS", [8, 128, NT * 128], F32)
    WIS = dscr("WIS", [NT * 128, 8], F32)
    MIX = dscr("MIX", [NT, 128, 2048], BF)
    dKT, dV, dKI, dFBT, dFIXL, dFIXR, dQS, dGS, dWIS, dMIX, dY = [Dep() for _ in range(11)]

    with ExitStack() as es:
        es.enter_context(nc.allow_low_precision("bf16 matmul operands, fp32 accumulation"))
        s = Sch(nc, es)
        op, dma = s.op, s.dma
        V_, A_, P_ = nc.vector, nc.scalar, nc.tensor

        nmc = [0]

        def sbt(stack, name, shape, dt):
            nmc[0] += 1
            return stack.enter_context(nc.sbuf_tensor("sb%d_%s" % (nmc[0], name), list(shape), dt))

        def ring(stack, name, n, shape, dt):
            return Ring([sbt(stack, "%s%d" % (name, i), shape, dt) for i in range(n)])

        PS = [es.enter_context(nc.psum_tensor("ps%d" % i, [128, 512], F32)) for i in range(7)]
        PSD = [[Dep()] * 4 for _ in range(7)]
        PST = es.enter_context(nc.psum_tensor("pst", [128, 1024], BF))
        PSTD = [Dep()] * 2

        identf = sbt(es, "identf", [128, 128], F32)
        identb = sbt(es, "identb", [128, 128], BF)
        onesb = sbt(es, "onesb", [128, 128], BF)
        onesf = sbt(es, "onesf", [128, 128], F32)
        modT = sbt(es, "modT", [128, 96], F32)
        aA = sbt(es, "aA", [128, 16], F32)
        aM = sbt(es, "aM", [128, 16], F32)
        hv = sbt(es, "hv", [128, NT], F32)
        dC = Dep()
        dma("sp", identf[:], identf_d, wr=[dC], own=dC)
        dma("sp", hv[:], hv_d, acc=[dC], own=dC)
        op("dve", lambda: V_.tensor_copy(out=identb[:], in_=identf[:]), rd=[dC], acc=[dC])
        op("dve", lambda: V_.memset(onesb[:], 1.0), acc=[dC])
        op("dve", lambda: V_.memset(onesf[:], 1.0), acc=[dC])

        with ExitStack() as ph:
            cact = sbt(ph, "cact", [128, 16], F32)
            g1 = sbt(ph, "g1", [128, 16], F32)
            g2 = sbt(ph, "g2", [128, 16], F32)
            bada = sbt(ph, "bada", [128, 96], F32)
            tmp16 = sbt(ph, "tmp16", [128, 16], F32)
            d0 = Dep()
            dma("sp", cact[:], cT_d, wr=[d0], own=d0)
            dma("sp", g1[:], gattn_d, acc=[d0], own=d0)
            dma("sp", g2[:], gmlp_d, acc=[d0], own=d0)
            dma("sp", bada[:], bada_d, acc=[d0], own=d0)
            dca = Dep()
            op("act", lambda: A_.activation(out=cact[:], in_=cact[:], func=AF.Silu), rd=[d0], wr=[dca])
            war = ring(ph, "wa", 3, [128, 2048], F32)
            for g in range(96):
                wt, wd = war.nxt()
                dma("sp", wt[:], wada_d[g], wr=[wd], own=wd)
                for c in range(16):
                    op("pe", lambda: P_.matmul(PS[0][:, g:g + 1], wt[:, c * 128:(c + 1) * 128], cact[:, c:c + 1],
                                               start=(c == 0), stop=(c == 15)),
                       rd=[wd, dca], wr=PSD[0] if (g == 0 and c == 0) else (), acc=() if (g == 0 and c == 0) else PSD[0])
            dmod = Dep()
            op("dve", lambda: V_.tensor_tensor(out=modT[:], in0=PS[0][:, 0:96], in1=bada[:], op=ALU.add),
               rd=PSD[0] + [d0], wr=[dmod])
            op("dve", lambda: V_.tensor_scalar(out=tmp16[:], in0=modT[:, 16:32], scalar1=1.0, scalar2=None, op0=ALU.add),
               rd=[dmod], wr=[d0])
            op("dve", lambda: V_.tensor_tensor(out=aA[:], in0=tmp16[:], in1=g1[:], op=ALU.mult), rd=[d0], acc=[dC])
            op("dve", lambda: V_.tensor_scalar(out=tmp16[:], in0=modT[:, 64:80], scalar1=1.0, scalar2=None, op0=ALU.add),
               rd=[dmod], wr=[d0])
            op("dve", lambda: V_.tensor_tensor(out=aM[:], in0=tmp16[:], in1=g2[:], op=ALU.mult), rd=[d0], acc=[dC])
            s.barrier()
        bA = modT[:, 0:16]
        bM = modT[:, 48:64]

        def norm_transpose(stack_tiles, x_ap, x_dep, aT, bT, dst_fn, dst_dep, first):
            junk, ss, ms, sd, rstd, xn, dsm, dxn = stack_tiles
            op("act", lambda: A_.activation(out=junk[:], in_=x_ap, func=AF.Square, accum_out=ss[:]),
               rd=[x_dep], wr=[dsm])
            op("dve", lambda: V_.tensor_scalar(out=ms[:], in0=ss[:], scalar1=1.0 / D, scalar2=EPS, op0=ALU.mult, op1=ALU.add),
               rd=[dsm], wr=[dsm])
            op("act", lambda: A_.activation(out=sd[:], in_=ms[:], func=AF.Sqrt), rd=[dsm], wr=[dsm])
            op("dve", lambda: V_.reciprocal(out=rstd[:], in_=sd[:]), rd=[dsm], wr=[dsm])
            op("dve", lambda: V_.tensor_scalar(out=xn[:], in0=x_ap, scalar1=rstd[:, 0:1], scalar2=None, op0=ALU.mult),
               rd=[dsm, x_dep], wr=[dxn])
            for j in range(4):
                b = 5 + j % 2
                for q in range(4):
                    c = 4 * j + q
                    op("pe", lambda: P_.transpose(PS[b][:, q * 128:(q + 1) * 128], xn[:, c * 128:(c + 1) * 128], identf[:]),
                       rd=[dxn, dC], wr=[PSD[b][q]] if q == 0 else (), acc=() if q == 0 else [PSD[b][q]])
                for q in range(4):
                    c = 4 * j + q
                    op("act", lambda: A_.activation(out=dst_fn(c), in_=PS[b][:, q * 128:(q + 1) * 128], func=AF.Identity,
                                                    scale=aT[:, c:c + 1], bias=bT[:, c:c + 1]),
                       rd=[PSD[b][q], dmod, dC], wr=[dst_dep] if (first and c == 0) else (), acc=() if (first and c == 0) else [dst_dep])

        ntc = [0]

        def nt_tiles(stack):
            ntc[0] += 1
            u = "_%d" % ntc[0]
            return (sbt(stack, "junk" + u, [128, D], BF), sbt(stack, "ss" + u, [128, 1], F32), sbt(stack, "ms" + u, [128, 1], F32),
                    sbt(stack, "sd" + u, [128, 1], F32), sbt(stack, "rstd" + u, [128, 1], F32), sbt(stack, "xn" + u, [128, D], F32),
                    Dep(), Dep())

        bankctr = [0]

        def next_bank(lo=0, hi=5):
            bankctr[0] = (bankctr[0] + 1) % (hi - lo)
            return lo + bankctr[0]

        evctr = [0]

        def evac_copy(out_ap, in_ap, rd, wr=(), acc=(), scale=None):
            evctr[0] += 1
            if evctr[0] % 2 == 0:
                if scale is None:
                    op("act", lambda: A_.copy(out=out_ap, in_=in_ap), rd=rd, wr=wr, acc=acc)
                else:
                    op("act", lambda: A_.activation(out=out_ap, in_=in_ap, func=AF.Copy, scale=scale), rd=rd, wr=wr, acc=acc)
            else:
                if scale is None:
                    op("dve", lambda: V_.tensor_copy(out=out_ap, in_=in_ap), rd=rd, wr=wr, acc=acc)
                else:
                    op("dve", lambda: V_.tensor_scalar(out=out_ap, in0=in_ap, scalar1=scale, scalar2=None, op0=ALU.mult),
                       rd=rd, wr=wr, acc=acc)

        def proj_fm(wring, hT, hT_dep, ntok, wsrc, M, evac):
            wt, wd = wring.nxt()
            dma("pool", wt[:, :16 * M], wsrc, wr=[wd], own=wd)
            for t0 in range(0, ntok, 512):
                n = min(512, ntok - t0)
                b = next_bank()
                for c in range(16):
                    op("pe", lambda: P_.matmul(PS[b][:M, :n], wt[:, c * M:(c + 1) * M], hT[:, c, t0:t0 + n],
                                               start=(c == 0), stop=(c == 15)),
                       rd=[wd, hT_dep], wr=PSD[b] if c == 0 else (), acc=() if c == 0 else PSD[b])
                evac(b, t0, n)

        with ExitStack() as ph:
            SB = 8
            hT = sbt(ph, "hT", [128, 16, SB * 128], BF)
            dhT = Dep()
            wv = sbt(ph, "wv", [128, 16 * 2048], BF)
            dwv = Dep()
            for j in range(16):
                dma("pool", wv[:, j * 2048:(j + 1) * 2048], wv_d[:, j * 2048:(j + 1) * 2048],
                    wr=[dwv] if j == 0 else (), acc=() if j == 0 else [dwv], own=dwv)
            xr = ring(ph, "xb", 2, [128, D], F32)
            ntt = nt_tiles(ph)
            wgr = ring(ph, "wg", 3, [128, 2048], BF)
            stg = ring(ph, "stg", 2, [128, SB * 128], BF)
            stf = sbt(ph, "stf", [8, SB * 128], F32)
            dstf = Dep()
            vst = ring(ph, "vst", 2, [128, 2048], BF)
            for sb0 in range(0, NBLK_R, SB):
                nb = min(SB, NBLK_R - sb0)
                ntok = nb * 128
                tok0 = sb0 * 128
                for bi in range(nb):
                    xt, xd = xr.nxt()
                    dma("sp", xt[:], xl[tok0 + bi * 128: tok0 + (bi + 1) * 128, :], wr=[xd], own=xd)
                    norm_transpose(ntt, xt[:], xd, aA, bA, lambda c: hT[:, c, bi * 128:(bi + 1) * 128], dhT, bi == 0)
                for g in range(17):
                    st, sd_ = stg.nxt()

                    def ev(b, t0, n, st=st, sd_=sd_):
                        evac_copy(st[:, t0:t0 + n], PS[b][:, :n], rd=PSD[b], wr=[sd_] if t0 == 0 else (),
                                  acc=() if t0 == 0 else [sd_])
                    proj_fm(wgr, hT, dhT, ntok, wkf_d[g], 128, ev)
                    if g < 16:
                        dma("sp", KT[g, :, tok0:tok0 + ntok], st[:, :ntok], rd=[sd_], acc=[dKT], own=sd_)
                    else:
                        dma("sp", KI[:, tok0:tok0 + ntok], st[:, :ntok], rd=[sd_], acc=[dKI], own=sd_)

                def evf(b, t0, n):
                    evac_copy(stf[:, t0:t0 + n], PS[b][:8, :n], rd=PSD[b], wr=[dstf] if t0 == 0 else (),
                              acc=() if t0 == 0 else [dstf])
                proj_fm(wgr, hT, dhT, ntok, wfb_d, 8, evf)
                dma("sp", FBT[:, tok0:tok0 + ntok], stf[:, :ntok], rd=[dstf], acc=[dFBT], own=dstf)
                for bi in range(nb):
                    vt, vd = vst.nxt()
                    for cg in range(4):
                        b = next_bank()
                        for c in range(16):
                            op("pe", lambda: P_.matmul(PS[b][:, :], hT[:, c, bi * 128:(bi + 1) * 128],
                                                       wv[:, c * 2048 + cg * 512: c * 2048 + (cg + 1) * 512],
                                                       start=(c == 0), stop=(c == 15)),
                               rd=[dwv, dhT], wr=PSD[b] if c == 0 else (), acc=() if c == 0 else PSD[b])
                        evac_copy(vt[:, cg * 512:(cg + 1) * 512], PS[b][:, :], rd=PSD[b], wr=[vd] if cg == 0 else (),
                                  acc=() if cg == 0 else [vd])
                    dma("sp", V[tok0 + bi * 128: tok0 + (bi + 1) * 128, :], vt[:], rd=[vd], acc=[dV], own=vd)
            s.barrier()

        with ExitStack() as ph:
            fl = sbt(ph, "fl", [128, SEG], F32)
            e1 = sbt(ph, "e1", [128, SEG], F32)
            onesg = sbt(ph, "onesg", [128, SEG], F32)
            Fc = sbt(ph, "Fc", [128, SEG], F32)
            r1 = sbt(ph, "r1", [128, SEG], F32)
            r2 = sbt(ph, "r2", [128, SEG], F32)
            fb16 = [sbt(ph, "fb16_%d" % i, [128, SEG], BF) for i in range(6)]
            tm = sbt(ph, "tm", [128, 128], F32)
            nbf = sbt(ph, "nbf", [128, 1], F32)
            off = sbt(ph, "off", [128, 1], F32)
            tot = sbt(ph, "tot", [128, 1], F32)
            dF = Dep()
            dF2 = Dep()
            dma("sp", fl[:], FBT.rearrange("h (g n) -> (h g) n", n=SEG), rd=[dFBT], wr=[dF], own=dF)
            dma("sp", tm[:], tm_d, acc=[dF], own=dF)
            dma("sp", nbf[:], bfor_d, acc=[dF], own=dF)
            dTL = Dep()
            for h in range(8):
                dma("sp", FIXL[:, h, :], fixl_t, acc=[dFIXL, dTL], own=dTL)
                dma("sp", FIXR[:, h, :], fixr_t, acc=[dFIXR, dTL], own=dTL)
            op("dve", lambda: V_.tensor_scalar(out=nbf[:], in0=nbf[:], scalar1=-1.0, scalar2=None, op0=ALU.mult),
               rd=[dF], wr=[dF2])
            op("dve", lambda: V_.memset(onesg[:], 1.0), acc=[dF2])
            op("act", lambda: A_.activation(out=e1[:], in_=fl[:], func=AF.Exp, scale=-1.0, bias=nbf[:, 0:1]),
               rd=[dF, dF2], acc=[dF2])
            op("act", lambda: A_.activation(out=e1[:], in_=e1[:], func=AF.Ln, bias=1.0, scale=1.0), rd=[dF2], wr=[dF2])
            op("dve", lambda: V_.tensor_tensor_scan(Fc[:], onesg[:], e1[:], 0.0, ALU.mult, ALU.subtract),
               rd=[dF2], wr=[dF])
            op("dve", lambda: V_.tensor_copy(out=tot[:], in_=Fc[:, SEG - 1:SEG]), rd=[dF], acc=[dF])
            op("pe", lambda: P_.matmul(PS[0][:, 0:1], tm[:], tot[:], start=True, stop=True), rd=[dF], wr=PSD[0])
            op("dve", lambda: V_.tensor_copy(out=off[:], in_=PS[0][:, 0:1]), rd=PSD[0], acc=[dF])
            op("dve", lambda: V_.tensor_scalar(out=Fc[:], in0=Fc[:], scalar1=off[:, 0:1], scalar2=None, op0=ALU.add),
               rd=[dF], wr=[dF])
            op("dve", lambda: V_.tensor_copy(out=fb16[0][:], in_=Fc[:]), rd=[dF], wr=[dF2])
            op("dve", lambda: V_.tensor_tensor(out=r1[:], in0=Fc[:], in1=fb16[0][:], op=ALU.subtract), rd=[dF, dF2], acc=[dF2])
            op("dve", lambda: V_.tensor_copy(out=fb16[1][:], in_=r1[:]), rd=[dF2], acc=[dF2])
            op("dve", lambda: V_.tensor_tensor(out=r2[:], in0=r1[:], in1=fb16[1][:], op=ALU.subtract), rd=[dF2], acc=[dF2])
            op("dve", lambda: V_.tensor_copy(out=fb16[2][:], in_=r2[:]), rd=[dF2], acc=[dF2])
            for j in range(3):
                op("dve", lambda: V_.tensor_scalar(out=fb16[3 + j][:], in0=fb16[j][:], scalar1=-1.0, scalar2=None, op0=ALU.mult),
                   rd=[dF2], acc=[dF2])
            dst = Dep()
            for j in range(3):
                dma("sp", FIXR[j].rearrange("h (g n) -> (h g) n", n=SEG), fb16[j][:], rd=[dF2, dTL], acc=[dFIXR], own=dst)
                dma("sp", FIXL[3 + j].rearrange("h (g n) -> (h g) n", n=SEG), fb16[3 + j][:], rd=[dF2, dTL], acc=[dFIXL], own=dst)
            s.barrier()

        with ExitStack() as ph:
            hq = sbt(ph, "hq", [128, 16, 512], BF)
            dhq = Dep()
            xr = ring(ph, "xq", 2, [128, D], F32)
            ntt = nt_tiles(ph)
            wgr = ring(ph, "wgq", 3, [128, 2048], BF)
            stq = ring(ph, "stq", 2, [128, 512], BF)
            stgf = ring(ph, "stgf", 2, [128, 512], F32)
            wwi = sbt(ph, "wwi", [128, 128], BF)
            wis = sbt(ph, "wis", [128, 8], F32)
            dwwi = Dep()
            dwis = Dep()
            dma("pool", wwi[:], wwi_d, wr=[dwwi], own=dwwi)
            for k0 in range(0, NT_R, 4):
                nt_ = min(4, NT_R - k0)
                ntok = nt_ * 128
                for ti in range(nt_):
                    k = k0 + ti
                    xt, xd = xr.nxt()
                    dma("sp", xt[:], xl[Lk(k): Lk(k) + 128, :], wr=[xd], own=xd)
                    norm_transpose(ntt, xt[:], xd, aA, bA, lambda c: hq[:, c, ti * 128:(ti + 1) * 128], dhq, ti == 0)
                for g in range(28):
                    if 16 <= g < 24:
                        st, sd_ = stgf.nxt()

                        def ev(b, t0, n, st=st, sd_=sd_):
                            op("act", lambda: A_.activation(out=st[:, t0:t0 + n], in_=PS[b][:, :n], func=AF.Sigmoid),
                               rd=PSD[b], wr=[sd_])
                        proj_fm(wgr, hq, dhq, ntok, wqf_d[g], 128, ev)
                        dma("sp", GS[g - 16, :, k0 * 128:k0 * 128 + ntok], st[:, :ntok], rd=[sd_], acc=[dGS], own=sd_)
                    else:
                        st, sd_ = stq.nxt()
                        sc = (128.0 ** -0.5) if g < 16 else 1.0

                        def ev(b, t0, n, st=st, sd_=sd_, sc=sc):
                            evac_copy(st[:, t0:t0 + n], PS[b][:, :n], rd=PSD[b], wr=[sd_], scale=sc)
                        proj_fm(wgr, hq, dhq, ntok, wqf_d[g], 128, ev)
                        gi = g if g < 16 else g - 8
                        dma("sp", QS[gi, :, k0 * 128:k0 * 128 + ntok], st[:, :ntok], rd=[sd_], acc=[dQS], own=sd_)
                for ti in range(nt_):
                    k = k0 + ti
                    b = next_bank()
                    for c in range(16):
                        op("pe", lambda: P_.matmul(PS[b][:, 0:8], hq[:, c, ti * 128:(ti + 1) * 128], wwi[:, c * 8:(c + 1) * 8],
                                                   start=(c == 0), stop=(c == 15)),
                           rd=[dwwi, dhq], wr=PSD[b] if c == 0 else (), acc=() if c == 0 else PSD[b])
                    op("dve", lambda: V_.tensor_copy(out=wis[:], in_=PS[b][:, 0:8]), rd=PSD[b], wr=[dwis])
                    dma("sp", WIS[k * 128:(k + 1) * 128, :], wis[:], rd=[dwis], acc=[dWIS], own=dwis)
            s.barrier()

        with ExitStack() as ph:
            SMAX = NTOK
            scores = sbt(ph, "scores", [128, SMAX], F32)
            dsc = Dep()
            maskT = sbt(ph, "maskT", [128, SMAX], U8)
            dmk = Dep()
            qT = sbt(ph, "qT", [128, 20, 128], BF)
            dq = Dep()
            wi = sbt(ph, "wi", [128, 8], F32)
            dwi = Dep()
            dg = sbt(ph, "dg", [128, 8, 128], BF)
            ddg = Dep()
            kir = ring(ph, "kich", 3, [128, 512], BF)
            rbr = ring(ph, "rbuf", 3, [128, 512], BF)
            mcr = ring(ph, "mch", 4, [128, 512], BF)
            ktr = ring(ph, "kt", 3, [128, 8, 256], BF)
            vcr = ring(ph, "vch", 3, [128, 2, 1024], BF)
            ptr_ = ring(ph, "pT", 3, [128, 512], BF)
            pmr = ring(ph, "pm", 3, [128, 512], BF)
            bankH = sbt(ph, "bankH", [128, 8, BW], BF)
            Cb = sbt(ph, "Cb", [128, BW], BF)
            Ib = sbt(ph, "Ib", [128, 512], F32)
            padb = sbt(ph, "padb", [128, 1024], F32)
            rb31 = sbt(ph, "rb31", [128, 8], F32)
            flr = ring(ph, "fixl", 3, [8, 8, 256], BF)
            fixr = sbt(ph, "fixr", [8, 8, 128], BF)
            dfr = Dep()
            GT = sbt(ph, "GT", [128, 8, 128], F32)
            dGT = Dep()
            rr = sbt(ph, "rr", [128, 1024], F32)
            drr = Dep()
            mixT = sbt(ph, "mixT", [128, 16, 128], BF)
            dmx = Dep()
            sm = {n: sbt(ph, "sm_" + n, [128, 1], F32) for n in ("mx", "mn", "w0", "lo", "mid", "cnt", "pred")}
            dsm = Dep()
            dK = Dep()
            bankf = scores[:, 0:8 * BW].rearrange("p (h u) -> p h u", u=BW)
            dma("sp", scores[:, 0:8 * BW], bankB_d, wr=[dsc], own=dsc)
            dma("sp", rb31[:], rb31_d, wr=[dK], own=dK)
            dma("sp", Ib[:], Ib_d, acc=[dK], own=dK)
            dma("sp", padb[:], padb_d, acc=[dK], own=dK)
            dK2 = Dep()
            dma("pool", Cb[:], Cb_d, wr=[dK2], own=dK2)
            dKb = Dep()
            for h in range(8):
                op("dve", lambda: V_.tensor_scalar(out=bankH[:, h, :], in0=bankf[:, h, :], scalar1=rb31[:, h:h + 1], scalar2=None,
                                                   op0=ALU.subtract), rd=[dK, dsc], acc=[dKb])

            for k in range(NT_R):
                L = Lk(k)
                bq, o = L // 128, L % 128
                NB = bq + 2
                S = NB * 128
                dma("sp", qT[:], QS[:, :, k * 128:(k + 1) * 128].rearrange("g p t -> p g t"), rd=[dQS], wr=[dq], own=dq)
                dma("sp", wi[:], WIS[k * 128:(k + 1) * 128, :], rd=[dWIS], wr=[dwi], own=dwi)
                dma("sp", GT[:], GS[:, :, k * 128:(k + 1) * 128].rearrange("g p t -> p g t"), rd=[dGS], wr=[dGT], own=dGT)
                dma("sp", fixr[:], FIXR[:, :, L:L + 128], rd=[dFIXR], wr=[dfr], own=dfr)
                for h in range(8):
                    op("dve", lambda: V_.tensor_scalar(out=dg[:, h, :], in0=identf[:], scalar1=wi[:, h:h + 1], scalar2=None,
                                                       op0=ALU.mult), rd=[dwi, dC], wr=[ddg] if h == 0 else (),
                       acc=() if h == 0 else [ddg])
                for c0 in range(0, S, 512):
                    n = min(512, S - c0)
                    kt_, kd = kir.nxt()
                    dma("sp", kt_[:, :n], KI[:, c0:c0 + n], rd=[dKI], wr=[kd], own=kd)
                    ba = 3 + (c0 // 512) % 2
                    for h in range(8):
                        g, po = h // 2, 64 * (h % 2)
                        b = (c0 // 512 * 8 + h) % 3
                        op("pe", lambda: P_.matmul(PS[b][:, :n], qT[po:po + 64, 16 + g, :], kt_[po:po + 64, :n],
                                                   start=True, stop=True), rd=[dq, kd], wr=PSD[b])
                        rt, rd_ = rbr.nxt()
                        op("act", lambda: A_.activation(out=rt[:, :n], in_=PS[b][:, :n], func=AF.Relu), rd=PSD[b], wr=[rd_])
                        op("pe", lambda: P_.matmul(PS[ba][:, :n], dg[:, h, :], rt[:, :n], start=(h == 0), stop=(h == 7)),
                           rd=[ddg, rd_], wr=PSD[ba] if h == 0 else (), acc=() if h == 0 else PSD[ba])
                    evac_copy(scores[:, c0:c0 + n], PS[ba][:, :n], rd=PSD[ba], wr=[dsc] if c0 == 0 else (),
                              acc=() if c0 == 0 else [dsc])
                def emit_topk():
                    op("dve", lambda: V_.tensor_reduce(out=sm["mx"][:], in_=scores[:, :S], axis=AX.X, op=ALU.max), rd=[dsc], wr=[dsm])
                    op("dve", lambda: V_.tensor_reduce(out=sm["mn"][:], in_=scores[:, :S], axis=AX.X, op=ALU.min), rd=[dsc], acc=[dsm])
                    op("dve", lambda: V_.tensor_tensor(out=scores[:, 0:1024], in0=scores[:, 0:1024], in1=padb[:], op=ALU.add),
                       rd=[dsc, dK], wr=[dsc])
                    op("dve", lambda: V_.tensor_tensor(out=scores[:, bq * 128:bq * 128 + 256], in0=scores[:, bq * 128:bq * 128 + 256],
                                                       in1=Ib[:, 128 - o:128 - o + 256], op=ALU.add), rd=[dsc, dK], wr=[dsc])
                    op("dve", lambda: V_.tensor_tensor(out=sm["w0"][:], in0=sm["mx"][:], in1=sm["mn"][:], op=ALU.subtract),
                       rd=[dsm], wr=[dsm])
                    op("dve", lambda: V_.tensor_scalar(out=sm["w0"][:], in0=sm["w0"][:], scalar1=1.0001, scalar2=1e-6,
                                                       op0=ALU.mult, op1=ALU.add), rd=[dsm], wr=[dsm])
                    op("dve", lambda: V_.tensor_copy(out=sm["lo"][:], in_=sm["mn"][:]), rd=[dsm], wr=[dsm])
                    for it in range(NITER):
                        hf = 2.0 ** -(it + 1)
                        op("dve", lambda: V_.scalar_tensor_tensor(out=sm["mid"][:], in0=sm["w0"][:], scalar=hf, in1=sm["lo"][:],
                                                                  op0=ALU.mult, op1=ALU.add), rd=[dsm], wr=[dsm])
                        op("dve", lambda: V_.tensor_scalar(out=maskT[:, :S], in0=scores[:, :S], scalar1=sm["mid"][:, 0:1], scalar2=None,
                                                           op0=ALU.is_ge, op1=ALU.add, accum_out=sm["cnt"][:]),
                           rd=[dsc, dsm], wr=[dmk, dsm])
                        op("dve", lambda: V_.tensor_scalar(out=sm["pred"][:], in0=sm["cnt"][:], scalar1=255.5, scalar2=hf,
                                                           op0=ALU.is_ge, op1=ALU.mult), rd=[dsm], wr=[dsm])
                        op("dve", lambda: V_.scalar_tensor_tensor(out=sm["lo"][:], in0=sm["pred"][:], scalar=sm["w0"][:, 0:1],
                                                                  in1=sm["lo"][:], op0=ALU.mult, op1=ALU.add), rd=[dsm], wr=[dsm])
                    tgt = [(PST[:, 0:512], PSTD[0]), (PS[4][:, :].bitcast(BF)[:, 0:512], PSD[4][0]),
                           (PS[5][:, :].bitcast(BF)[:, 0:512], PSD[5][0]), (PS[6][:, :].bitcast(BF)[:, 0:512], PSD[6][0])]
                    for c0 in range(0, S, 512):
                        n = min(512, S - c0)
                        mt, md = mcr.nxt()
                        op("dve", lambda: V_.tensor_scalar(out=mt[:, :n], in0=scores[:, c0:c0 + n], scalar1=sm["lo"][:, 0:1], scalar2=None,
                                                           op0=ALU.is_ge), rd=[dsc, dsm], wr=[md])
                        tap, tdep = tgt[(c0 // 512) % 4]
                        for jb in range(n // 128):
                            op("pe", lambda: P_.transpose(tap[:, jb * 128:(jb + 1) * 128],
                                                          mt[:, jb * 128:(jb + 1) * 128], identb[:]),
                               rd=[md, dC], wr=[tdep] if jb == 0 else (), acc=() if jb == 0 else [tdep])
                        op("act", lambda: A_.copy(out=maskT[:, c0:c0 + n], in_=tap[:, 0:n]),
                           rd=[tdep], wr=[dmk] if c0 == 0 else (), acc=() if c0 == 0 else [dmk])

                def emit_pass(fox):
                    units = [(bb_, grp) for bb_ in range(NB) for grp in range(2)]
                    bufs = {}
                    state = {}

                    def stage_a(ui):
                        b, grp = units[ui]
                        b2, bb = (b // 2) * 2, b % 2
                        if b2 not in bufs:
                            kt_, kd = ktr.nxt()
                            dma("sp", kt_[:], KT[8 * fox:8 * fox + 8, :, b2 * 128:b2 * 128 + 256].rearrange("h d s -> d h s"),
                                rd=[dKT], wr=[kd], own=kd)
                            vt_, vd = vcr.nxt()
                            dma("sp", vt_[:], V[b2 * 128:b2 * 128 + 256, 1024 * fox:1024 * fox + 1024].rearrange("(b s) c -> s b c", s=128),
                                rd=[dV], wr=[vd], own=vd)
                            fl_, fd = None, None
                            if fox:
                                fl_, fd = flr.nxt()
                                dma("sp", fl_[:], FIXL[:, :, b2 * 128:b2 * 128 + 256], rd=[dFIXL], wr=[fd], own=fd)
                            bufs[b2] = (kt_, kd, vt_, vd, fl_, fd)
                        kt_, kd, vt_, vd, fl_, fd = bufs[b2]
                        m = bq - b
                        ws = o + 128 * m + 128
                        pb = 4 + ui % 3
                        for hh in range(4):
                            h = 4 * grp + hh
                            reg = PS[pb][:, hh * 128:(hh + 1) * 128]
                            extra = (fox == 1) or (m <= 1)
                            op("pe", lambda: P_.matmul(reg, kt_[:, h, bb * 128:(bb + 1) * 128], qT[:, 8 * fox + h, :],
                                                       start=True, stop=not extra),
                               rd=[kd, dq], wr=[PSD[pb][hh]])
                            if fox:
                                last = not (m <= 0)
                                op("pe", lambda: P_.matmul(reg, fl_[0:8, h, bb * 128:(bb + 1) * 128], fixr[0:8, h, :],
                                                           start=False, stop=last),
                                   rd=[fd, dfr], acc=[PSD[pb][hh]])
                                if m <= 0:
                                    op("pe", lambda: P_.matmul(reg, identb[:], Cb[:, ws:ws + 128], start=False, stop=True),
                                       rd=[dC, dK2], acc=[PSD[pb][hh]])
                            elif m <= 1:
                                op("pe", lambda: P_.matmul(reg, identb[:], bankH[:, h, ws:ws + 128], start=False, stop=True),
                                   rd=[dC, dKb], acc=[PSD[pb][hh]])
                        pt, pd = ptr_.nxt()
                        op("act", lambda: A_.activation(out=pt[:], in_=PS[pb][:, :], func=AF.Exp), rd=PSD[pb], wr=[pd])
                        if fox:
                            pm_, pmd = pt, pd
                        else:
                            pm_, pmd = pmr.nxt()
                            op("dve", lambda: V_.tensor_tensor(
                                out=pm_[:].rearrange("p (h t) -> p h t", h=4),
                                in0=pt[:].rearrange("p (h t) -> p h t", h=4),
                                in1=maskT[:, b * 128:(b + 1) * 128].unsqueeze(1).broadcast_to([128, 4, 128]),
                                op=ALU.mult), rd=[pd, dmk], wr=[pmd])
                        state[ui] = (pm_, pmd, vt_, vd)

                    def stage_b(ui):
                        b, grp = units[ui]
                        bb = b % 2
                        pm_, pmd, vt_, vd = state.pop(ui)
                        for hh in range(4):
                            h = 4 * grp + hh
                            first = (b == 0 and hh == 0)
                            op("pe", lambda: P_.matmul(PS[grp][:, hh * 128:(hh + 1) * 128],
                                                       vt_[:, bb, h * 128:(h + 1) * 128], pm_[:, hh * 128:(hh + 1) * 128],
                                                       start=first, stop=(b == NB - 1), skip_group_check=True),
                               rd=[vd, pmd], wr=PSD[grp] if first else (), acc=() if first else PSD[grp])
                        op("pe", lambda: P_.matmul(PS[2 + grp][:, :], onesb[:], pm_[:], start=(b == 0), stop=(b == NB - 1)),
                           rd=[dC, pmd], wr=PSD[2 + grp] if b == 0 else (), acc=() if b == 0 else PSD[2 + grp])

                    LAG = 2
                    for ui in range(len(units) + LAG):
                        if ui < len(units):
                            stage_a(ui)
                        if ui - LAG >= 0:
                            stage_b(ui - LAG)

                def emit_norm(fox):
                    for grp in range(2):
                        op("dve", lambda: V_.tensor_scalar(out=rr[:, grp * 512:(grp + 1) * 512], in0=PS[2 + grp][:, :], scalar1=1e-30,
                                                           scalar2=None, op0=ALU.max), rd=PSD[2 + grp], wr=[drr] if grp == 0 else (),
                           acc=() if grp == 0 else [drr])
                    op("dve", lambda: V_.reciprocal(out=rr[:], in_=rr[:]), rd=[drr], wr=[drr])
                    for grp in range(2):
                        first = (fox == 1 and grp == 0)
                        if fox:
                            op("dve", lambda: V_.tensor_tensor(out=rr[:, grp * 512:(grp + 1) * 512], in0=rr[:, grp * 512:(grp + 1) * 512],
                                                               in1=GT[:, 4 * grp:4 * grp + 4, :].rearrange("p h t -> p (h t)"),
                                                               op=ALU.mult), rd=[drr, dGT], wr=[drr])
                        op("dve", lambda: V_.tensor_tensor(out=mixT[:, 8 * fox + 4 * grp: 8 * fox + 4 * grp + 4, :].rearrange("p h t -> p (h t)"),
                                                           in0=PS[grp][:, :], in1=rr[:, grp * 512:(grp + 1) * 512], op=ALU.mult),
                           rd=PSD[grp] + [drr], wr=[dmx] if first else (), acc=() if first else [dmx])

                emit_pass(1)
                emit_topk()
                emit_norm(1)
                emit_pass(0)
                emit_norm(0)
                dma("sp", MIX[k], mixT[:].rearrange("p h t -> p (h t)"), rd=[dmx], acc=[dMIX], own=dmx)
            s.barrier()

        with ExitStack() as ph:
            CB = 3
            xs = sbt(ph, "xs", [128, CB, D], F32)
            dxs = [Dep() for _ in range(CB)]
            h2 = sbt(ph, "h2", [128, 16, CB * 128], BF)
            dh2 = Dep()
            ntt = nt_tiles(ph)
            mixb = sbt(ph, "mixb", [128, CB, 16, 128], BF)
            dmb = [Dep() for _ in range(CB)]
            wo = sbt(ph, "wo", [128, 16 * 512], BF)
            dwo = Dep()
            gA = sbt(ph, "gA", [128, D], F32)
            gM = sbt(ph, "gM", [128, D], F32)
            gFt = sbt(ph, "gFt", [128, D], F32)
            dgt = Dep()
            wgr = ring(ph, "wgu", 3, [128, 2048], BF)
            aT = sbt(ph, "aT", [128, NFC, CB * 128], BF)
            daT = Dep()
            t1r = ring(ph, "t1", 2, [128, 512], F32)
            t2r = ring(ph, "t2", 2, [128, 512], F32)
            wdr = ring(ph, "wd", 3, [128, 2048], BF)
            cw = sbt(ph, "cw", [128, 3 * NG_UP], F32)
            cb = sbt(ph, "cb", [128, NG_UP], F32)
            dgr = sbt(ph, "dgr", [128, 128], F32)
            ddgr = Dep()
            dma("sp", cw[:], cw_d, wr=[dgt], own=dgt)
            dma("sp", cb[:], cb_d, acc=[dgt], own=dgt)
            dma("sp", gFt[:], gF_d, acc=[dgt], own=dgt)
            for (gt_, col0) in ((gA, 32), (gM, 80)):
                for c in range(16):
                    op("dve", lambda: V_.tensor_scalar(out=dgr[:], in0=identf[:], scalar1=modT[:, col0 + c:col0 + c + 1], scalar2=None,
                                                       op0=ALU.mult), rd=[dC, dmod], wr=[ddgr])
                    b = next_bank()
                    op("pe", lambda: P_.matmul(PS[b][:, 0:128], onesf[:], dgr[:], start=True, stop=True), rd=[dC, ddgr], wr=PSD[b])
                    op("act", lambda: A_.copy(out=gt_[:, c * 128:(c + 1) * 128], in_=PS[b][:, 0:128]), rd=PSD[b], acc=[dgt])

            for k0 in range(0, NT_R, CB):
                nt_ = min(CB, NT_R - k0)
                ntok = nt_ * 128
                for ti in range(nt_):
                    k = k0 + ti
                    dma("sp", xs[:, ti, :], xl[Lk(k):Lk(k) + 128, :], wr=[dxs[ti]], own=dxs[ti])
                    dma("sp", mixb[:, ti].rearrange("p h t -> p (h t)"), MIX[k], rd=[dMIX], wr=[dmb[ti]], own=dmb[ti])
                for cg in range(4):
                    dma("pool", wo[:], wout_d[cg], wr=[dwo], own=dwo, max_dma_last_dim=8192)
                    for ti in range(nt_):
                        b = next_bank()
                        for c in range(16):
                            op("pe", lambda: P_.matmul(PS[b][:, :], mixb[:, ti, c, :], wo[:, c * 512:(c + 1) * 512],
                                                       start=(c == 0), stop=(c == 15)),
                               rd=[dmb[ti], dwo], wr=PSD[b] if c == 0 else (), acc=() if c == 0 else PSD[b])
                        t1, t1d = t1r.nxt()
                        op("dve", lambda: V_.tensor_tensor(out=t1[:], in0=PS[b][:, :], in1=gA[:, cg * 512:(cg + 1) * 512], op=ALU.mult),
                           rd=PSD[b] + [dgt], wr=[t1d])
                        op("dve", lambda: V_.tensor_tensor(out=xs[:, ti, cg * 512:(cg + 1) * 512], in0=xs[:, ti, cg * 512:(cg + 1) * 512],
                                                           in1=t1[:], op=ALU.add), rd=[t1d], wr=[dxs[ti]])
                for ti in range(nt_):
                    norm_transpose(ntt, xs[:, ti, :], dxs[ti], aM, bM, lambda c: h2[:, c, ti * 128:(ti + 1) * 128], dh2, ti == 0)
                for g in range(NFC):
                    res = []
                    for half in range(2):
                        gg = g + NFC * half
                        wt, wd = wgr.nxt()
                        dma("pool", wt[:], wup_d[gg], wr=[wd], own=wd)
                        b = next_bank()
                        for c in range(16):
                            op("pe", lambda: P_.matmul(PS[b][:, :ntok], wt[:, c * 128:(c + 1) * 128], h2[:, c, :ntok],
                                                       start=(c == 0), stop=(c == 15)),
                               rd=[wd, dh2], wr=PSD[b] if c == 0 else (), acc=() if c == 0 else PSD[b])
                        pv = PS[b][:, :ntok].rearrange("p (t r) -> p t r", r=128)
                        op("dve", lambda: V_.tensor_tensor(out=pv[:, :, 0:2], in0=pv[:, :, 0:2],
                                                           in1=hv[:, k0:k0 + nt_].unsqueeze(2).broadcast_to([128, nt_, 2]),
                                                           op=ALU.mult), rd=PSD[b] + [dC], wr=PSD[b])
                        t, td = (t1r if half == 0 else t2r).nxt()
                        tv = t[:, :ntok].rearrange("p (t r) -> p t r", r=128)
                        op("act", lambda: A_.activation(out=t[:, :ntok], in_=PS[b][:, :ntok], func=AF.Identity,
                                                        scale=cw[:, 2 * NG_UP + gg:2 * NG_UP + gg + 1], bias=cb[:, gg:gg + 1]),
                           rd=PSD[b] + [dgt], wr=[td])
                        op("dve", lambda: V_.scalar_tensor_tensor(out=tv[:, :, 2:128], in0=pv[:, :, 1:127],
                                                                  scalar=cw[:, NG_UP + gg:NG_UP + gg + 1], in1=tv[:, :, 2:128],
                                                                  op0=ALU.mult, op1=ALU.add), rd=PSD[b] + [td, dgt], wr=[td])
                        op("dve", lambda: V_.scalar_tensor_tensor(out=tv[:, :, 2:128], in0=pv[:, :, 0:126],
                                                                  scalar=cw[:, gg:gg + 1], in1=tv[:, :, 2:128],
                                                                  op0=ALU.mult, op1=ALU.add), rd=PSD[b] + [td, dgt], wr=[td])
                        res.append((t, td))
                    (tg, tgd), (tv_, tvd) = res
                    op("act", lambda: A_.activation(out=tg[:, :ntok], in_=tg[:, :ntok], func=AF.Silu), rd=[tgd], wr=[tgd])
                    op("dve", lambda: V_.tensor_tensor(out=aT[:, g, :ntok], in0=tg[:, :ntok], in1=tv_[:, :ntok], op=ALU.mult),
                       rd=[tgd, tvd], wr=[daT] if g == 0 else (), acc=() if g == 0 else [daT])
                for half in range(2):
                    for f in range(NFC):
                        wt, wd = wdr.nxt()
                        dma("pool", wt[:, 0:1024], wdown_d[f, :, half * 1024:(half + 1) * 1024], wr=[wd], own=wd)
                        for ti in range(nt_):
                            for c2 in range(2):
                                bk = ti * 2 + c2
                                op("pe", lambda: P_.matmul(PS[bk][:, :], aT[:, f, ti * 128:(ti + 1) * 128], wt[:, c2 * 512:(c2 + 1) * 512],
                                                           start=(f == 0), stop=(f == NFC - 1)),
                                   rd=[wd, daT], wr=PSD[bk] if f == 0 else (), acc=() if f == 0 else PSD[bk])
                    for ti in range(nt_):
                        for c2 in range(2):
                            bk = ti * 2 + c2
                            cg = half * 2 + c2
                            t1, t1d = t1r.nxt()
                            op("dve", lambda: V_.tensor_tensor(out=t1[:], in0=PS[bk][:, :], in1=gM[:, cg * 512:(cg + 1) * 512], op=ALU.mult),
                               rd=PSD[bk] + [dgt], wr=[t1d])
                            op("dve", lambda: V_.tensor_tensor(out=xs[:, ti, cg * 512:(cg + 1) * 512], in0=xs[:, ti, cg * 512:(cg + 1) * 512],
                                                               in1=t1[:], op=ALU.add), rd=[t1d], wr=[dxs[ti]])
                for ti in range(nt_):
                    k = k0 + ti
                    junk, ss, ms, sd, rstd, xn, dsm2, dxn = ntt
                    xa = xs[:, ti, :]
                    op("act", lambda: A_.activation(out=junk[:], in_=xa, func=AF.Square, accum_out=ss[:]), rd=[dxs[ti]], wr=[dsm2])
                    op("dve", lambda: V_.tensor_scalar(out=ms[:], in0=ss[:], scalar1=1.0 / D, scalar2=EPS, op0=ALU.mult, op1=ALU.add),
                       rd=[dsm2], wr=[dsm2])
                    op("act", lambda: A_.activation(out=sd[:], in_=ms[:], func=AF.Sqrt), rd=[dsm2], wr=[dsm2])
                    op("dve", lambda: V_.reciprocal(out=rstd[:], in_=sd[:]), rd=[dsm2], wr=[dsm2])
                    op("dve", lambda: V_.scalar_tensor_tensor(out=xn[:], in0=xa, scalar=rstd[:, 0:1], in1=gFt[:], op0=ALU.mult, op1=ALU.mult),
                       rd=[dsm2, dxs[ti], dgt], wr=[dxn])
                    dma("sp", y[k * 128:(k + 1) * 128, :], xn[:], rd=[dxn], acc=[dY], own=dxn)
            s.barrier()
    return nc


def _t5_bucket_np(d):
    d = np.maximum(d, 0)
    df = np.maximum(d, 1).astype(np.float32)
    large = 16 + (np.log(df / np.float32(16)) / np.float32(math.log(8.0)) * np.float32(16)).astype(np.int32)
    large = np.minimum(large, 31)
    return np.where(d < 16, d, large)


def _lay(w, gsz):
    ncols = w.shape[1]
    G = ncols // gsz
    return np.ascontiguousarray(w.reshape(16, 128, G, gsz).transpose(2, 1, 0, 3)).reshape(G, 128, 16 * gsz)


_PROGRAM = None


def prep(x, c, rel_bias, w_ada, b_ada, g_attn, w_in, b_forget, w_out, g_mlp, w_up, conv_w, conv_b, w_down, g_final):
    f32 = np.float32
    x = np.asarray(x, f32)[0]
    w_in = np.asarray(w_in, f32)[0]
    qa, ka, va = w_in[:, 0:1024], w_in[:, 1024:2048], w_in[:, 2048:3072]
    qi, ki, wi = w_in[:, 3072:3584], w_in[:, 3584:3648], w_in[:, 3648:3656]
    qb, kb, vb = w_in[:, 3656:4680], w_in[:, 4680:5704], w_in[:, 5704:6728]
    gb, fb = w_in[:, 6728:7752], w_in[:, 7752:7760]

    common = {}
    common["cT"] = np.ascontiguousarray(np.asarray(c, f32)[0].reshape(16, 128).T)
    common["gattn"] = np.ascontiguousarray(np.asarray(g_attn, f32)[0].reshape(16, 128).T)
    common["gmlp"] = np.ascontiguousarray(np.asarray(g_mlp, f32)[0].reshape(16, 128).T)
    common["bada"] = np.ascontiguousarray(np.asarray(b_ada, f32)[0].reshape(96, 128).T)
    common["wada"] = _lay(np.asarray(w_ada, f32)[0], 128)
    common["wkf"] = _lay(np.concatenate([ka, kb, ki, ki], axis=1), 128)
    wfb = np.zeros((128, 128), f32)
    wfb[:, :] = fb.reshape(16, 128, 8).transpose(1, 0, 2).reshape(128, 128)
    common["wfb"] = wfb
    common["wv"] = np.ascontiguousarray(np.concatenate([va, vb], axis=1).reshape(16, 128, 2048).transpose(1, 0, 2)).reshape(128, 16 * 2048)
    common["wqf"] = _lay(np.concatenate([qa, qb, gb, qi], axis=1), 128)
    common["wwi"] = np.ascontiguousarray(wi.reshape(16, 128, 8).transpose(1, 0, 2)).reshape(128, 128)
    common["bfor"] = np.ascontiguousarray(np.repeat(np.asarray(b_forget, f32)[0], 16).reshape(128, 1))
    wo = np.asarray(w_out, f32)[0]
    common["wout"] = np.ascontiguousarray(wo.reshape(16, 128, 4, 512).transpose(2, 1, 0, 3)).reshape(4, 128, 16 * 512)
    common["wup"] = _lay(np.asarray(w_up, f32)[0], 128)
    cwv = np.asarray(conv_w, f32)[0]
    common["cw"] = np.ascontiguousarray(cwv.reshape(3, NG_UP, 128).transpose(2, 0, 1)).reshape(128, 3 * NG_UP)
    common["cb"] = np.ascontiguousarray(np.asarray(conv_b, f32)[0].reshape(NG_UP, 128).T)
    common["wdown"] = np.ascontiguousarray(np.asarray(w_down, f32)[0].reshape(NFC, 128, 2048))
    common["gF"] = np.ascontiguousarray(np.broadcast_to(np.asarray(g_final, f32)[None, :], (128, D)))
    common["identf"] = np.eye(128, dtype=f32)
    tm = np.zeros((128, 128), f32)
    for h in range(8):
        for s2 in range(16):
            tm[h * 16: h * 16 + s2, h * 16 + s2] = 1.0
    common["tm"] = tm
    tl = np.arange(128)[:, None]
    u = np.arange(512)[None, :]
    common["Ib"] = np.where(u - 128 <= tl, 0.0, NEG).astype(f32)
    ub = np.arange(BW)[None, :]
    dd = ub - tl - 128
    common["Cb"] = np.where(dd < 0, NEG, 0.0).astype(f32)
    rb = np.asarray(rel_bias, f32)
    bk = _t5_bucket_np(dd)
    bank = rb[bk]
    bank = np.where((dd >= 0)[:, :, None], bank, np.float32(0.0))
    common["bankB"] = np.ascontiguousarray(bank.transpose(0, 2, 1)).reshape(128, 8 * BW).astype(f32)
    common["rb31"] = np.ascontiguousarray(np.broadcast_to(rb[31][None, :], (128, 8))).astype(f32)

    in_maps = []
    for i in range(NCORE):
        pad = pad_of(i)
        n = min(SEQ, NTOK - pad)
        xloc = np.zeros((NTOK, D), f32)
        xloc[pad:pad + n] = x[:n]
        p = np.arange(NTOK)
        isp = (p < pad) | (p >= pad + SEQ)
        padrow = np.where(isp, NEG, 0.0).astype(f32)
        m = dict(common)
        m["xl"] = xloc
        m["padb"] = np.ascontiguousarray(np.broadcast_to(padrow[None, :1024], (128, 1024)))
        hvv = np.ones((NT,), f32)
        if i == 0:
            hvv[0] = 0.0
        m["hv"] = np.ascontiguousarray(np.broadcast_to(hvv[None, :], (128, NT)))
        fl_t = np.zeros((8, NTOK), f32)
        fl_t[0:3] = 1.0
        fl_t[6] = padrow
        fr_t = np.zeros((8, NTOK), f32)
        fr_t[3:7] = 1.0
        m["fixl_t"] = fl_t.astype(ml_dtypes.bfloat16)
        m["fixr_t"] = fr_t.astype(ml_dtypes.bfloat16)
        in_maps.append(m)
    return in_maps


def kernel(**inputs):
    global _PROGRAM
    f32 = np.float32
    in_maps = prep(**inputs)
    if _PROGRAM is None:
        _PROGRAM = build_program()
    res = run_bass_kernel_spmd(_PROGRAM, in_maps, core_ids=list(range(NCORE)), trace=True)
    out = np.zeros((1, SEQ, D), f32)
    for i in range(NCORE):
        yi = np.asarray(res.results[i]["y"]).reshape(NT, 128, D)
        for k in range(NT):
            j = 8 * k + i
            g0 = 126 * j
            if g0 >= SEQ:
                continue
            cnt = min(126, SEQ - g0)
            out[0, g0:g0 + cnt] = yi[k, 2:2 + cnt]
    return out
```

```python
import math
from contextlib import ExitStack

import numpy as np
import ml_dtypes

import concourse.bass as bass
import concourse.mybir as mybir
from concourse.bass_utils import run_bass_kernel_spmd

F32 = mybir.dt.float32
BF = mybir.dt.bfloat16
U8 = mybir.dt.uint8
AF = mybir.ActivationFunctionType
ALU = mybir.AluOpType
AX = mybir.AxisListType

D = 2048
SEQ = 16384
NCORE = 8
NT = 17
NBLK = 134
NTOK = NBLK * 128
SEG = NTOK // 16
NEG = -1.0e30
EPS = 1e-6
NITER = 24
BW = 512
DFF = 5504
NG_UP = 86
NFC = 43


def Lk(k):
    return 1008 * k + 882


def pad_of(i):
    return 884 - 126 * i


class Dep:
    __slots__ = ("w", "r", "p", "ds")

    def __init__(self):
        self.w = {}
        self.r = {}
        self.p = {}
        self.ds = None

    def reset(self, key, ev):
        p = dict(self.w)
        for k, (s_, v) in self.r.items():
            if k not in p or p[k][1] < v:
                p[k] = (s_, v)
        self.p = p
        self.w = {key: ev}
        self.r = {}


class DSem:
    def __init__(self, sem, key):
        self.sem = sem
        self.key = key
        self.n = 0


class Sch:
    def __init__(self, nc, es):
        self.nc = nc
        self.es = es
        self.E = {"pe": nc.tensor, "act": nc.scalar, "dve": nc.vector, "pool": nc.gpsimd, "sp": nc.sync}
        self.sem = {k: es.enter_context(nc.semaphore("s_" + k)) for k in self.E}
        self.cnt = {k: 0 for k in self.E}
        self.waited = {k: {} for k in self.E}
        self.dsems = []

    def _need(self, rd, wr, acc):
        need = {}

        def add(dct):
            for k, (s, v) in dct.items():
                if k not in need or need[k][1] < v:
                    need[k] = (s, v)

        for d in rd:
            add(d.w)
        for d in wr:
            add(d.w)
            add(d.r)
        for d in acc:
            add(d.r)
            add(d.p)
        return need

    def _wait(self, eng, need):
        for key, (sem, val) in need.items():
            if self.waited[eng].get(key, 0) < val:
                self.E[eng].wait_ge(sem, val)
                self.waited[eng][key] = val

    def op(self, eng, fn, rd=(), wr=(), acc=()):
        need = self._need(rd, wr, acc)
        if eng == "pe":
            need.pop("pe", None)
        self._wait(eng, need)
        inst = fn()
        self.cnt[eng] += 1
        v = self.cnt[eng]
        inst.then_inc(self.sem[eng], 1)
        ev = (self.sem[eng], v)
        for d in wr:
            d.reset(eng, ev)
        for d in acc:
            d.w[eng] = ev
        for d in rd:
            d.r[eng] = ev
        return inst

    def _dsem(self, d):
        if d.ds is None:
            key = "d%d" % len(self.dsems)
            sem = self.es.enter_context(self.nc.semaphore(key))
            d.ds = DSem(sem, key)
            self.dsems.append(d.ds)
        return d.ds

    def dma(self, q, out, in_, rd=(), wr=(), acc=(), own=None, **kw):
        need = self._need(rd, wr, acc)
        self._wait(q, need)
        inst = self.E[q].dma_start(out=out, in_=in_, **kw)
        ds = self._dsem(own)
        ds.n += 16
        inst.then_inc(ds.sem, 16)
        ev = (ds.sem, ds.n)
        for d in wr:
            d.reset(ds.key, ev)
        for d in acc:
            d.w[ds.key] = ev
        for d in rd:
            d.r[ds.key] = ev
        return inst

    def barrier(self):
        need = {k: (self.sem[k], self.cnt[k]) for k in self.E if self.cnt[k] > 0}
        for ds in self.dsems:
            if ds.n > 0:
                need[ds.key] = (ds.sem, ds.n)
        for e in self.E:
            nd = dict(need)
            if e in nd and e != "sp":
                pass
            self._wait(e, nd)


class Ring:
    def __init__(self, tiles):
        self.t = tiles
        self.d = [Dep() for _ in tiles]
        self.i = -1

    def nxt(self):
        self.i = (self.i + 1) % len(self.t)
        return self.t[self.i], self.d[self.i]


RUN_NT = NT
RUN_NBLK = NBLK


def build_program():
    NT_R, NBLK_R = RUN_NT, RUN_NBLK
    nc = bass.Bass("TRN2", target_bir_lowering=False)

    def din(name, shape, dt=F32):
        return nc.dram_tensor(name, list(shape), dt, kind="ExternalInput").ap()

    def dscr(name, shape, dt):
        return nc.dram_tensor(name, list(shape), dt, kind="Internal").ap()

    xl = din("xl", [NTOK, D])
    cT_d = din("cT", [128, 16])
    gattn_d = din("gattn", [128, 16])
    gmlp_d = din("gmlp", [128, 16])
    bada_d = din("bada", [128, 96])
    wada_d = din("wada", [96, 128, 2048])
    wkf_d = din("wkf", [17, 128, 2048])
    wfb_d = din("wfb", [128, 128])
    wv_d = din("wv", [128, 16 * 2048])
    wqf_d = din("wqf", [28, 128, 2048])
    wwi_d = din("wwi", [128, 128])
    bfor_d = din("bfor", [128, 1])
    wout_d = din("wout", [4, 128, 16 * 512])
    wup_d = din("wup", [NG_UP, 128, 2048])
    cw_d = din("cw", [128, 3 * NG_UP])
    cb_d = din("cb", [128, NG_UP])
    wdown_d = din("wdown", [NFC, 128, 2048])
    gF_d = din("gF", [128, D])
    identf_d = din("identf", [128, 128])
    tm_d = din("tm", [128, 128])
    padb_d = din("padb", [128, 1024])
    Ib_d = din("Ib", [128, 512])
    Cb_d = din("Cb", [128, BW])
    bankB_d = din("bankB", [128, 8 * BW])
    rb31_d = din("rb31", [128, 8])
    hv_d = din("hv", [128, NT])
    fixl_t = din("fixl_t", [8, NTOK], BF)
    fixr_t = din("fixr_t", [8, NTOK], BF)

    y = nc.dram_tensor("y", [NT * 128, D], F32, kind="ExternalOutput").ap()

    KT = dscr("KT", [16, 128, NTOK], BF)
    V = dscr("V", [NTOK, 2048], BF)
    KI = dscr("KI", [128, NTOK], BF)
    FBT = dscr("FBT", [8, NTOK], F32)
    FIXL = dscr("FIXL", [8, 8, NTOK], BF)
    FIXR = dscr("FIXR", [8, 8, NTOK], BF)
    QS = dscr("QS", [20, 128, NT * 128], BF)
    GS = dscr("GS", [8, 128, NT * 128], F32)
    WIS = dscr("WIS", [NT * 128, 8], F32)
    MIX = dscr("MIX", [NT, 128, 2048], BF)
    dKT, dV, dKI, dFBT, dFIXL, dFIXR, dQS, dGS, dWIS, dMIX, dY = [Dep() for _ in range(11)]

    with ExitStack() as es:
        es.enter_context(nc.allow_low_precision("bf16 matmul operands, fp32 accumulation"))
        s = Sch(nc, es)
        op, dma = s.op, s.dma
        V_, A_, P_ = nc.vector, nc.scalar, nc.tensor

        nmc = [0]

        def sbt(stack, name, shape, dt):
            nmc[0] += 1
            return stack.enter_context(nc.sbuf_tensor("sb%d_%s" % (nmc[0], name), list(shape), dt))

        def ring(stack, name, n, shape, dt):
            return Ring([sbt(stack, "%s%d" % (name, i), shape, dt) for i in range(n)])

        PS = [es.enter_context(nc.psum_tensor("ps%d" % i, [128, 512], F32)) for i in range(7)]
        PSD = [[Dep()] * 4 for _ in range(7)]
        PST = es.enter_context(nc.psum_tensor("pst", [128, 1024], BF))
        PSTD = [Dep()] * 2

        identf = sbt(es, "identf", [128, 128], F32)
        identb = sbt(es, "identb", [128, 128], BF)
        onesb = sbt(es, "onesb", [128, 128], BF)
        onesf = sbt(es, "onesf", [128, 128], F32)
        modT = sbt(es, "modT", [128, 96], F32)
        aA = sbt(es, "aA", [128, 16], F32)
        aM = sbt(es, "aM", [128, 16], F32)
        hv = sbt(es, "hv", [128, NT], F32)
        dC = Dep()
        dma("sp", identf[:], identf_d, wr=[dC], own=dC)
        dma("sp", hv[:], hv_d, acc=[dC], own=dC)
        op("dve", lambda: V_.tensor_copy(out=identb[:], in_=identf[:]), rd=[dC], acc=[dC])
        op("dve", lambda: V_.memset(onesb[:], 1.0), acc=[dC])
        op("dve", lambda: V_.memset(onesf[:], 1.0), acc=[dC])

        with ExitStack() as ph:
            cact = sbt(ph, "cact", [128, 16], F32)
            g1 = sbt(ph, "g1", [128, 16], F32)
            g2 = sbt(ph, "g2", [128, 16], F32)
            bada = sbt(ph, "bada", [128, 96], F32)
            tmp16 = sbt(ph, "tmp16", [128, 16], F32)
            d0 = Dep()
            dma("sp", cact[:], cT_d, wr=[d0], own=d0)
            dma("sp", g1[:], gattn_d, acc=[d0], own=d0)
            dma("sp", g2[:], gmlp_d, acc=[d0], own=d0)
            dma("sp", bada[:], bada_d, acc=[d0], own=d0)
            dca = Dep()
            op("act", lambda: A_.activation(out=cact[:], in_=cact[:], func=AF.Silu), rd=[d0], wr=[dca])
            war = ring(ph, "wa", 3, [128, 2048], F32)
            for g in range(96):
                wt, wd = war.nxt()
                dma("sp", wt[:], wada_d[g], wr=[wd], own=wd)
                for c in range(16):
                    op("pe", lambda: P_.matmul(PS[0][:, g:g + 1], wt[:, c * 128:(c + 1) * 128], cact[:, c:c + 1],
                                               start=(c == 0), stop=(c == 15)),
                       rd=[wd, dca], wr=PSD[0] if (g == 0 and c == 0) else (), acc=() if (g == 0 and c == 0) else PSD[0])
            dmod = Dep()
            op("dve", lambda: V_.tensor_tensor(out=modT[:], in0=PS[0][:, 0:96], in1=bada[:], op=ALU.add),
               rd=PSD[0] + [d0], wr=[dmod])
            op("dve", lambda: V_.tensor_scalar(out=tmp16[:], in0=modT[:, 16:32], scalar1=1.0, scalar2=None, op0=ALU.add),
               rd=[dmod], wr=[d0])
            op("dve", lambda: V_.tensor_tensor(out=aA[:], in0=tmp16[:], in1=g1[:], op=ALU.mult), rd=[d0], acc=[dC])
            op("dve", lambda: V_.tensor_scalar(out=tmp16[:], in0=modT[:, 64:80], scalar1=1.0, scalar2=None, op0=ALU.add),
               rd=[dmod], wr=[d0])
            op("dve", lambda: V_.tensor_tensor(out=aM[:], in0=tmp16[:], in1=g2[:], op=ALU.mult), rd=[d0], acc=[dC])
            s.barrier()
        bA = modT[:, 0:16]
        bM = modT[:, 48:64]

        def norm_transpose(stack_tiles, x_ap, x_dep, aT, bT, dst_fn, dst_dep, first):
            junk, ss, ms, sd, rstd, xn, dsm, dxn = stack_tiles
            op("act", lambda: A_.activation(out=junk[:], in_=x_ap, func=AF.Square, accum_out=ss[:]),
               rd=[x_dep], wr=[dsm])
            op("dve", lambda: V_.tensor_scalar(out=ms[:], in0=ss[:], scalar1=1.0 / D, scalar2=EPS, op0=ALU.mult, op1=ALU.add),
               rd=[dsm], wr=[dsm])
            op("act", lambda: A_.activation(out=sd[:], in_=ms[:], func=AF.Sqrt), rd=[dsm], wr=[dsm])
            op("dve", lambda: V_.reciprocal(out=rstd[:], in_=sd[:]), rd=[dsm], wr=[dsm])
            op("dve", lambda: V_.tensor_scalar(out=xn[:], in0=x_ap, scalar1=rstd[:, 0:1], scalar2=None, op0=ALU.mult),
               rd=[dsm, x_dep], wr=[dxn])
            for j in range(4):
                b = 5 + j % 2
                for q in range(4):
                    c = 4 * j + q
                    op("pe", lambda: P_.transpose(PS[b][:, q * 128:(q + 1) * 128], xn[:, c * 128:(c + 1) * 128], identf[:]),
                       rd=[dxn, dC], wr=[PSD[b][q]] if q == 0 else (), acc=() if q == 0 else [PSD[b][q]])
                for q in range(4):
                    c = 4 * j + q
                    op("act", lambda: A_.activation(out=dst_fn(c), in_=PS[b][:, q * 128:(q + 1) * 128], func=AF.Identity,
                                                    scale=aT[:, c:c + 1], bias=bT[:, c:c + 1]),
                       rd=[PSD[b][q], dmod, dC], wr=[dst_dep] if (first and c == 0) else (), acc=() if (first and c == 0) else [dst_dep])

        ntc = [0]

        def nt_tiles(stack):
            ntc[0] += 1
            u = "_%d" % ntc[0]
            return (sbt(stack, "junk" + u, [128, D], BF), sbt(stack, "ss" + u, [128, 1], F32), sbt(stack, "ms" + u, [128, 1], F32),
                    sbt(stack, "sd" + u, [128, 1], F32), sbt(stack, "rstd" + u, [128, 1], F32), sbt(stack, "xn" + u, [128, D], F32),
                    Dep(), Dep())

        bankctr = [0]

        def next_bank(lo=0, hi=5):
            bankctr[0] = (bankctr[0] + 1) % (hi - lo)
            return lo + bankctr[0]

        evctr = [0]

        def evac_copy(out_ap, in_ap, rd, wr=(), acc=(), scale=None):
            evctr[0] += 1
            if evctr[0] % 2 == 0:
                if scale is None:
                    op("act", lambda: A_.copy(out=out_ap, in_=in_ap), rd=rd, wr=wr, acc=acc)
                else:
                    op("act", lambda: A_.activation(out=out_ap, in_=in_ap, func=AF.Copy, scale=scale), rd=rd, wr=wr, acc=acc)
            else:
                if scale is None:
                    op("dve", lambda: V_.tensor_copy(out=out_ap, in_=in_ap), rd=rd, wr=wr, acc=acc)
                else:
                    op("dve", lambda: V_.tensor_scalar(out=out_ap, in0=in_ap, scalar1=scale, scalar2=None, op0=ALU.mult),
                       rd=rd, wr=wr, acc=acc)

        def proj_fm(wring, hT, hT_dep, ntok, wsrc, M, evac):
            wt, wd = wring.nxt()
            dma("pool", wt[:, :16 * M], wsrc, wr=[wd], own=wd)
            for t0 in range(0, ntok, 512):
                n = min(512, ntok - t0)
                b = next_bank()
                for c in range(16):
                    op("pe", lambda: P_.matmul(PS[b][:M, :n], wt[:, c * M:(c + 1) * M], hT[:, c, t0:t0 + n],
                                               start=(c == 0), stop=(c == 15)),
                       rd=[wd, hT_dep], wr=PSD[b] if c == 0 else (), acc=() if c == 0 else PSD[b])
                evac(b, t0, n)

        with ExitStack() as ph:
            SB = 8
            hT = sbt(ph, "hT", [128, 16, SB * 128], BF)
            dhT = Dep()
            wv = sbt(ph, "wv", [128, 16 * 2048], BF)
            dwv = Dep()
            for j in range(16):
                dma("pool", wv[:, j * 2048:(j + 1) * 2048], wv_d[:, j * 2048:(j + 1) * 2048],
                    wr=[dwv] if j == 0 else (), acc=() if j == 0 else [dwv], own=dwv)
            xr = ring(ph, "xb", 2, [128, D], F32)
            ntt = nt_tiles(ph)
            wgr = ring(ph, "wg", 3, [128, 2048], BF)
            stg = ring(ph, "stg", 2, [128, SB * 128], BF)
            stf = sbt(ph, "stf", [8, SB * 128], F32)
            dstf = Dep()
            vst = ring(ph, "vst", 2, [128, 2048], BF)
            for sb0 in range(0, NBLK_R, SB):
                nb = min(SB, NBLK_R - sb0)
                ntok = nb * 128
                tok0 = sb0 * 128
                for bi in range(nb):
                    xt, xd = xr.nxt()
                    dma("sp", xt[:], xl[tok0 + bi * 128: tok0 + (bi + 1) * 128, :], wr=[xd], own=xd)
                    norm_transpose(ntt, xt[:], xd, aA, bA, lambda c: hT[:, c, bi * 128:(bi + 1) * 128], dhT, bi == 0)
                for g in range(17):
                    st, sd_ = stg.nxt()

                    def ev(b, t0, n, st=st, sd_=sd_):
                        evac_copy(st[:, t0:t0 + n], PS[b][:, :n], rd=PSD[b], wr=[sd_] if t0 == 0 else (),
                                  acc=() if t0 == 0 else [sd_])
                    proj_fm(wgr, hT, dhT, ntok, wkf_d[g], 128, ev)
                    if g < 16:
                        dma("sp", KT[g, :, tok0:tok0 + ntok], st[:, :ntok], rd=[sd_], acc=[dKT], own=sd_)
                    else:
                        dma("sp", KI[:, tok0:tok0 + ntok], st[:, :ntok], rd=[sd_], acc=[dKI], own=sd_)

                def evf(b, t0, n):
                    evac_copy(stf[:, t0:t0 + n], PS[b][:8, :n], rd=PSD[b], wr=[dstf] if t0 == 0 else (),
                              acc=() if t0 == 0 else [dstf])
                proj_fm(wgr, hT, dhT, ntok, wfb_d, 8, evf)
                dma("sp", FBT[:, tok0:tok0 + ntok], stf[:, :ntok], rd=[dstf], acc=[dFBT], own=dstf)
                for bi in range(nb):
                    vt, vd = vst.nxt()
                    for cg in range(4):
                        b = next_bank()
                        for c in range(16):
                            op("pe", lambda: P_.matmul(PS[b][:, :], hT[:, c, bi * 128:(bi + 1) * 128],
                                                       wv[:, c * 2048 + cg * 512: c * 2048 + (cg + 1) * 512],
                                                       start=(c == 0), stop=(c == 15)),
                               rd=[dwv, dhT], wr=PSD[b] if c == 0 else (), acc=() if c == 0 else PSD[b])
                        evac_copy(vt[:, cg * 512:(cg + 1) * 512], PS[b][:, :], rd=PSD[b], wr=[vd] if cg == 0 else (),
                                  acc=() if cg == 0 else [vd])
                    dma("sp", V[tok0 + bi * 128: tok0 + (bi + 1) * 128, :], vt[:], rd=[vd], acc=[dV], own=vd)
            s.barrier()

        with ExitStack() as ph:
            fl = sbt(ph, "fl", [128, SEG], F32)
            e1 = sbt(ph, "e1", [128, SEG], F32)
            onesg = sbt(ph, "onesg", [128, SEG], F32)
            Fc = sbt(ph, "Fc", [128, SEG], F32)
            r1 = sbt(ph, "r1", [128, SEG], F32)
            r2 = sbt(ph, "r2", [128, SEG], F32)
            fb16 = [sbt(ph, "fb16_%d" % i, [128, SEG], BF) for i in range(6)]
            tm = sbt(ph, "tm", [128, 128], F32)
            nbf = sbt(ph, "nbf", [128, 1], F32)
            off = sbt(ph, "off", [128, 1], F32)
            tot = sbt(ph, "tot", [128, 1], F32)
            dF = Dep()
            dF2 = Dep()
            dma("sp", fl[:], FBT.rearrange("h (g n) -> (h g) n", n=SEG), rd=[dFBT], wr=[dF], own=dF)
            dma("sp", tm[:], tm_d, acc=[dF], own=dF)
            dma("sp", nbf[:], bfor_d, acc=[dF], own=dF)
            dTL = Dep()
            for h in range(8):
                dma("sp", FIXL[:, h, :], fixl_t, acc=[dFIXL, dTL], own=dTL)
                dma("sp", FIXR[:, h, :], fixr_t, acc=[dFIXR, dTL], own=dTL)
            op("dve", lambda: V_.tensor_scalar(out=nbf[:], in0=nbf[:], scalar1=-1.0, scalar2=None, op0=ALU.mult),
               rd=[dF], wr=[dF2])
            op("dve", lambda: V_.memset(onesg[:], 1.0), acc=[dF2])
            op("act", lambda: A_.activation(out=e1[:], in_=fl[:], func=AF.Exp, scale=-1.0, bias=nbf[:, 0:1]),
               rd=[dF, dF2], acc=[dF2])
            op("act", lambda: A_.activation(out=e1[:], in_=e1[:], func=AF.Ln, bias=1.0, scale=1.0), rd=[dF2], wr=[dF2])
            op("dve", lambda: V_.tensor_tensor_scan(Fc[:], onesg[:], e1[:], 0.0, ALU.mult, ALU.subtract),
               rd=[dF2], wr=[dF])
            op("dve", lambda: V_.tensor_copy(out=tot[:], in_=Fc[:, SEG - 1:SEG]), rd=[dF], acc=[dF])
            op("pe", lambda: P_.matmul(PS[0][:, 0:1], tm[:], tot[:], start=True, stop=True), rd=[dF], wr=PSD[0])
            op("dve", lambda: V_.tensor_copy(out=off[:], in_=PS[0][:, 0:1]), rd=PSD[0], acc=[dF])
            op("dve", lambda: V_.tensor_scalar(out=Fc[:], in0=Fc[:], scalar1=off[:, 0:1], scalar2=None, op0=ALU.add),
               rd=[dF], wr=[dF])
            op("dve", lambda: V_.tensor_copy(out=fb16[0][:], in_=Fc[:]), rd=[dF], wr=[dF2])
            op("dve", lambda: V_.tensor_tensor(out=r1[:], in0=Fc[:], in1=fb16[0][:], op=ALU.subtract), rd=[dF, dF2], acc=[dF2])
            op("dve", lambda: V_.tensor_copy(out=fb16[1][:], in_=r1[:]), rd=[dF2], acc=[dF2])
            op("dve", lambda: V_.tensor_tensor(out=r2[:], in0=r1[:], in1=fb16[1][:], op=ALU.subtract), rd=[dF2], acc=[dF2])
            op("dve", lambda: V_.tensor_copy(out=fb16[2][:], in_=r2[:]), rd=[dF2], acc=[dF2])
            for j in range(3):
                op("dve", lambda: V_.tensor_scalar(out=fb16[3 + j][:], in0=fb16[j][:], scalar1=-1.0, scalar2=None, op0=ALU.mult),
                   rd=[dF2], acc=[dF2])
            dst = Dep()
            for j in range(3):
                dma("sp", FIXR[j].rearrange("h (g n) -> (h g) n", n=SEG), fb16[j][:], rd=[dF2, dTL], acc=[dFIXR], own=dst)
                dma("sp", FIXL[3 + j].rearrange("h (g n) -> (h g) n", n=SEG), fb16[3 + j][:], rd=[dF2, dTL], acc=[dFIXL], own=dst)
            s.barrier()

        with ExitStack() as ph:
            hq = sbt(ph, "hq", [128, 16, 512], BF)
            dhq = Dep()
            xr = ring(ph, "xq", 2, [128, D], F32)
            ntt = nt_tiles(ph)
            wgr = ring(ph, "wgq", 3, [128, 2048], BF)
            stq = ring(ph, "stq", 2, [128, 512], BF)
            stgf = ring(ph, "stgf", 2, [128, 512], F32)
            wwi = sbt(ph, "wwi", [128, 128], BF)
            wis = sbt(ph, "wis", [128, 8], F32)
            dwwi = Dep()
            dwis = Dep()
            dma("pool", wwi[:], wwi_d, wr=[dwwi], own=dwwi)
            for k0 in range(0, NT_R, 4):
                nt_ = min(4, NT_R - k0)
                ntok = nt_ * 128
                for ti in range(nt_):
                    k = k0 + ti
                    xt, xd = xr.nxt()
                    dma("sp", xt[:], xl[Lk(k): Lk(k) + 128, :], wr=[xd], own=xd)
                    norm_transpose(ntt, xt[:], xd, aA, bA, lambda c: hq[:, c, ti * 128:(ti + 1) * 128], dhq, ti == 0)
                for g in range(28):
                    if 16 <= g < 24:
                        st, sd_ = stgf.nxt()

                        def ev(b, t0, n, st=st, sd_=sd_):
                            op("act", lambda: A_.activation(out=st[:, t0:t0 + n], in_=PS[b][:, :n], func=AF.Sigmoid),
                               rd=PSD[b], wr=[sd_])
                        proj_fm(wgr, hq, dhq, ntok, wqf_d[g], 128, ev)
                        dma("sp", GS[g - 16, :, k0 * 128:k0 * 128 + ntok], st[:, :ntok], rd=[sd_], acc=[dGS], own=sd_)
                    else:
                        st, sd_ = stq.nxt()
                        sc = (128.0 ** -0.5) if g < 16 else 1.0

                        def ev(b, t0, n, st=st, sd_=sd_, sc=sc):
                            evac_copy(st[:, t0:t0 + n], PS[b][:, :n], rd=PSD[b], wr=[sd_], scale=sc)
                        proj_fm(wgr, hq, dhq, ntok, wqf_d[g], 128, ev)
                        gi = g if g < 16 else g - 8
                        dma("sp", QS[gi, :, k0 * 128:k0 * 128 + ntok], st[:, :ntok], rd=[sd_], acc=[dQS], own=sd_)
                for ti in range(nt_):
                    k = k0 + ti
                    b = next_bank()
                    for c in range(16):
                        op("pe", lambda: P_.matmul(PS[b][:, 0:8], hq[:, c, ti * 128:(ti + 1) * 128], wwi[:, c * 8:(c + 1) * 8],
                                                   start=(c == 0), stop=(c == 15)),
                           rd=[dwwi, dhq], wr=PSD[b] if c == 0 else (), acc=() if c == 0 else PSD[b])
                    op("dve", lambda: V_.tensor_copy(out=wis[:], in_=PS[b][:, 0:8]), rd=PSD[b], wr=[dwis])
                    dma("sp", WIS[k * 128:(k + 1) * 128, :], wis[:], rd=[dwis], acc=[dWIS], own=dwis)
            s.barrier()

        with ExitStack() as ph:
            SMAX = NTOK
            scores = sbt(ph, "scores", [128, SMAX], F32)
            dsc = Dep()
            maskT = sbt(ph, "maskT", [128, SMAX], U8)
            dmk = Dep()
            qT = sbt(ph, "qT", [128, 20, 128], BF)
            dq = Dep()
            wi = sbt(ph, "wi", [128, 8], F32)
            dwi = Dep()
            dg = sbt(ph, "dg", [128, 8, 128], BF)
            ddg = Dep()
            kir = ring(ph, "kich", 3, [128, 512], BF)
            rbr = ring(ph, "rbuf", 3, [128, 512], BF)
            mcr = ring(ph, "mch", 4, [128, 512], BF)
            ktr = ring(ph, "kt", 3, [128, 8, 256], BF)
            vcr = ring(ph, "vch", 3, [128, 2, 1024], BF)
            ptr_ = ring(ph, "pT", 3, [128, 512], BF)
            pmr = ring(ph, "pm", 3, [128, 512], BF)
            bankH = sbt(ph, "bankH", [128, 8, BW], BF)
            Cb = sbt(ph, "Cb", [128, BW], BF)
            Ib = sbt(ph, "Ib", [128, 512], F32)
            padb = sbt(ph, "padb", [128, 1024], F32)
            rb31 = sbt(ph, "rb31", [128, 8], F32)
            flr = ring(ph, "fixl", 3, [8, 8, 256], BF)
            fixr = sbt(ph, "fixr", [8, 8, 128], BF)
            dfr = Dep()
            GT = sbt(ph, "GT", [128, 8, 128], F32)
            dGT = Dep()
            rr = sbt(ph, "rr", [128, 1024], F32)
            drr = Dep()
            mixT = sbt(ph, "mixT", [128, 16, 128], BF)
            dmx = Dep()
            sm = {n: sbt(ph, "sm_" + n, [128, 1], F32) for n in ("mx", "mn", "w0", "lo", "mid", "cnt", "pred")}
            dsm = Dep()
            dK = Dep()
            bankf = scores[:, 0:8 * BW].rearrange("p (h u) -> p h u", u=BW)
            dma("sp", scores[:, 0:8 * BW], bankB_d, wr=[dsc], own=dsc)
            dma("sp", rb31[:], rb31_d, wr=[dK], own=dK)
            dma("sp", Ib[:], Ib_d, acc=[dK], own=dK)
            dma("sp", padb[:], padb_d, acc=[dK], own=dK)
            dK2 = Dep()
            dma("pool", Cb[:], Cb_d, wr=[dK2], own=dK2)
            dKb = Dep()
            for h in range(8):
                op("dve", lambda: V_.tensor_scalar(out=bankH[:, h, :], in0=bankf[:, h, :], scalar1=rb31[:, h:h + 1], scalar2=None,
                                                   op0=ALU.subtract), rd=[dK, dsc], acc=[dKb])

            for k in range(NT_R):
                L = Lk(k)
                bq, o = L // 128, L % 128
                NB = bq + 2
                S = NB * 128
                dma("sp", qT[:], QS[:, :, k * 128:(k + 1) * 128].rearrange("g p t -> p g t"), rd=[dQS], wr=[dq], own=dq)
                dma("sp", wi[:], WIS[k * 128:(k + 1) * 128, :], rd=[dWIS], wr=[dwi], own=dwi)
                dma("sp", GT[:], GS[:, :, k * 128:(k + 1) * 128].rearrange("g p t -> p g t"), rd=[dGS], wr=[dGT], own=dGT)
                dma("sp", fixr[:], FIXR[:, :, L:L + 128], rd=[dFIXR], wr=[dfr], own=dfr)
                for h in range(8):
                    op("dve", lambda: V_.tensor_scalar(out=dg[:, h, :], in0=identf[:], scalar1=wi[:, h:h + 1], scalar2=None,
                                                       op0=ALU.mult), rd=[dwi, dC], wr=[ddg] if h == 0 else (),
                       acc=() if h == 0 else [ddg])
                iunits = [(c0, h) for c0 in range(0, S, 512) for h in range(8)]
                ikb = {}
                ist = {}

                def idx_a(ui):
                    c0, h = iunits[ui]
                    n = min(512, S - c0)
                    if c0 not in ikb:
                        kt_, kd = kir.nxt()
                        dma("sp", kt_[:, :n], KI[:, c0:c0 + n], rd=[dKI], wr=[kd], own=kd)
                        ikb[c0] = (kt_, kd)
                    kt_, kd = ikb[c0]
                    g, po = h // 2, 64 * (h % 2)
                    b = ui % 3
                    op("pe", lambda: P_.matmul(PS[b][:, :n], qT[po:po + 64, 16 + g, :], kt_[po:po + 64, :n],
                                               start=True, stop=True), rd=[dq, kd], wr=PSD[b])
                    rt, rd_ = rbr.nxt()
                    op("act", lambda: A_.activation(out=rt[:, :n], in_=PS[b][:, :n], func=AF.Relu), rd=PSD[b], wr=[rd_])
                    ist[ui] = (rt, rd_)

                def idx_b(ui):
                    c0, h = iunits[ui]
                    n = min(512, S - c0)
                    ba = 3 + (c0 // 512) % 2
                    rt, rd_ = ist.pop(ui)
                    op("pe", lambda: P_.matmul(PS[ba][:, :n], dg[:, h, :], rt[:, :n], start=(h == 0), stop=(h == 7)),
                       rd=[ddg, rd_], wr=PSD[ba] if h == 0 else (), acc=() if h == 0 else PSD[ba])
                    if h == 7:
                        evac_copy(scores[:, c0:c0 + n], PS[ba][:, :n], rd=PSD[ba], wr=[dsc] if c0 == 0 else (),
                                  acc=() if c0 == 0 else [dsc])

                for ui in range(len(iunits) + 2):
                    if ui < len(iunits):
                        idx_a(ui)
                    if ui >= 2:
                        idx_b(ui - 2)
                def emit_topk():
                    op("dve", lambda: V_.tensor_reduce(out=sm["mx"][:], in_=scores[:, :S], axis=AX.X, op=ALU.max), rd=[dsc], wr=[dsm])
                    op("dve", lambda: V_.tensor_reduce(out=sm["mn"][:], in_=scores[:, :S], axis=AX.X, op=ALU.min), rd=[dsc], acc=[dsm])
                    op("dve", lambda: V_.tensor_tensor(out=scores[:, 0:1024], in0=scores[:, 0:1024], in1=padb[:], op=ALU.add),
                       rd=[dsc, dK], wr=[dsc])
                    op("dve", lambda: V_.tensor_tensor(out=scores[:, bq * 128:bq * 128 + 256], in0=scores[:, bq * 128:bq * 128 + 256],
                                                       in1=Ib[:, 128 - o:128 - o + 256], op=ALU.add), rd=[dsc, dK], wr=[dsc])
                    op("dve", lambda: V_.tensor_tensor(out=sm["w0"][:], in0=sm["mx"][:], in1=sm["mn"][:], op=ALU.subtract),
                       rd=[dsm], wr=[dsm])
                    op("dve", lambda: V_.tensor_scalar(out=sm["w0"][:], in0=sm["w0"][:], scalar1=1.0001, scalar2=1e-6,
                                                       op0=ALU.mult, op1=ALU.add), rd=[dsm], wr=[dsm])
                    op("dve", lambda: V_.tensor_copy(out=sm["lo"][:], in_=sm["mn"][:]), rd=[dsm], wr=[dsm])
                    for it in range(NITER):
                        hf = 2.0 ** -(it + 1)
                        op("dve", lambda: V_.scalar_tensor_tensor(out=sm["mid"][:], in0=sm["w0"][:], scalar=hf, in1=sm["lo"][:],
                                                                  op0=ALU.mult, op1=ALU.add), rd=[dsm], wr=[dsm])
                        op("dve", lambda: V_.tensor_scalar(out=maskT[:, :S], in0=scores[:, :S], scalar1=sm["mid"][:, 0:1], scalar2=None,
                                                           op0=ALU.is_ge, op1=ALU.add, accum_out=sm["cnt"][:]),
                           rd=[dsc, dsm], wr=[dmk, dsm])
                        op("dve", lambda: V_.tensor_scalar(out=sm["pred"][:], in0=sm["cnt"][:], scalar1=255.5, scalar2=hf,
                                                           op0=ALU.is_ge, op1=ALU.mult), rd=[dsm], wr=[dsm])
                        op("dve", lambda: V_.scalar_tensor_tensor(out=sm["lo"][:], in0=sm["pred"][:], scalar=sm["w0"][:, 0:1],
                                                                  in1=sm["lo"][:], op0=ALU.mult, op1=ALU.add), rd=[dsm], wr=[dsm])
                    tgt = [(PST[:, 0:512], PSTD[0]), (PS[4][:, :].bitcast(BF)[:, 0:512], PSD[4][0]),
                           (PS[5][:, :].bitcast(BF)[:, 0:512], PSD[5][0]), (PS[6][:, :].bitcast(BF)[:, 0:512], PSD[6][0])]
                    for c0 in range(0, S, 512):
                        n = min(512, S - c0)
                        mt, md = mcr.nxt()
                        op("dve", lambda: V_.tensor_scalar(out=mt[:, :n], in0=scores[:, c0:c0 + n], scalar1=sm["lo"][:, 0:1], scalar2=None,
                                                           op0=ALU.is_ge), rd=[dsc, dsm], wr=[md])
                        tap, tdep = tgt[(c0 // 512) % 4]
                        for jb in range(n // 128):
                            op("pe", lambda: P_.transpose(tap[:, jb * 128:(jb + 1) * 128],
                                                          mt[:, jb * 128:(jb + 1) * 128], identb[:]),
                               rd=[md, dC], wr=[tdep] if jb == 0 else (), acc=() if jb == 0 else [tdep])
                        op("act", lambda: A_.copy(out=maskT[:, c0:c0 + n], in_=tap[:, 0:n]),
                           rd=[tdep], wr=[dmk] if c0 == 0 else (), acc=() if c0 == 0 else [dmk])

                def emit_pass(fox):
                    units = [(bb_, grp) for bb_ in range(NB) for grp in range(2)]
                    bufs = {}
                    state = {}

                    def stage_a(ui):
                        b, grp = units[ui]
                        b2, bb = (b // 2) * 2, b % 2
                        if b2 not in bufs:
                            kt_, kd = ktr.nxt()
                            dma("sp", kt_[:], KT[8 * fox:8 * fox + 8, :, b2 * 128:b2 * 128 + 256].rearrange("h d s -> d h s"),
                                rd=[dKT], wr=[kd], own=kd)
                            vt_, vd = vcr.nxt()
                            dma("sp", vt_[:], V[b2 * 128:b2 * 128 + 256, 1024 * fox:1024 * fox + 1024].rearrange("(b s) c -> s b c", s=128),
                                rd=[dV], wr=[vd], own=vd)
                            fl_, fd = None, None
                            if fox:
                                fl_, fd = flr.nxt()
                                dma("sp", fl_[:], FIXL[:, :, b2 * 128:b2 * 128 + 256], rd=[dFIXL], wr=[fd], own=fd)
                            bufs[b2] = (kt_, kd, vt_, vd, fl_, fd)
                        kt_, kd, vt_, vd, fl_, fd = bufs[b2]
                        m = bq - b
                        ws = o + 128 * m + 128
                        pb = 4 + ui % 3
                        for hh in range(4):
                            h = 4 * grp + hh
                            reg = PS[pb][:, hh * 128:(hh + 1) * 128]
                            extra = (fox == 1) or (m <= 1)
                            op("pe", lambda: P_.matmul(reg, kt_[:, h, bb * 128:(bb + 1) * 128], qT[:, 8 * fox + h, :],
                                                       start=True, stop=not extra),
                               rd=[kd, dq], wr=[PSD[pb][hh]])
                            if fox:
                                last = not (m <= 0)
                                op("pe", lambda: P_.matmul(reg, fl_[0:8, h, bb * 128:(bb + 1) * 128], fixr[0:8, h, :],
                                                           start=False, stop=last),
                                   rd=[fd, dfr], acc=[PSD[pb][hh]])
                                if m <= 0:
                                    op("pe", lambda: P_.matmul(reg, identb[:], Cb[:, ws:ws + 128], start=False, stop=True),
                                       rd=[dC, dK2], acc=[PSD[pb][hh]])
                            elif m <= 1:
                                op("pe", lambda: P_.matmul(reg, identb[:], bankH[:, h, ws:ws + 128], start=False, stop=True),
                                   rd=[dC, dKb], acc=[PSD[pb][hh]])
                        pt, pd = ptr_.nxt()
                        op("act", lambda: A_.activation(out=pt[:], in_=PS[pb][:, :], func=AF.Exp), rd=PSD[pb], wr=[pd])
                        if fox:
                            pm_, pmd = pt, pd
                        else:
                            pm_, pmd = pmr.nxt()
                            op("dve", lambda: V_.tensor_tensor(
                                out=pm_[:].rearrange("p (h t) -> p h t", h=4),
                                in0=pt[:].rearrange("p (h t) -> p h t", h=4),
                                in1=maskT[:, b * 128:(b + 1) * 128].unsqueeze(1).broadcast_to([128, 4, 128]),
                                op=ALU.mult), rd=[pd, dmk], wr=[pmd])
                        state[ui] = (pm_, pmd, vt_, vd)

                    def stage_b(ui):
                        b, grp = units[ui]
                        bb = b % 2
                        pm_, pmd, vt_, vd = state.pop(ui)
                        for hh in range(4):
                            h = 4 * grp + hh
                            first = (b == 0 and hh == 0)
                            op("pe", lambda: P_.matmul(PS[grp][:, hh * 128:(hh + 1) * 128],
                                                       vt_[:, bb, h * 128:(h + 1) * 128], pm_[:, hh * 128:(hh + 1) * 128],
                                                       start=first, stop=(b == NB - 1), skip_group_check=True),
                               rd=[vd, pmd], wr=PSD[grp] if first else (), acc=() if first else PSD[grp])
                        op("pe", lambda: P_.matmul(PS[2 + grp][:, :], onesb[:], pm_[:], start=(b == 0), stop=(b == NB - 1)),
                           rd=[dC, pmd], wr=PSD[2 + grp] if b == 0 else (), acc=() if b == 0 else PSD[2 + grp])

                    LAG = 2
                    for ui in range(len(units) + LAG):
                        if ui < len(units):
                            stage_a(ui)
                        if ui - LAG >= 0:
                            stage_b(ui - LAG)

                def emit_norm(fox):
                    for grp in range(2):
                        op("dve", lambda: V_.tensor_scalar(out=rr[:, grp * 512:(grp + 1) * 512], in0=PS[2 + grp][:, :], scalar1=1e-30,
                                                           scalar2=None, op0=ALU.max), rd=PSD[2 + grp], wr=[drr] if grp == 0 else (),
                           acc=() if grp == 0 else [drr])
                    op("dve", lambda: V_.reciprocal(out=rr[:], in_=rr[:]), rd=[drr], wr=[drr])
                    for grp in range(2):
                        first = (fox == 1 and grp == 0)
                        if fox:
                            op("dve", lambda: V_.tensor_tensor(out=rr[:, grp * 512:(grp + 1) * 512], in0=rr[:, grp * 512:(grp + 1) * 512],
                                                               in1=GT[:, 4 * grp:4 * grp + 4, :].rearrange("p h t -> p (h t)"),
                                                               op=ALU.mult), rd=[drr, dGT], wr=[drr])
                        op("dve", lambda: V_.tensor_tensor(out=mixT[:, 8 * fox + 4 * grp: 8 * fox + 4 * grp + 4, :].rearrange("p h t -> p (h t)"),
                                                           in0=PS[grp][:, :], in1=rr[:, grp * 512:(grp + 1) * 512], op=ALU.mult),
                           rd=PSD[grp] + [drr], wr=[dmx] if first else (), acc=() if first else [dmx])

                emit_pass(1)
                emit_topk()
                emit_norm(1)
                emit_pass(0)
                emit_norm(0)
                dma("sp", MIX[k], mixT[:].rearrange("p h t -> p (h t)"), rd=[dmx], acc=[dMIX], own=dmx)
            s.barrier()

        with ExitStack() as ph:
            CB = 3
            xs = sbt(ph, "xs", [128, CB, D], F32)
            dxs = [Dep() for _ in range(CB)]
            h2 = sbt(ph, "h2", [128, 16, CB * 128], BF)
            dh2 = Dep()
            ntt = nt_tiles(ph)
            mixb = sbt(ph, "mixb", [128, CB, 16, 128], BF)
            dmb = [Dep() for _ in range(CB)]
            wo = sbt(ph, "wo", [128, 16 * 512], BF)
            dwo = Dep()
            gA = sbt(ph, "gA", [128, D], F32)
            gM = sbt(ph, "gM", [128, D], F32)
            gFt = sbt(ph, "gFt", [128, D], F32)
            dgt = Dep()
            wgr = ring(ph, "wgu", 3, [128, 2048], BF)
            aT = sbt(ph, "aT", [128, NFC, CB * 128], BF)
            daT = Dep()
            t1r = ring(ph, "t1", 2, [128, 512], F32)
            t2r = ring(ph, "t2", 2, [128, 512], F32)
            wdr = ring(ph, "wd", 3, [128, 2048], BF)
            cw = sbt(ph, "cw", [128, 3 * NG_UP], F32)
            cb = sbt(ph, "cb", [128, NG_UP], F32)
            dgr = sbt(ph, "dgr", [128, 128], F32)
            ddgr = Dep()
            dma("sp", cw[:], cw_d, wr=[dgt], own=dgt)
            dma("sp", cb[:], cb_d, acc=[dgt], own=dgt)
            dma("sp", gFt[:], gF_d, acc=[dgt], own=dgt)
            for (gt_, col0) in ((gA, 32), (gM, 80)):
                for c in range(16):
                    op("dve", lambda: V_.tensor_scalar(out=dgr[:], in0=identf[:], scalar1=modT[:, col0 + c:col0 + c + 1], scalar2=None,
                                                       op0=ALU.mult), rd=[dC, dmod], wr=[ddgr])
                    b = next_bank()
                    op("pe", lambda: P_.matmul(PS[b][:, 0:128], onesf[:], dgr[:], start=True, stop=True), rd=[dC, ddgr], wr=PSD[b])
                    op("act", lambda: A_.copy(out=gt_[:, c * 128:(c + 1) * 128], in_=PS[b][:, 0:128]), rd=PSD[b], acc=[dgt])

            for k0 in range(0, NT_R, CB):
                nt_ = min(CB, NT_R - k0)
                ntok = nt_ * 128
                for ti in range(nt_):
                    k = k0 + ti
                    dma("sp", xs[:, ti, :], xl[Lk(k):Lk(k) + 128, :], wr=[dxs[ti]], own=dxs[ti])
                    dma("sp", mixb[:, ti].rearrange("p h t -> p (h t)"), MIX[k], rd=[dMIX], wr=[dmb[ti]], own=dmb[ti])
                for cg in range(4):
                    dma("pool", wo[:], wout_d[cg], wr=[dwo], own=dwo, max_dma_last_dim=8192)
                    for ti in range(nt_):
                        b = next_bank()
                        for c in range(16):
                            op("pe", lambda: P_.matmul(PS[b][:, :], mixb[:, ti, c, :], wo[:, c * 512:(c + 1) * 512],
                                                       start=(c == 0), stop=(c == 15)),
                               rd=[dmb[ti], dwo], wr=PSD[b] if c == 0 else (), acc=() if c == 0 else PSD[b])
                        t1, t1d = t1r.nxt()
                        op("dve", lambda: V_.tensor_tensor(out=t1[:], in0=PS[b][:, :], in1=gA[:, cg * 512:(cg + 1) * 512], op=ALU.mult),
                           rd=PSD[b] + [dgt], wr=[t1d])
                        op("dve", lambda: V_.tensor_tensor(out=xs[:, ti, cg * 512:(cg + 1) * 512], in0=xs[:, ti, cg * 512:(cg + 1) * 512],
                                                           in1=t1[:], op=ALU.add), rd=[t1d], wr=[dxs[ti]])
                for ti in range(nt_):
                    norm_transpose(ntt, xs[:, ti, :], dxs[ti], aM, bM, lambda c: h2[:, c, ti * 128:(ti + 1) * 128], dh2, ti == 0)
                for g in range(NFC):
                    res = []
                    for half in range(2):
                        gg = g + NFC * half
                        wt, wd = wgr.nxt()
                        dma("pool", wt[:], wup_d[gg], wr=[wd], own=wd)
                        b = next_bank()
                        for c in range(16):
                            op("pe", lambda: P_.matmul(PS[b][:, :ntok], wt[:, c * 128:(c + 1) * 128], h2[:, c, :ntok],
                                                       start=(c == 0), stop=(c == 15)),
                               rd=[wd, dh2], wr=PSD[b] if c == 0 else (), acc=() if c == 0 else PSD[b])
                        pv = PS[b][:, :ntok].rearrange("p (t r) -> p t r", r=128)
                        op("dve", lambda: V_.tensor_tensor(out=pv[:, :, 0:2], in0=pv[:, :, 0:2],
                                                           in1=hv[:, k0:k0 + nt_].unsqueeze(2).broadcast_to([128, nt_, 2]),
                                                           op=ALU.mult), rd=PSD[b] + [dC], wr=PSD[b])
                        t, td = (t1r if half == 0 else t2r).nxt()
                        tv = t[:, :ntok].rearrange("p (t r) -> p t r", r=128)
                        op("act", lambda: A_.activation(out=t[:, :ntok], in_=PS[b][:, :ntok], func=AF.Identity,
                                                        scale=cw[:, 2 * NG_UP + gg:2 * NG_UP + gg + 1], bias=cb[:, gg:gg + 1]),
                           rd=PSD[b] + [dgt], wr=[td])
                        op("dve", lambda: V_.scalar_tensor_tensor(out=tv[:, :, 2:128], in0=pv[:, :, 1:127],
                                                                  scalar=cw[:, NG_UP + gg:NG_UP + gg + 1], in1=tv[:, :, 2:128],
                                                                  op0=ALU.mult, op1=ALU.add), rd=PSD[b] + [td, dgt], wr=[td])
                        op("dve", lambda: V_.scalar_tensor_tensor(out=tv[:, :, 2:128], in0=pv[:, :, 0:126],
                                                                  scalar=cw[:, gg:gg + 1], in1=tv[:, :, 2:128],
                                                                  op0=ALU.mult, op1=ALU.add), rd=PSD[b] + [td, dgt], wr=[td])
                        res.append((t, td))
                    (tg, tgd), (tv_, tvd) = res
                    op("act", lambda: A_.activation(out=tg[:, :ntok], in_=tg[:, :ntok], func=AF.Silu), rd=[tgd], wr=[tgd])
                    op("dve", lambda: V_.tensor_tensor(out=aT[:, g, :ntok], in0=tg[:, :ntok], in1=tv_[:, :ntok], op=ALU.mult),
                       rd=[tgd, tvd], wr=[daT] if g == 0 else (), acc=() if g == 0 else [daT])
                for half in range(2):
                    for f in range(NFC):
                        wt, wd = wdr.nxt()
                        dma("pool", wt[:, 0:1024], wdown_d[f, :, half * 1024:(half + 1) * 1024], wr=[wd], own=wd)
                        for ti in range(nt_):
                            for c2 in range(2):
                                bk = ti * 2 + c2
                                op("pe", lambda: P_.matmul(PS[bk][:, :], aT[:, f, ti * 128:(ti + 1) * 128], wt[:, c2 * 512:(c2 + 1) * 512],
                                                           start=(f == 0), stop=(f == NFC - 1)),
                                   rd=[wd, daT], wr=PSD[bk] if f == 0 else (), acc=() if f == 0 else PSD[bk])
                    for ti in range(nt_):
                        for c2 in range(2):
                            bk = ti * 2 + c2
                            cg = half * 2 + c2
                            t1, t1d = t1r.nxt()
                            op("dve", lambda: V_.tensor_tensor(out=t1[:], in0=PS[bk][:, :], in1=gM[:, cg * 512:(cg + 1) * 512], op=ALU.mult),
                               rd=PSD[bk] + [dgt], wr=[t1d])
                            op("dve", lambda: V_.tensor_tensor(out=xs[:, ti, cg * 512:(cg + 1) * 512], in0=xs[:, ti, cg * 512:(cg + 1) * 512],
                                                               in1=t1[:], op=ALU.add), rd=[t1d], wr=[dxs[ti]])
                for ti in range(nt_):
                    k = k0 + ti
                    junk, ss, ms, sd, rstd, xn, dsm2, dxn = ntt
                    xa = xs[:, ti, :]
                    op("act", lambda: A_.activation(out=junk[:], in_=xa, func=AF.Square, accum_out=ss[:]), rd=[dxs[ti]], wr=[dsm2])
                    op("dve", lambda: V_.tensor_scalar(out=ms[:], in0=ss[:], scalar1=1.0 / D, scalar2=EPS, op0=ALU.mult, op1=ALU.add),
                       rd=[dsm2], wr=[dsm2])
                    op("act", lambda: A_.activation(out=sd[:], in_=ms[:], func=AF.Sqrt), rd=[dsm2], wr=[dsm2])
                    op("dve", lambda: V_.reciprocal(out=rstd[:], in_=sd[:]), rd=[dsm2], wr=[dsm2])
                    op("dve", lambda: V_.scalar_tensor_tensor(out=xn[:], in0=xa, scalar=rstd[:, 0:1], in1=gFt[:], op0=ALU.mult, op1=ALU.mult),
                       rd=[dsm2, dxs[ti], dgt], wr=[dxn])
                    dma("sp", y[k * 128:(k + 1) * 128, :], xn[:], rd=[dxn], acc=[dY], own=dxn)
            s.barrier()
    return nc


def _t5_bucket_np(d):
    d = np.maximum(d, 0)
    df = np.maximum(d, 1).astype(np.float32)
    large = 16 + (np.log(df / np.float32(16)) / np.float32(math.log(8.0)) * np.float32(16)).astype(np.int32)
    large = np.minimum(large, 31)
    return np.where(d < 16, d, large)


def _lay(w, gsz):
    ncols = w.shape[1]
    G = ncols // gsz
    return np.ascontiguousarray(w.reshape(16, 128, G, gsz).transpose(2, 1, 0, 3)).reshape(G, 128, 16 * gsz)


_PROGRAM = None


def prep(x, c, rel_bias, w_ada, b_ada, g_attn, w_in, b_forget, w_out, g_mlp, w_up, conv_w, conv_b, w_down, g_final):
    f32 = np.float32
    x = np.asarray(x, f32)[0]
    w_in = np.asarray(w_in, f32)[0]
    qa, ka, va = w_in[:, 0:1024], w_in[:, 1024:2048], w_in[:, 2048:3072]
    qi, ki, wi = w_in[:, 3072:3584], w_in[:, 3584:3648], w_in[:, 3648:3656]
    qb, kb, vb = w_in[:, 3656:4680], w_in[:, 4680:5704], w_in[:, 5704:6728]
    gb, fb = w_in[:, 6728:7752], w_in[:, 7752:7760]

    common = {}
    common["cT"] = np.ascontiguousarray(np.asarray(c, f32)[0].reshape(16, 128).T)
    common["gattn"] = np.ascontiguousarray(np.asarray(g_attn, f32)[0].reshape(16, 128).T)
    common["gmlp"] = np.ascontiguousarray(np.asarray(g_mlp, f32)[0].reshape(16, 128).T)
    common["bada"] = np.ascontiguousarray(np.asarray(b_ada, f32)[0].reshape(96, 128).T)
    common["wada"] = _lay(np.asarray(w_ada, f32)[0], 128)
    common["wkf"] = _lay(np.concatenate([ka, kb, ki, ki], axis=1), 128)
    wfb = np.zeros((128, 128), f32)
    wfb[:, :] = fb.reshape(16, 128, 8).transpose(1, 0, 2).reshape(128, 128)
    common["wfb"] = wfb
    common["wv"] = np.ascontiguousarray(np.concatenate([va, vb], axis=1).reshape(16, 128, 2048).transpose(1, 0, 2)).reshape(128, 16 * 2048)
    common["wqf"] = _lay(np.concatenate([qa, qb, gb, qi], axis=1), 128)
    common["wwi"] = np.ascontiguousarray(wi.reshape(16, 128, 8).transpose(1, 0, 2)).reshape(128, 128)
    common["bfor"] = np.ascontiguousarray(np.repeat(np.asarray(b_forget, f32)[0], 16).reshape(128, 1))
    wo = np.asarray(w_out, f32)[0]
    common["wout"] = np.ascontiguousarray(wo.reshape(16, 128, 4, 512).transpose(2, 1, 0, 3)).reshape(4, 128, 16 * 512)
    common["wup"] = _lay(np.asarray(w_up, f32)[0], 128)
    cwv = np.asarray(conv_w, f32)[0]
    common["cw"] = np.ascontiguousarray(cwv.reshape(3, NG_UP, 128).transpose(2, 0, 1)).reshape(128, 3 * NG_UP)
    common["cb"] = np.ascontiguousarray(np.asarray(conv_b, f32)[0].reshape(NG_UP, 128).T)
    common["wdown"] = np.ascontiguousarray(np.asarray(w_down, f32)[0].reshape(NFC, 128, 2048))
    common["gF"] = np.ascontiguousarray(np.broadcast_to(np.asarray(g_final, f32)[None, :], (128, D)))
    common["identf"] = np.eye(128, dtype=f32)
    tm = np.zeros((128, 128), f32)
    for h in range(8):
        for s2 in range(16):
            tm[h * 16: h * 16 + s2, h * 16 + s2] = 1.0
    common["tm"] = tm
    tl = np.arange(128)[:, None]
    u = np.arange(512)[None, :]
    common["Ib"] = np.where(u - 128 <= tl, 0.0, NEG).astype(f32)
    ub = np.arange(BW)[None, :]
    dd = ub - tl - 128
    common["Cb"] = np.where(dd < 0, NEG, 0.0).astype(f32)
    rb = np.asarray(rel_bias, f32)
    bk = _t5_bucket_np(dd)
    bank = rb[bk]
    bank = np.where((dd >= 0)[:, :, None], bank, np.float32(0.0))
    common["bankB"] = np.ascontiguousarray(bank.transpose(0, 2, 1)).reshape(128, 8 * BW).astype(f32)
    common["rb31"] = np.ascontiguousarray(np.broadcast_to(rb[31][None, :], (128, 8))).astype(f32)

    in_maps = []
    for i in range(NCORE):
        pad = pad_of(i)
        n = min(SEQ, NTOK - pad)
        xloc = np.zeros((NTOK, D), f32)
        xloc[pad:pad + n] = x[:n]
        p = np.arange(NTOK)
        isp = (p < pad) | (p >= pad + SEQ)
        padrow = np.where(isp, NEG, 0.0).astype(f32)
        m = dict(common)
        m["xl"] = xloc
        m["padb"] = np.ascontiguousarray(np.broadcast_to(padrow[None, :1024], (128, 1024)))
        hvv = np.ones((NT,), f32)
        if i == 0:
            hvv[0] = 0.0
        m["hv"] = np.ascontiguousarray(np.broadcast_to(hvv[None, :], (128, NT)))
        fl_t = np.zeros((8, NTOK), f32)
        fl_t[0:3] = 1.0
        fl_t[6] = padrow
        fr_t = np.zeros((8, NTOK), f32)
        fr_t[3:7] = 1.0
        m["fixl_t"] = fl_t.astype(ml_dtypes.bfloat16)
        m["fixr_t"] = fr_t.astype(ml_dtypes.bfloat16)
        in_maps.append(m)
    return in_maps


def kernel(**inputs):
    global _PROGRAM
    f32 = np.float32
    in_maps = prep(**inputs)
    if _PROGRAM is None:
        _PROGRAM = build_program()
    res = run_bass_kernel_spmd(_PROGRAM, in_maps, core_ids=list(range(NCORE)), trace=True)
    out = np.zeros((1, SEQ, D), f32)
    for i in range(NCORE):
        yi = np.asarray(res.results[i]["y"]).reshape(NT, 128, D)
        for k in range(NT):
            j = 8 * k + i
            g0 = 126 * j
            if g0 >= SEQ:
                continue
            cnt = min(126, SEQ - g0)
            out[0, g0:g0 + cnt] = yi[k, 2:2 + cnt]
    return out
```
